# Optimizing a Trainium2 kernel written in Bass

```python
import numpy as np
import jax
import jax.numpy as jnp
from jax import lax

D_MODEL = 2048
BATCH = 1
SEQ = 8192
DEPTH = 4

POOL_WINDOWS = (2, 4, 8, 16)
N_POOL_GROUPS = 4
POOL_WIDTH = D_MODEL // 2
POOL_GROUP = POOL_WIDTH // N_POOL_GROUPS
HEAD_DIM = 128
ATTN_WIDTH = D_MODEL
N_HEADS = ATTN_WIDTH // HEAD_DIM
MOBA_BLOCK = 256
MOBA_TOPK = 3
Q_CHUNK = 32
IN_WIDTH = POOL_WIDTH + 3 * ATTN_WIDTH + 2 * D_MODEL
SPLITS = tuple(int(s) for s in np.cumsum([POOL_WIDTH, ATTN_WIDTH, ATTN_WIDTH, ATTN_WIDTH, D_MODEL]))
D_FF = 256 * ((8 * D_MODEL // 3 + 255) // 256)
N_EXPERTS = 8
MOE_TOPK = 2
D_FF_EXPERT = 7 * D_MODEL // 2
ROW_BLOCK = 512
N_DENSE = (DEPTH + 1) // 2
N_MOE = DEPTH // 2
ALPHA = (2 * DEPTH) ** 0.25
BETA = (8 * DEPTH) ** -0.25
LN_EPS = 1e-5

kernel_name = 'hybrid_pool_moba_moe_deepnorm'


def layer_norm(x, g, b):
    xf = x.astype(jnp.float32)
    mu = jnp.mean(xf, axis=-1, keepdims=True)
    var = jnp.mean(jnp.square(xf - mu), axis=-1, keepdims=True)
    return ((xf - mu) * lax.rsqrt(var + LN_EPS) * g + b).astype(x.dtype)


def pool_mixer(u, pool_w, pool_scale):
    B, S, _ = u.shape
    uf = u.astype(jnp.float32).reshape(B, S, N_POOL_GROUPS, POOL_GROUP)
    cs = jnp.cumsum(uf, axis=1)
    t = jnp.arange(S)
    outs = []
    for g, w in enumerate(POOL_WINDOWS):
        c = cs[:, :, g]
        lag = jnp.pad(c, ((0, 0), (w, 0), (0, 0)))[:, :S]
        cnt = jnp.minimum(t + 1, w).astype(jnp.float32)[None, :, None]
        outs.append((c - lag) / cnt - uf[:, :, g])
    pooled = jnp.stack(outs, axis=2).astype(u.dtype)
    y = jnp.einsum('bsgc,gcd->bsgd', pooled, pool_w)
    return y.reshape(B, S, POOL_WIDTH) * pool_scale


def moba_attention(q, k, v):
    B, H, S, Dh = q.shape
    nb = -(-S // MOBA_BLOCK)
    pad = nb * MOBA_BLOCK - S
    kb = jnp.pad(k, ((0, 0), (0, 0), (0, pad), (0, 0))).reshape(B, H, nb, MOBA_BLOCK, Dh)
    vb = jnp.pad(v, ((0, 0), (0, 0), (0, pad), (0, 0))).reshape(B, H, nb, MOBA_BLOCK, Dh)
    k_mean = jnp.sum(kb.astype(jnp.float32), axis=3) / MOBA_BLOCK
    n_sel = min(MOBA_TOPK, nb)
    scale = HEAD_DIM ** -0.5
    nq = S // Q_CHUNK
    q_chunks = q.reshape(B, H, nq, Q_CHUNK, Dh).transpose(2, 0, 1, 3, 4)
    gather_blocks = jax.vmap(jax.vmap(lambda blocks, idx: blocks[idx]))

    def one_chunk(args):
        c, qc = args
        t0 = c * Q_CHUNK
        blk = t0 // MOBA_BLOCK
        q_pos = t0 + jnp.arange(Q_CHUNK)
        gate = jnp.einsum('bhqd,bhnd->bhqn', qc.astype(jnp.float32), k_mean)
        gate = jnp.where(jnp.arange(nb) < blk, gate, -jnp.inf)
        _, sel = lax.top_k(gate, n_sel)
        sel_valid = jnp.arange(n_sel) < blk
        k_sel = gather_blocks(kb, sel)
        v_sel = gather_blocks(vb, sel)
        s_sel = jnp.einsum('bhqd,bhqrkd->bhqrk', qc, k_sel).astype(jnp.float32) * scale
        s_sel = jnp.where(sel_valid[:, None], s_sel, -jnp.inf)
        k_own = lax.dynamic_index_in_dim(kb, blk, axis=2, keepdims=False)
        v_own = lax.dynamic_index_in_dim(vb, blk, axis=2, keepdims=False)
        s_own = jnp.einsum('bhqd,bhkd->bhqk', qc, k_own).astype(jnp.float32) * scale
        k_pos = blk * MOBA_BLOCK + jnp.arange(MOBA_BLOCK)
        s_own = jnp.where(k_pos[None, :] <= q_pos[:, None], s_own, -jnp.inf)
        s = jnp.concatenate([s_sel.reshape(B, H, Q_CHUNK, n_sel * MOBA_BLOCK), s_own], axis=-1)
        p = jax.nn.softmax(s, axis=-1).astype(v.dtype)
        p_sel = p[..., :n_sel * MOBA_BLOCK].reshape(B, H, Q_CHUNK, n_sel, MOBA_BLOCK)
        p_own = p[..., n_sel * MOBA_BLOCK:]
        return (jnp.einsum('bhqrk,bhqrkd->bhqd', p_sel, v_sel)
                + jnp.einsum('bhqk,bhkd->bhqd', p_own, v_own))

    out = lax.map(one_chunk, (jnp.arange(nq), q_chunks))
    return out.transpose(1, 2, 0, 3, 4).reshape(B, H, S, Dh)


def hybrid_mixer(x, w_in, pool_w, pool_scale, w_up_pool, w_up_attn, w_o):
    B, S, _ = x.shape
    z = x @ w_in
    u, q, k, v, g_pool, g_attn = jnp.split(z, SPLITS, axis=-1)
    y_pool = pool_mixer(u, pool_w, pool_scale) @ w_up_pool
    to_heads = lambda t: t.reshape(B, S, N_HEADS, HEAD_DIM).transpose(0, 2, 1, 3)
    o = moba_attention(to_heads(q), to_heads(k), to_heads(v))
    y_attn = o.transpose(0, 2, 1, 3).reshape(B, S, ATTN_WIDTH) @ w_up_attn
    m = jax.nn.sigmoid(g_pool) * y_pool + jax.nn.sigmoid(g_attn) * y_attn
    return m @ w_o


def swiglu(h, w_gate, w_up, w_down):
    return (jax.nn.silu(h @ w_gate) * (h @ w_up)) @ w_down


def moe_swiglu(h, router_w, w_gate, w_up, w_down):
    T, D = h.shape
    logits = (h @ router_w).astype(jnp.float32)
    top_logit, top_e = lax.top_k(logits, MOE_TOPK)
    top_w = jax.nn.softmax(top_logit, axis=-1)
    n_assign = T * MOE_TOPK
    flat_e = top_e.reshape(-1)
    flat_tok = jnp.arange(n_assign, dtype=jnp.int32) // MOE_TOPK
    flat_w = top_w.reshape(-1)
    order = jnp.argsort(flat_e)
    se, stok, sw = flat_e[order], flat_tok[order], flat_w[order]
    counts = jnp.bincount(flat_e, length=N_EXPERTS)
    padded = (counts + ROW_BLOCK - 1) // ROW_BLOCK * ROW_BLOCK
    start = jnp.cumsum(counts) - counts
    ends = jnp.cumsum(padded)
    pstart = ends - padded
    dest = pstart[se] + jnp.arange(n_assign) - start[se]
    n_rows = (-(-n_assign // ROW_BLOCK) + N_EXPERTS) * ROW_BLOCK
    n_blocks = n_rows // ROW_BLOCK
    row_tok = jnp.full((n_rows,), T, dtype=jnp.int32).at[dest].set(stok)
    row_w = jnp.zeros((n_rows,), jnp.float32).at[dest].set(sw)
    block_e = jnp.minimum(jnp.searchsorted(ends, jnp.arange(n_blocks) * ROW_BLOCK, side='right'),
                          N_EXPERTS - 1)
    h_pad = jnp.concatenate([h, jnp.zeros((1, D), h.dtype)], axis=0)

    def expert_block(args):
        e, toks = args
        xb = h_pad[toks]
        return swiglu(xb, w_gate[e], w_up[e], w_down[e])

    y = lax.map(expert_block, (block_e, row_tok.reshape(n_blocks, ROW_BLOCK)))
    y = y.reshape(n_rows, D) * row_w[:, None].astype(y.dtype)
    out = jnp.zeros((T + 1, D), y.dtype).at[row_tok].add(y)
    return out[:T]


def setup_inputs(seed: int = 0) -> dict:
    key = jax.random.key(seed)
    ks = jax.random.split(key, 20)
    D = D_MODEL
    nrm = lambda k, shape, s: jax.random.normal(k, shape, jnp.float32) * s
    return {
        'x': nrm(ks[0], (BATCH, SEQ, D), 1.0),
        'ln_in_g': 1.0 + nrm(ks[1], (D,), 0.05),
        'ln_in_b': nrm(ks[2], (D,), 0.02),
        'w_in': nrm(ks[3], (DEPTH, D, IN_WIDTH), D ** -0.5),
        'pool_w': nrm(ks[4], (DEPTH, N_POOL_GROUPS, POOL_GROUP, POOL_GROUP), POOL_GROUP ** -0.5),
        'pool_scale': 1.0 + nrm(ks[5], (DEPTH, POOL_WIDTH), 0.1),
        'w_up_pool': nrm(ks[6], (DEPTH, POOL_WIDTH, D), POOL_WIDTH ** -0.5),
        'w_up_attn': nrm(ks[7], (DEPTH, ATTN_WIDTH, D), ATTN_WIDTH ** -0.5),
        'w_o': nrm(ks[8], (DEPTH, D, D), BETA * D ** -0.5),
        'ln_mix_g': 1.0 + nrm(ks[9], (DEPTH, D), 0.05),
        'ln_mix_b': nrm(ks[10], (DEPTH, D), 0.02),
        'ffn_w_gate': nrm(ks[11], (N_DENSE, D, D_FF), D ** -0.5),
        'ffn_w_up': nrm(ks[12], (N_DENSE, D, D_FF), D ** -0.5),
        'ffn_w_down': nrm(ks[13], (N_DENSE, D_FF, D), BETA * D_FF ** -0.5),
        'moe_router': nrm(ks[14], (N_MOE, D, N_EXPERTS), D ** -0.5),
        'moe_w_gate': nrm(ks[15], (N_MOE, N_EXPERTS, D, D_FF_EXPERT), D ** -0.5),
        'moe_w_up': nrm(ks[16], (N_MOE, N_EXPERTS, D, D_FF_EXPERT), D ** -0.5),
        'moe_w_down': nrm(ks[17], (N_MOE, N_EXPERTS, D_FF_EXPERT, D), BETA * D_FF_EXPERT ** -0.5),
        'ln_ffn_g': 1.0 + nrm(ks[18], (DEPTH, D), 0.05),
        'ln_ffn_b': nrm(ks[19], (DEPTH, D), 0.02),
    }


def reference(x, ln_in_g, ln_in_b, w_in, pool_w, pool_scale, w_up_pool, w_up_attn, w_o,
              ln_mix_g, ln_mix_b, ffn_w_gate, ffn_w_up, ffn_w_down, moe_router, moe_w_gate,
              moe_w_up, moe_w_down, ln_ffn_g, ln_ffn_b):
    B, S, D = x.shape
    x = layer_norm(x, ln_in_g, ln_in_b)
    for l in range(DEPTH):
        mix = hybrid_mixer(x, w_in[l], pool_w[l], pool_scale[l], w_up_pool[l], w_up_attn[l], w_o[l])
        x = layer_norm(ALPHA * x + mix, ln_mix_g[l], ln_mix_b[l])
        i = l // 2
        if l % 2 == 0:
            f = swiglu(x, ffn_w_gate[i], ffn_w_up[i], ffn_w_down[i])
        else:
            f = moe_swiglu(x.reshape(B * S, D), moe_router[i], moe_w_gate[i], moe_w_up[i],
                           moe_w_down[i]).reshape(B, S, D)
        x = layer_norm(ALPHA * x + f, ln_ffn_g[l], ln_ffn_b[l])
    return x
```

```python
import contextlib
import numpy as np
import ml_dtypes
import concourse.bass as bass
import concourse.mybir as mybir
from concourse.bass_utils import run_bass_kernel_spmd

F32 = mybir.dt.float32
BF16 = mybir.dt.bfloat16
ALU = mybir.AluOpType
AF = mybir.ActivationFunctionType
AX = mybir.AxisListType
NPBF = ml_dtypes.bfloat16

NCORES = 8
D = 2048
SEQ = 8192
DEPTH = 4
TOK = SEQ // NCORES
HALO = 16
POOL_WINDOWS = (2, 4, 8, 16)
POOL_WIDTH = 1024
NH = 16
DH = 128
BLK = 256
NBLK = SEQ // BLK
TOPK = 3
IN_WIDTH = POOL_WIDTH + 3 * D + 2 * D
D_FF = 5632
NE = 8
D_FFE = 7168
ALPHA = (2 * DEPTH) ** 0.25
LN_EPS = 1e-5

COMPUTE = ("pe", "act", "dve", "pool")


class Buf:
    __slots__ = ("name", "w", "r", "dsem")

    def __init__(self, name):
        self.name = name
        self.w = None
        self.r = []
        self.dsem = None


class Prog:
    def __init__(self):
        self.nc = bass.Bass("TRN2", target_bir_lowering=False)
        self.es = contextlib.ExitStack()
        self.streams = {e: [] for e in ("pe", "act", "dve", "pool", "sp")}
        self.sems = {}
        self.count = {}
        self.waited = {e: {} for e in self.streams}
        self.ndsem = 0
        for e in COMPUTE:
            self._mksem("c_" + e)
        self.out_bufs = []

    def _mksem(self, key):
        self.sems[key] = self.es.enter_context(self.nc.semaphore(key))
        self.count[key] = 0
        return key

    def dram_in(self, name, shape, dtype):
        return self.nc.dram_tensor(name, list(shape), dtype, kind="ExternalInput").ap()

    def dram_out(self, name, shape, dtype):
        return self.nc.dram_tensor(name, list(shape), dtype, kind="ExternalOutput").ap()

    def sbuf(self, name, shape, dtype):
        return self.es.enter_context(self.nc.sbuf_tensor(name, list(shape), dtype))

    def psum(self, name, shape, dtype=F32):
        return self.es.enter_context(self.nc.psum_tensor(name, list(shape), dtype))

    def _need(self, eng, ev):
        if ev is None:
            return
        key, val = ev
        if self.waited[eng].get(key, 0) >= val:
            return
        self.waited[eng][key] = val
        self.streams[eng].append(("wait", key, val))

    def _deps(self, eng, reads, writes):
        for b in reads:
            self._need(eng, b.w)
        for b in writes:
            self._need(eng, b.w)
            for ev in b.r:
                self._need(eng, ev)

    def _commit(self, ev, reads, writes):
        for b in reads:
            b.r.append(ev)
        for b in writes:
            b.w = ev
            b.r = []

    def op(self, eng, fn, reads=(), writes=(), signal=True):
        self._deps(eng, reads, writes)
        key = "c_" + eng
        if signal:
            self.count[key] += 1
            ev = (key, self.count[key])
            self.streams[eng].append(("op", fn, key, 1))
            self._commit(ev, reads, writes)
        else:
            self.streams[eng].append(("op", fn, None, 0))

    def dma(self, q, out_ap, in_ap, reads=(), writes=(), cont=False):
        bufs = list(reads) + list(writes)
        owner = bufs[0]
        if owner.dsem is None:
            owner.dsem = self._mksem("d%d_%s" % (self.ndsem, owner.name))
            self.ndsem += 1
        key = owner.dsem
        for b in bufs[1:]:
            assert b.dsem is None or b.dsem == key
            b.dsem = key
        if not cont:
            self._deps(q, reads, writes)
        self.count[key] += 16
        ev = (key, self.count[key])
        self.streams[q].append(("op", lambda e, o=out_ap, i=in_ap: e.dma_start(out=o, in_=i), key, 16))
        self._commit(ev, reads, writes)
        return ev

    def dma_out(self, q, out_ap, in_ap, src):
        ev = self.dma(q, out_ap, in_ap, reads=[src])
        self.final_events = getattr(self, "final_events", {})
        self.final_events[ev[0]] = ev[1]

    def finish(self):
        nc = self.nc
        for key, val in getattr(self, "final_events", {}).items():
            self.streams["sp"].append(("wait", key, val))
        for e in COMPUTE:
            if self.count["c_" + e]:
                self.streams["sp"].append(("wait", "c_" + e, self.count["c_" + e]))
        sems = self.sems
        streams = self.streams

        def replay(eng_obj, items):
            for it in items:
                if it[0] == "wait":
                    eng_obj.wait_ge(sems[it[1]], it[2])
                else:
                    ins = it[1](eng_obj)
                    if it[2] is not None:
                        ins.then_inc(sems[it[2]], it[3])

        with nc.Block() as block:
            @block.tensor
            def _(e):
                replay(e, streams["pe"])

            @block.scalar
            def _(e):
                replay(e, streams["act"])

            @block.vector
            def _(e):
                replay(e, streams["dve"])

            @block.gpsimd
            def _(e):
                replay(e, streams["pool"])

            @block.sync
            def _(e):
                replay(e, streams["sp"])
        self.es.close()
        return nc


def run_prog(nc, in_maps):
    res = run_bass_kernel_spmd(nc, in_maps, core_ids=list(range(len(in_maps))))
    return res.results


class PsumRing:
    def __init__(self, P, n, name="ps"):
        self.tiles = [P.psum("%s%d" % (name, i), [128, 512]) for i in range(n)]
        self.bufs = [Buf("%s%d" % (name, i)) for i in range(n)]
        self.i = 0

    def next(self):
        i = self.i % len(self.tiles)
        self.i += 1
        return self.tiles[i], self.bufs[i]


class WRing:
    def __init__(self, P, name, nslots, kc, width):
        self.P = P
        self.kc = kc
        self.width = width
        self.tiles = [P.sbuf("%s%d" % (name, i), [128, kc, width], BF16) for i in range(nslots)]
        self.bufs = [Buf("%s%d" % (name, i)) for i in range(nslots)]
        self.i = 0

    def load(self, w_ap, c0, width=None, kc=None, q="pool"):
        width = width or self.width
        kc = kc or self.kc
        i = self.i % len(self.tiles)
        self.i += 1
        t, b = self.tiles[i], self.bufs[i]
        src = w_ap.rearrange("(k p) f -> p k f", p=128)[:, :, c0:c0 + width]
        h = kc // 2 if kc >= 2 else kc
        self.P.dma(q, t[:, 0:h, 0:width], src[:, 0:h, :], writes=[b])
        if h < kc:
            self.P.dma(q, t[:, h:kc, 0:width], src[:, h:kc, :], writes=[b], cont=True)
        return t, b


def mm_group(P, ps_ap, ps_buf, pairs, reads):
    n = len(pairs)
    for i, (l, r) in enumerate(pairs):
        last = i == n - 1
        P.op("pe", lambda e, l=l, r=r, i=i, last=last: e.matmul(ps_ap, l, r, start=(i == 0), stop=last),
             reads=reads if (i == 0 or last) else (), writes=[ps_buf], signal=last)


def build_A():
    P = Prog()
    TH = TOK + HALO
    xT = P.dram_in("xT", [D, TH], BF16)
    w_in = P.dram_in("w_in", [D, IN_WIDTH], F32)
    pool_w = P.dram_in("pool_w", [4 * 256, 256], F32)
    pool_scale = P.dram_in("pool_scale", [128, 8], F32)
    cnt_tab = P.dram_in("cnt_tab", [128, 4 * HALO], F32)
    w_up_pool = P.dram_in("w_up_pool", [POOL_WIDTH, D], F32)
    qkvT = P.dram_out("qkvT", [3 * D, TOK], BF16)
    sgaT = P.dram_out("sgaT", [D, TOK], BF16)
    mpT = P.dram_out("mpT", [D, TOK], BF16)

    x_sb = P.sbuf("x_sb", [128, 16, TH], BF16)
    x_b = Buf("x")
    u_sb = P.sbuf("u_sb", [128, 8, TH], F32)
    u_b = [Buf("u%d" % c) for c in range(8)]
    pooled = P.sbuf("pooled", [128, 8, TOK], BF16)
    pooled_b = [Buf("pl%d" % c) for c in range(8)]
    ypool = P.sbuf("ypool", [128, 8, TOK], BF16)
    ypool_b = [Buf("yp%d" % c) for c in range(8)]
    tmpa = P.sbuf("tmpa", [128, TH], F32)
    tmpb = P.sbuf("tmpb", [128, TH], F32)
    tmpa_b, tmpb_b = Buf("tmpa"), Buf("tmpb")
    pw_sb = P.sbuf("pw_sb", [128, 8, 256], BF16)
    pw_b = Buf("pw")
    ps_sb = P.sbuf("ps_sb", [128, 8], F32)
    ps_b = Buf("psc")
    tab_sb = P.sbuf("tab_sb", [128, 4 * HALO], F32)
    tab_b = Buf("tab")
    NSTG = 4
    stg = [P.sbuf("stg%d" % i, [128, TOK], BF16) for i in range(NSTG)]
    stg_b = [Buf("stg%d" % i) for i in range(NSTG)]
    sg = [P.sbuf("sg%d" % i, [128, 4, TOK], BF16) for i in range(2)]
    sg_b = [Buf("sg%d" % i) for i in range(2)]
    ring = WRing(P, "win", 3, 16, 512)
    ring2 = WRing(P, "wup", 2, 8, 512)
    psr = PsumRing(P, 8)
    state = {"stg": 0, "evac": 0}

    xv = xT.rearrange("(k p) t -> p k t", p=128)
    for k0 in range(0, 16, 4):
        P.dma("sp", x_sb[:, k0:k0 + 4, :], xv[:, k0:k0 + 4, :], writes=[x_b], cont=k0 > 0)
    P.dma("sp", ps_sb[:, :], pool_scale[:, :], writes=[ps_b])
    P.dma("sp", tab_sb[:, :], cnt_tab[:, :], writes=[tab_b])
    P.dma("pool", pw_sb[:, :, :], pool_w.rearrange("(k p) f -> p k f", p=128), writes=[pw_b])

    def evac_engine():
        state["evac"] += 1
        return "act" if state["evac"] % 2 else "dve"

    def copy_out(eng, dst, src, reads, writes):
        if eng == "act":
            P.op("act", lambda e: e.activation(dst, src, AF.Copy), reads=reads, writes=writes)
        else:
            P.op("dve", lambda e: e.tensor_copy(dst, src), reads=reads, writes=writes)

    def inproj_group(j, kind):
        wt, wb = ring.load(w_in, j * 512)
        for m in range(4):
            f0 = j * 512 + m * 128
            if kind == "u":
                c = f0 // 128
                for (t0, tn) in ((HALO, 512), (HALO + 512, 512), (0, HALO)):
                    pt, pb = psr.next()
                    mm_group(P, pt[:, 0:tn], pb,
                             [(wt[:, k, m * 128:(m + 1) * 128], x_sb[:, k, t0:t0 + tn]) for k in range(16)],
                             reads=[wb, x_b])
                    copy_out(evac_engine(), u_sb[:, c, t0:t0 + tn], pt[:, 0:tn], [pb], [u_b[c]])
                continue
            if kind == "gp":
                dst_t, dst_b = sg[state["sgi"] % 2], sg_b[state["sgi"] % 2]
            else:
                si = state["stg"] % NSTG
                state["stg"] += 1
                dst_t, dst_b = stg[si], stg_b[si]
            for n in range(2):
                pt, pb = psr.next()
                mm_group(P, pt[:, :], pb,
                         [(wt[:, k, m * 128:(m + 1) * 128], x_sb[:, k, HALO + n * 512:HALO + (n + 1) * 512])
                          for k in range(16)], reads=[wb, x_b])
                if kind == "gp":
                    d = dst_t[:, m, n * 512:(n + 1) * 512]
                    P.op("act", lambda e, d=d, s=pt[:, :]: e.activation(d, s, AF.Sigmoid), reads=[pb], writes=[dst_b])
                elif kind == "ga":
                    d = dst_t[:, n * 512:(n + 1) * 512]
                    P.op("act", lambda e, d=d, s=pt[:, :]: e.activation(d, s, AF.Sigmoid), reads=[pb], writes=[dst_b])
                else:
                    copy_out(evac_engine(), dst_t[:, n * 512:(n + 1) * 512], pt[:, :], [pb], [dst_b])
            if kind == "qkv":
                r0 = f0 - POOL_WIDTH
                P.dma_out("sp", qkvT[r0:r0 + 128, :], dst_t[:, :], dst_b)
            elif kind == "ga":
                r0 = f0 - (POOL_WIDTH + 4 * D)
                P.dma_out("sp", sgaT[r0:r0 + 128, :], dst_t[:, :], dst_b)

    def pool_path():
        for c in range(8):
            g = c // 2
            w = POOL_WINDOWS[g]
            cur, cur_b = u_sb[:, c, :], u_b[c]
            s = 1
            outs = [(tmpa, tmpa_b), (tmpb, tmpb_b)]
            oi = 0
            while s < w:
                ot, ob = outs[oi % 2]
                oi += 1
                P.op("dve", lambda e, o=ot[:, s:TH], a=(cur[:, s:TH]), b=(cur[:, 0:TH - s]): e.tensor_tensor(o, a, b, ALU.add),
                     reads=[cur_b], writes=[ob])
                cur, cur_b = ot, ob
                s *= 2
            P.op("dve", lambda e, o=pooled[:, c, :], a=cur[:, HALO:TH], b=u_sb[:, c, HALO:TH], w=w:
                 e.scalar_tensor_tensor(o, a, 1.0 / w, b, ALU.mult, ALU.subtract),
                 reads=[cur_b, u_b[c]], writes=[pooled_b[c]])
            fix, fix_b = outs[oi % 2]
            P.op("dve", lambda e, o=fix[:, 0:HALO], a=cur[:, HALO:2 * HALO], b=tab_sb[:, g * HALO:(g + 1) * HALO]:
                 e.tensor_tensor(o, a, b, ALU.mult), reads=[cur_b, tab_b], writes=[fix_b])
            P.op("dve", lambda e, o=pooled[:, c, 0:HALO], a=fix[:, 0:HALO], b=u_sb[:, c, HALO:2 * HALO]:
                 e.tensor_tensor(o, a, b, ALU.subtract), reads=[fix_b, u_b[c]], writes=[pooled_b[c]])

    def poolw_mm():
        for c in range(8):
            g = c // 2
            mo = c % 2
            for n in range(2):
                pt, pb = psr.next()
                mm_group(P, pt[:, :], pb,
                         [(pw_sb[:, g * 2 + kk, mo * 128:(mo + 1) * 128], pooled[:, g * 2 + kk, n * 512:(n + 1) * 512])
                          for kk in range(2)], reads=[pw_b, pooled_b[g * 2], pooled_b[g * 2 + 1]])
                P.op("act", lambda e, d=ypool[:, c, n * 512:(n + 1) * 512], s=pt[:, :], sc=ps_sb[:, c:c + 1]:
                     e.activation(d, s, AF.Copy, scale=sc), reads=[pb, ps_b], writes=[ypool_b[c]])

    def uppool_group(j):
        wt, wb = ring2.load(w_up_pool, j * 512)
        sgt, sgb = sg[state["sgi"] % 2], sg_b[state["sgi"] % 2]
        for m in range(4):
            si = state["stg"] % NSTG
            state["stg"] += 1
            for n in range(2):
                pt, pb = psr.next()
                mm_group(P, pt[:, :], pb,
                         [(wt[:, k, m * 128:(m + 1) * 128], ypool[:, k, n * 512:(n + 1) * 512]) for k in range(8)],
                         reads=[wb] + ypool_b)
                P.op("dve", lambda e, d=stg[si][:, n * 512:(n + 1) * 512], a=pt[:, :], b=sgt[:, m, n * 512:(n + 1) * 512]:
                     e.tensor_tensor(d, a, b, ALU.mult), reads=[pb, sgb], writes=[stg_b[si]])
            r0 = j * 512 + m * 128
            P.dma_out("sp", mpT[r0:r0 + 128, :], stg[si][:, :], stg_b[si])

    state["sgi"] = 0
    inproj_group(0, "u")
    inproj_group(1, "u")
    for j in range(2, 6):
        inproj_group(j, "qkv")
    pool_path()
    poolw_mm()
    for j in range(6, 14):
        inproj_group(j, "qkv")
    for jj in range(4):
        state["sgi"] = jj
        inproj_group(14 + jj, "gp")
        uppool_group(jj)
    for j in range(18, 22):
        inproj_group(j, "ga")
    return P.finish()


def build_B(nblk=NBLK, hpc=2):
    P = Prog()
    S = nblk * BLK
    NT = S // 128
    qT = P.dram_in("qT", [hpc * 128, S], BF16)
    kT = P.dram_in("kT", [hpc * 128, S], BF16)
    v = P.dram_in("v", [hpc * S, 128], BF16)
    cmask = P.dram_in("cmask", [128, 512], BF16)
    o = P.dram_out("o", [hpc * S, 128], BF16)

    q_sb = [P.sbuf("q_sb%d" % h, [128, S], BF16) for h in range(hpc)]
    k_sb = [P.sbuf("k_sb%d" % h, [128, S], BF16) for h in range(hpc)]
    v_sb = [P.sbuf("v_sb%d" % h, [128, NT, 130], BF16) for h in range(hpc)]
    q_b = [Buf("q%d" % h) for h in range(hpc)]
    k_b = [Buf("k%d" % h) for h in range(hpc)]
    v_b = [Buf("v%d" % h) for h in range(hpc)]
    cm_sb = P.sbuf("cm_sb", [128, 512], BF16)
    cm_b = Buf("cm")
    km32 = P.sbuf("km32", [128, nblk], F32)
    kmh = P.sbuf("kmh", [128, nblk], BF16)
    kml32 = P.sbuf("kml32", [128, nblk], F32)
    kml = P.sbuf("kml", [128, nblk], BF16)
    km_b = Buf("km")
    NG = 2
    g_sb = [P.sbuf("g_sb%d" % i, [128, 2, 32], F32) for i in range(NG)]
    m_sb = [P.sbuf("m_sb%d" % i, [128, 2, 32], F32) for i in range(NG)]
    mx_sb = [P.sbuf("mx_sb%d" % i, [128, 2, 8], F32) for i in range(NG)]
    g_b = [Buf("g%d" % i) for i in range(NG)]
    m_b = [Buf("m%d" % i) for i in range(NG)]
    acc = [P.sbuf("acc%d" % i, [128, 2, 130], F32) for i in range(NG)]
    acc_b = [Buf("acc%d" % i) for i in range(NG)]
    rc = [P.sbuf("rc%d" % i, [128, 2], F32) for i in range(NG)]
    ob = [P.sbuf("ob%d" % i, [128, 2, 128], BF16) for i in range(NG)]
    ob_b = [Buf("ob%d" % i) for i in range(NG)]
    NPT = 3
    pT = [P.sbuf("pT%d" % i, [128, 512], BF16) for i in range(NPT)]
    pT_b = [Buf("pT%d" % i) for i in range(NPT)]
    ps_s = PsumRing(P, 3, "pss")
    ps_o = PsumRing(P, 3, "pso")
    ps_g = PsumRing(P, 2, "psg")
    scale = float(DH) ** -0.5

    P.dma("sp", cm_sb[:, :], cmask[:, :], writes=[cm_b])
    for h in range(hpc):
        P.dma("sp", q_sb[h][:, :], qT[h * 128:(h + 1) * 128, :], writes=[q_b[h]])
        P.dma("sp", k_sb[h][:, :], kT[h * 128:(h + 1) * 128, :], writes=[k_b[h]])
        P.op("pool", lambda e, t=v_sb[h]: e.memset(t[:, :, 128:130], 1.0), writes=[v_b[h]])
        vv = v[h * S:(h + 1) * S, :].rearrange("(t p) d -> p t d", p=128)
        half = NT // 2
        P.dma("sp", v_sb[h][:, 0:half, 0:128], vv[:, 0:half, :], writes=[v_b[h]])
        P.dma("sp", v_sb[h][:, half:NT, 0:128], vv[:, half:NT, :], writes=[v_b[h]], cont=True)

    pti = 0
    for h in range(hpc):
        P.op("dve", lambda e, h=h: e.tensor_reduce(km32[:, :], k_sb[h][:, :].rearrange("p (n j) -> p n j", j=BLK), AX.X, ALU.add),
             reads=[k_b[h]], writes=[km_b])
        P.op("dve", lambda e: e.tensor_scalar(km32[:, :], km32[:, :], 1.0 / BLK, None, ALU.mult), reads=[km_b], writes=[km_b])
        P.op("dve", lambda e: e.tensor_copy(kmh[:, :], km32[:, :]), reads=[km_b], writes=[km_b])
        P.op("dve", lambda e: e.tensor_tensor(kml32[:, :], km32[:, :], kmh[:, :], ALU.subtract), reads=[km_b], writes=[km_b])
        P.op("dve", lambda e: e.tensor_copy(kml[:, :], kml32[:, :]), reads=[km_b], writes=[km_b])
        for qb in range(nblk):
            gi = (h * nblk + qb) % NG
            q0 = qb * BLK
            if qb > TOPK:
                gt, gb = ps_g.next()
                for t in range(2):
                    ql = q_sb[h][:, q0 + t * 128:q0 + (t + 1) * 128]
                    mm_group(P, gt[:, t * 32:t * 32 + qb], gb,
                             [(ql, kmh[:, 0:qb]), (ql, kml[:, 0:qb])], reads=[q_b[h], km_b])
                P.op("dve", lambda e, gi=gi: e.memset(g_sb[gi][:, :, :], -1e30), writes=[g_b[gi]])
                P.op("dve", lambda e, gi=gi, gt=gt, qb=qb: e.tensor_copy(
                    g_sb[gi][:, :, 0:qb], gt[:, 0:64].rearrange("p (t n) -> p t n", n=32)[:, :, 0:qb]),
                    reads=[gb], writes=[g_b[gi]])
                for t in range(2):
                    w8 = max(qb, 8)
                    P.op("dve", lambda e, gi=gi, t=t, w8=w8: e.max(out=mx_sb[gi][:, t, :], in_=g_sb[gi][:, t, 0:w8]),
                         reads=[g_b[gi]], writes=[m_b[gi]])
                    P.op("dve", lambda e, gi=gi, t=t, qb=qb: e.tensor_scalar(
                        m_sb[gi][:, t, 0:qb], g_sb[gi][:, t, 0:qb], mx_sb[gi][:, t, 2:3], None, ALU.is_ge),
                        reads=[g_b[gi], m_b[gi]], writes=[m_b[gi]])
            order = [qb] + list(range(qb))
            for n in order:
                st, sb = ps_s.next()
                for kh in range(2):
                    k0 = n * BLK + kh * 128
                    P.op("pe", lambda e, st=st, kh=kh, k0=k0, q0=q0, h=h: e.matmul(
                        st[:, kh * 256:(kh + 1) * 256], k_sb[h][:, k0:k0 + 128], q_sb[h][:, q0:q0 + 256],
                        start=True, stop=True), reads=[k_b[h], q_b[h]] if kh == 0 else (), writes=[sb], signal=(kh == 1))
                pi = pti % NPT
                pti += 1
                P.op("act", lambda e, pi=pi, st=st: e.activation(pT[pi][:, :], st[:, :], AF.Exp, scale=scale),
                     reads=[sb], writes=[pT_b[pi]])
                if n == qb:
                    P.op("pool", lambda e, pi=pi: e.tensor_tensor(pT[pi][:, :], pT[pi][:, :], cm_sb[:, :], ALU.mult),
                         reads=[pT_b[pi], cm_b], writes=[pT_b[pi]])
                ot, otb = ps_o.next()
                for t in range(2):
                    for kh in range(2):
                        P.op("pe", lambda e, ot=ot, t=t, kh=kh, pi=pi, n=n, h=h: e.matmul(
                            ot[:, t * 130:t * 130 + 129], pT[pi][:, kh * 256 + t * 128:kh * 256 + (t + 1) * 128],
                            v_sb[h][:, n * 2 + kh, 0:129], start=(kh == 0), stop=(kh == 1)),
                            reads=[pT_b[pi], v_b[h]] if (t == 0 and kh == 0) else (), writes=[otb],
                            signal=(t == 1 and kh == 1))
                if n == qb:
                    P.op("dve", lambda e, gi=gi, ot=ot: e.tensor_copy(
                        acc[gi][:, :, 0:129], ot[:, 0:260].rearrange("p (t c) -> p t c", c=130)[:, :, 0:129]),
                        reads=[otb], writes=[acc_b[gi]])
                elif qb <= TOPK:
                    P.op("dve", lambda e, gi=gi, ot=ot: e.tensor_tensor(
                        acc[gi][:, :, 0:129], acc[gi][:, :, 0:129],
                        ot[:, 0:260].rearrange("p (t c) -> p t c", c=130)[:, :, 0:129], ALU.add),
                        reads=[otb, acc_b[gi]], writes=[acc_b[gi]])
                else:
                    for t in range(2):
                        P.op("dve", lambda e, gi=gi, ot=ot, t=t, n=n: e.scalar_tensor_tensor(
                            acc[gi][:, t, 0:129], ot[:, t * 130:t * 130 + 129], m_sb[gi][:, t, n:n + 1],
                            acc[gi][:, t, 0:129], ALU.mult, ALU.add),
                            reads=[otb, acc_b[gi], m_b[gi]], writes=[acc_b[gi]])
            P.op("dve", lambda e, gi=gi: e.reciprocal(rc[gi][:, :], acc[gi][:, :, 128]), reads=[acc_b[gi]], writes=[acc_b[gi]])
            for t in range(2):
                P.op("dve", lambda e, gi=gi, t=t: e.tensor_scalar(
                    ob[gi][:, t, :], acc[gi][:, t, 0:128], rc[gi][:, t:t + 1], None, ALU.mult),
                    reads=[acc_b[gi]], writes=[ob_b[gi]])
            dst = o[h * S + q0:h * S + q0 + 256, :].rearrange("(t p) d -> p t d", p=128)
            P.dma_out("sp", dst, ob[gi][:, :, :], ob_b[gi])
    return P.finish()


def causal_mask_tile():
    m = np.zeros((128, 512), np.float32)
    p = np.arange(128)[:, None]
    for kh in range(2):
        qq = np.arange(256)[None, :]
        m[:, kh * 256:(kh + 1) * 256] = (kh * 128 + p <= qq)
    return m.astype(NPBF)


class Ring:
    def __init__(self, P, name, n, shape, dtype):
        self.tiles = [P.sbuf("%s%d" % (name, i), shape, dtype) for i in range(n)]
        self.bufs = [Buf("%s%d" % (name, i)) for i in range(n)]
        self.i = 0

    def next(self):
        i = self.i % len(self.tiles)
        self.i += 1
        return self.tiles[i], self.bufs[i]


def emit_linear(P, psr, ring, w_ap, kc, nout, rhs_fn, rhs_bufs, ntok, evac, gw=256):
    for j in range(nout // gw):
        wt, wb = ring.load(w_ap, j * gw, width=gw, kc=kc)
        for m in range(gw // 128):
            for n in range(ntok // 512):
                pt, pb = psr.next()
                mm_group(P, pt[:, :], pb,
                         [(wt[:, k, m * 128:(m + 1) * 128], rhs_fn(k, n * 512, (n + 1) * 512)) for k in range(kc)],
                         reads=[wb] + list(rhs_bufs))
                evac(j * (gw // 128) + m, n, pt, pb)


def emit_ln(P, psr, x32, x_b, xb, xb_b, g_sb, b_sb, gb_b, ones_sb, ones_b, tmp, stat, ntok):
    st_mean, st_rstd, st_msq = stat["mean"], stat["rstd"], stat["msq"]
    st_b = stat["buf"]
    def one(n):
        c0, c1 = n * 512, (n + 1) * 512
        p1, p1b = psr.next()
        mm_group(P, p1[:, :], p1b, [(ones_sb[:, :], x32[:, k, c0:c1]) for k in range(16)], reads=[ones_b] + list(x_b))
        p2, p2b = psr.next()
        for k in range(16):
            sq, sqb = tmp.next()
            P.op("act", lambda e, sq=sq, k=k: e.activation(sq[:, :], x32[:, k, c0:c1], AF.Square), reads=[x_b[k]], writes=[sqb])
            P.op("pe", lambda e, sq=sq, k=k: e.matmul(p2[:, :], ones_sb[:, :], sq[:, :], start=(k == 0), stop=(k == 15)),
                 reads=[sqb, ones_b], writes=[p2b], signal=True)
        P.op("dve", lambda e: e.tensor_scalar(st_mean[:, :], p1[:, :], 1.0 / D, None, ALU.mult), reads=[p1b], writes=[st_b])
        P.op("dve", lambda e: e.tensor_tensor(st_msq[:, :], st_mean[:, :], st_mean[:, :], ALU.mult), reads=[st_b], writes=[st_b])
        P.op("dve", lambda e: e.scalar_tensor_tensor(st_msq[:, :], p2[:, :], 1.0 / D, st_msq[:, :], ALU.mult, ALU.subtract),
             reads=[p2b, st_b], writes=[st_b])
        P.op("dve", lambda e: e.tensor_scalar(st_msq[:, :], st_msq[:, :], LN_EPS, None, ALU.add), reads=[st_b], writes=[st_b])
        P.op("act", lambda e: e.activation(st_rstd[:, :], st_msq[:, :], AF.Sqrt), reads=[st_b], writes=[st_b])
        P.op("dve", lambda e: e.reciprocal(st_rstd[:, :], st_rstd[:, :]), reads=[st_b], writes=[st_b])
        for k in range(16):
            t1, t1b = tmp.next()
            P.op("dve", lambda e, t1=t1, k=k: e.tensor_tensor(t1[:, :], x32[:, k, c0:c1], st_mean[:, :], ALU.subtract),
                 reads=[x_b[k], st_b], writes=[t1b])
            P.op("pool", lambda e, t1=t1: e.tensor_tensor(t1[:, :], t1[:, :], st_rstd[:, :], ALU.mult), reads=[t1b, st_b], writes=[t1b])
            P.op("act", lambda e, t1=t1, k=k: e.activation(x32[:, k, c0:c1], t1[:, :], AF.Identity,
                                                           bias=b_sb[:, k:k + 1], scale=g_sb[:, k:k + 1]),
                 reads=[t1b, gb_b], writes=[x_b[k]])
            P.op("pool", lambda e, k=k: e.tensor_copy(xb[:, k, c0:c1], x32[:, k, c0:c1]), reads=[x_b[k]], writes=[xb_b[k]])

    for n in range(ntok // 512):
        one(n)


def emit_ffn(P, psr, ring_gu, ring_d, hring, tmp, xb, xb_b, acc, acc_b, wg, wu, wd, dff, ntok, first_copy):
    GW = 256
    for j in range(dff // GW):
        wgt, wgb = ring_gu.load(wg, j * GW, width=GW, kc=16)
        wut, wub = ring_gu.load(wu, j * GW, width=GW, kc=16)
        ht, hb = hring.next()
        for m in range(GW // 128):
            for n in range(ntok // 512):
                c0, c1 = n * 512, (n + 1) * 512
                pg, pgb = psr.next()
                mm_group(P, pg[:, :], pgb, [(wgt[:, k, m * 128:(m + 1) * 128], xb[:, k, c0:c1]) for k in range(16)],
                         reads=[wgb] + list(xb_b))
                pu, pub = psr.next()
                mm_group(P, pu[:, :], pub, [(wut[:, k, m * 128:(m + 1) * 128], xb[:, k, c0:c1]) for k in range(16)],
                         reads=[wub] + list(xb_b))
                s, sb = tmp.next()
                P.op("act", lambda e, s=s, pg=pg: e.activation(s[:, :], pg[:, :], AF.Silu), reads=[pgb], writes=[sb])
                P.op("dve", lambda e, ht=ht, m=m, s=s, pu=pu, c0=c0, c1=c1: e.tensor_tensor(ht[:, m, c0:c1], s[:, :], pu[:, :], ALU.mult),
                     reads=[sb, pub], writes=[hb])
        wdt, wdb = ring_d.load_rows(wd, j * GW, GW // 128)
        for fo in range(16):
            for n in range(ntok // 512):
                c0, c1 = n * 512, (n + 1) * 512
                pt, pb = psr.next()
                mm_group(P, pt[:, :], pb, [(wdt[:, kk, fo * 128:(fo + 1) * 128], ht[:, kk, c0:c1]) for kk in range(GW // 128)],
                         reads=[wdb, hb])
                if first_copy and j == 0:
                    P.op("pool" if False else "dve", lambda e, fo=fo, pt=pt, c0=c0, c1=c1: e.tensor_copy(acc[:, fo, c0:c1], pt[:, :]),
                         reads=[pb], writes=[acc_b[fo]])
                else:
                    P.op("dve", lambda e, fo=fo, pt=pt, c0=c0, c1=c1: e.tensor_tensor(acc[:, fo, c0:c1], acc[:, fo, c0:c1], pt[:, :], ALU.add),
                         reads=[pb, acc_b[fo]], writes=[acc_b[fo]])


class DRing:
    def __init__(self, P, name, nslots, rk):
        self.P = P
        self.tiles = [P.sbuf("%s%d" % (name, i), [128, rk, D], BF16) for i in range(nslots)]
        self.bufs = [Buf("%s%d" % (name, i)) for i in range(nslots)]
        self.i = 0

    def load_rows(self, w_ap, r0, rk, q="pool"):
        i = self.i % len(self.tiles)
        self.i += 1
        t, b = self.tiles[i], self.bufs[i]
        src = w_ap[r0:r0 + rk * 128, :].rearrange("(k p) f -> p k f", p=128)
        self.P.dma(q, t[:, 0:rk, :], src, writes=[b])
        return t, b


def load_small(P, name, dram_ap, shape, dtype=F32, q="sp"):
    t = P.sbuf(name, shape, dtype)
    b = Buf(name)
    P.dma(q, t[tuple(slice(None) for _ in shape)], dram_ap, writes=[b])
    return t, b


def build_C(variant, ntok=TOK):
    P = Prog()
    oT = P.dram_in("oT", [D, ntok], BF16)
    sgaT = P.dram_in("sgaT", [D, ntok], BF16)
    mpT = P.dram_in("mpT", [D, ntok], BF16)
    xT32 = P.dram_in("xT32", [D, ntok], F32)
    w_up_attn = P.dram_in("w_up_attn", [D, D], F32)
    w_o = P.dram_in("w_o", [D, D], F32)
    lnm_g = P.dram_in("lnm_g", [128, 16], F32)
    lnm_b = P.dram_in("lnm_b", [128, 16], F32)
    ones_d = P.dram_in("ones", [128, 128], F32)
    if variant == "dense":
        w_gate = P.dram_in("w_gate", [D, D_FF], F32)
        w_up = P.dram_in("w_up", [D, D_FF], F32)
        w_down = P.dram_in("w_down", [D_FF, D], F32)
        lnf_g = P.dram_in("lnf_g", [128, 16], F32)
        lnf_b = P.dram_in("lnf_b", [128, 16], F32)
    else:
        router = P.dram_in("router", [D, NE], F32)
        wfull = P.dram_out("wfull", [ntok, NE], F32)
    xo32 = P.dram_out("xo32", [D, ntok], F32)
    xob = P.dram_out("xob", [D, ntok], BF16)

    x32 = P.sbuf("x32", [128, 16, ntok], F32)
    x_b = [Buf("x32_%d" % k) for k in range(16)]
    ob = P.sbuf("ob", [128, 16, ntok], BF16)
    ob_b = [Buf("ob_%d" % k) for k in range(16)]
    mb = P.sbuf("mb", [128, 16, ntok], BF16)
    mb_b = [Buf("mb_%d" % k) for k in range(16)]
    ring = WRing(P, "w", 4, 16, 256)
    psr = PsumRing(P, 8)
    tmp = Ring(P, "tmp", 4, [128, 512], F32)
    stat = {"mean": P.sbuf("st_mean", [128, 512], F32), "rstd": P.sbuf("st_rstd", [128, 512], F32),
            "msq": P.sbuf("st_msq", [128, 512], F32), "buf": Buf("stat")}
    sgr = Ring(P, "sgr", 2, [128, ntok], BF16)
    mpr = Ring(P, "mpr", 2, [128, ntok], BF16)
    ones_sb, ones_b = load_small(P, "ones_sb", ones_d[:, :], [128, 128])
    g1, g1b = load_small(P, "lnm_g_sb", lnm_g[:, :], [128, 16])
    b1, b1b = load_small(P, "lnm_b_sb", lnm_b[:, :], [128, 16])
    gb1 = Buf("gb1")
    gb1.w = None
    if variant == "dense":
        g2, g2b = load_small(P, "lnf_g_sb", lnf_g[:, :], [128, 16])
        b2, b2b = load_small(P, "lnf_b_sb", lnf_b[:, :], [128, 16])
    else:
        rt_sb = P.sbuf("rt_sb", [128, 16, NE], F32)
        rt_b = Buf("rt")
        P.dma("sp", rt_sb[:, :, :], router.rearrange("(k p) e -> p k e", p=128), writes=[rt_b])

    ov = oT.rearrange("(k p) t -> p k t", p=128)
    xv = xT32.rearrange("(k p) t -> p k t", p=128)
    for k in range(16):
        P.dma("sp", ob[:, k, :], ov[:, k, :], writes=[ob_b[k]])
    for k in range(16):
        P.dma("act", x32[:, k, :], xv[:, k, :], writes=[x_b[k]])

    cur = {}

    def evac_up(fc, n, pt, pb):
        if n == 0:
            st, sb = sgr.next()
            mt, mtb = mpr.next()
            P.dma("sp", st[:, :], sgaT[fc * 128:(fc + 1) * 128, :], writes=[sb])
            P.dma("sp", mt[:, :], mpT[fc * 128:(fc + 1) * 128, :], writes=[mtb])
            cur["s"] = (st, sb, mt, mtb)
        st, sb, mt, mtb = cur["s"]
        c0, c1 = n * 512, (n + 1) * 512
        t1, t1b = tmp.next()
        P.op("dve", lambda e: e.tensor_tensor(t1[:, :], pt[:, :], st[:, c0:c1], ALU.mult), reads=[pb, sb], writes=[t1b])
        P.op("pool", lambda e: e.tensor_tensor(mb[:, fc, c0:c1], t1[:, :], mt[:, c0:c1], ALU.add), reads=[t1b, mtb], writes=[mb_b[fc]])

    emit_linear(P, psr, ring, w_up_attn, 16, D, lambda k, c0, c1: ob[:, k, c0:c1], ob_b, ntok, evac_up)

    def evac_o(fc, n, pt, pb):
        c0, c1 = n * 512, (n + 1) * 512
        P.op("dve", lambda e: e.scalar_tensor_tensor(x32[:, fc, c0:c1], x32[:, fc, c0:c1], float(ALPHA), pt[:, :], ALU.mult, ALU.add),
             reads=[pb, x_b[fc]], writes=[x_b[fc]])

    emit_linear(P, psr, ring, w_o, 16, D, lambda k, c0, c1: mb[:, k, c0:c1], mb_b, ntok, evac_o)

    gbb = Buf("gbb")
    P.op("dve", lambda e: e.tensor_copy(g1[:, :], g1[:, :]), reads=[g1b, b1b], writes=[gbb])
    emit_ln(P, psr, x32, x_b, ob, ob_b, g1, b1, gbb, ones_sb, ones_b, tmp, stat, ntok)

    if variant == "moe":
        xo32v = xo32.rearrange("(k p) t -> p k t", p=128)
        xobv = xob.rearrange("(k p) t -> p k t", p=128)
        for k in range(16):
            P.dma_out("sp", xo32v[:, k, :], x32[:, k, :], x_b[k])
            P.dma_out("act", xobv[:, k, :], ob[:, k, :], ob_b[k])
        ntile = ntok // 128
        lg = P.sbuf("lg", [128, ntile, NE], F32)
        mx = P.sbuf("mx", [128, ntile, 8], F32)
        msk = P.sbuf("msk", [128, ntile, NE], F32)
        ex = P.sbuf("ex", [128, ntile, NE], F32)
        ssum = P.sbuf("ssum", [128, ntile], F32)
        wf = P.sbuf("wf", [128, ntile, NE], F32)
        r_b = Buf("router_work")
        for tt in range(ntile):
            pt, pb = psr.next()
            mm_group(P, pt[:, 0:NE], pb, [(x32[:, k, tt * 128:(tt + 1) * 128], rt_sb[:, k, :]) for k in range(16)],
                     reads=[rt_b] + x_b)
            P.op("dve", lambda e, tt=tt, pt=pt: e.tensor_copy(lg[:, tt, :], pt[:, 0:NE]), reads=[pb], writes=[r_b])
            P.op("dve", lambda e, tt=tt: e.max(out=mx[:, tt, :], in_=lg[:, tt, :]), reads=[r_b], writes=[r_b])
            P.op("dve", lambda e, tt=tt: e.tensor_scalar(msk[:, tt, :], lg[:, tt, :], mx[:, tt, 1:2], None, ALU.is_ge), reads=[r_b], writes=[r_b])
            P.op("dve", lambda e, tt=tt: e.tensor_scalar(lg[:, tt, :], lg[:, tt, :], mx[:, tt, 0:1], None, ALU.subtract), reads=[r_b], writes=[r_b])
            P.op("act", lambda e, tt=tt: e.activation(ex[:, tt, :], lg[:, tt, :], AF.Exp), reads=[r_b], writes=[r_b])
            P.op("dve", lambda e, tt=tt: e.tensor_tensor(ex[:, tt, :], ex[:, tt, :], msk[:, tt, :], ALU.mult), reads=[r_b], writes=[r_b])
            P.op("dve", lambda e, tt=tt: e.tensor_reduce(ssum[:, tt:tt + 1], ex[:, tt, :], AX.X, ALU.add), reads=[r_b], writes=[r_b])
            P.op("dve", lambda e, tt=tt: e.reciprocal(ssum[:, tt:tt + 1], ssum[:, tt:tt + 1]), reads=[r_b], writes=[r_b])
            P.op("dve", lambda e, tt=tt: e.tensor_scalar(wf[:, tt, :], ex[:, tt, :], ssum[:, tt:tt + 1], None, ALU.mult), reads=[r_b], writes=[r_b])
        P.dma_out("sp", wfull.rearrange("(t p) e -> p t e", p=128), wf[:, :, :], r_b)
        return P.finish()

    for k in range(16):
        for n in range(ntok // 512):
            c0, c1 = n * 512, (n + 1) * 512
            P.op("pool", lambda e, k=k, c0=c0, c1=c1: e.tensor_scalar(x32[:, k, c0:c1], x32[:, k, c0:c1], float(ALPHA), None, ALU.mult),
                 reads=[x_b[k]], writes=[x_b[k]])
    ring_d = DRing(P, "wd", 2, 2)
    class HR:
        def __init__(self):
            self.i = 0

        def next(self):
            s = self.i % 4
            self.i += 1
            return mb[:, 2 * s:2 * s + 2, :], HRB[s]
    HRB = [Buf("h%d" % s) for s in range(4)]
    for s in range(4):
        HRB[s].r = list(mb_b[2 * s].r) + list(mb_b[2 * s + 1].r)
        HRB[s].w = mb_b[2 * s].w
    emit_ffn(P, psr, ring, ring_d, HR(), tmp, ob, ob_b, x32, x_b, w_gate, w_up, w_down, D_FF, ntok, first_copy=False)
    gbb2 = Buf("gbb2")
    P.op("dve", lambda e: e.tensor_copy(g2[:, :], g2[:, :]), reads=[g2b, b2b], writes=[gbb2])
    emit_ln(P, psr, x32, x_b, ob, ob_b, g2, b2, gbb2, ones_sb, ones_b, tmp, stat, ntok)
    xo32v = xo32.rearrange("(k p) t -> p k t", p=128)
    xobv = xob.rearrange("(k p) t -> p k t", p=128)
    for k in range(16):
        P.dma_out("sp", xo32v[:, k, :], x32[:, k, :], x_b[k])
        P.dma_out("act", xobv[:, k, :], ob[:, k, :], ob_b[k])
    return P.finish()


def build_E(C):
    P = Prog()
    xeT = P.dram_in("xeT", [D, C], BF16)
    w_gate = P.dram_in("w_gate", [D, D_FFE], F32)
    w_up = P.dram_in("w_up", [D, D_FFE], F32)
    w_down = P.dram_in("w_down", [D_FFE, D], F32)
    yT = P.dram_out("yT", [D, C], F32)
    BW = 1024
    xb = P.sbuf("xb", [128, 16, BW], BF16)
    xb_b = [Buf("xb_%d" % k) for k in range(16)]
    acc = P.sbuf("acc", [128, 16, BW], F32)
    acc_b = [Buf("acc_%d" % k) for k in range(16)]
    ring = WRing(P, "w", 4, 16, 256)
    ring_d = DRing(P, "wd", 2, 2)
    hring = Ring(P, "h", 3, [128, 2, BW], BF16)
    tmp = Ring(P, "tmp", 3, [128, 512], F32)
    psr = PsumRing(P, 8)
    xv = xeT.rearrange("(k p) t -> p k t", p=128)
    yv = yT.rearrange("(k p) t -> p k t", p=128)
    off = 0
    while off < C:
        bw = min(BW, C - off)
        for k in range(16):
            P.dma("sp", xb[:, k, 0:bw], xv[:, k, off:off + bw], writes=[xb_b[k]])
        emit_ffn(P, psr, ring, ring_d, hring, tmp, xb, xb_b, acc, acc_b, w_gate, w_up, w_down, D_FFE, bw, first_copy=True)
        for k in range(16):
            P.dma_out("sp", yv[:, k, off:off + bw], acc[:, k, 0:bw], acc_b[k])
        off += bw
    return P.finish()


def build_F(ntok=TOK):
    P = Prog()
    xT32 = P.dram_in("xT32", [D, ntok], F32)
    y1T = P.dram_in("y1T", [D, ntok], F32)
    y2T = P.dram_in("y2T", [D, ntok], F32)
    wb = P.dram_in("wb", [128, 2 * ntok], F32)
    lnf_g = P.dram_in("lnf_g", [128, 16], F32)
    lnf_b = P.dram_in("lnf_b", [128, 16], F32)
    ones_d = P.dram_in("ones", [128, 128], F32)
    xo32 = P.dram_out("xo32", [D, ntok], F32)
    xob = P.dram_out("xob", [D, ntok], BF16)
    x32 = P.sbuf("x32", [128, 16, ntok], F32)
    x_b = [Buf("x32_%d" % k) for k in range(16)]
    ob = P.sbuf("ob", [128, 16, ntok], BF16)
    ob_b = [Buf("ob_%d" % k) for k in range(16)]
    y1r = Ring(P, "y1r", 2, [128, ntok], F32)
    y2r = Ring(P, "y2r", 2, [128, ntok], F32)
    tmp = Ring(P, "tmp", 4, [128, 512], F32)
    stat = {"mean": P.sbuf("st_mean", [128, 512], F32), "rstd": P.sbuf("st_rstd", [128, 512], F32),
            "msq": P.sbuf("st_msq", [128, 512], F32), "buf": Buf("stat")}
    psr = PsumRing(P, 8)
    ones_sb, ones_b = load_small(P, "ones_sb", ones_d[:, :], [128, 128])
    g2, g2b = load_small(P, "lnf_g_sb", lnf_g[:, :], [128, 16])
    b2, b2b = load_small(P, "lnf_b_sb", lnf_b[:, :], [128, 16])
    wb_sb, wb_b = load_small(P, "wb_sb", wb[:, :], [128, 2 * ntok])
    xv = xT32.rearrange("(k p) t -> p k t", p=128)
    for k in range(16):
        P.dma("act", x32[:, k, :], xv[:, k, :], writes=[x_b[k]])

    def one(k):
        y1, y1b = y1r.next()
        y2, y2b = y2r.next()
        P.dma("sp", y1[:, :], y1T[k * 128:(k + 1) * 128, :], writes=[y1b])
        P.dma("sp", y2[:, :], y2T[k * 128:(k + 1) * 128, :], writes=[y2b])
        P.op("dve", lambda e: e.tensor_tensor(y1[:, :], y1[:, :], wb_sb[:, 0:ntok], ALU.mult), reads=[y1b, wb_b], writes=[y1b])
        P.op("pool", lambda e: e.tensor_tensor(y2[:, :], y2[:, :], wb_sb[:, ntok:2 * ntok], ALU.mult), reads=[y2b, wb_b], writes=[y2b])
        P.op("dve", lambda e: e.scalar_tensor_tensor(x32[:, k, :], x32[:, k, :], float(ALPHA), y1[:, :], ALU.mult, ALU.add),
             reads=[x_b[k], y1b], writes=[x_b[k]])
        P.op("dve", lambda e: e.tensor_tensor(x32[:, k, :], x32[:, k, :], y2[:, :], ALU.add), reads=[x_b[k], y2b], writes=[x_b[k]])

    for k in range(16):
        one(k)
    gbb = Buf("gbb")
    P.op("dve", lambda e: e.tensor_copy(g2[:, :], g2[:, :]), reads=[g2b, b2b], writes=[gbb])
    emit_ln(P, psr, x32, x_b, ob, ob_b, g2, b2, gbb, ones_sb, ones_b, tmp, stat, ntok)
    xo32v = xo32.rearrange("(k p) t -> p k t", p=128)
    xobv = xob.rearrange("(k p) t -> p k t", p=128)
    for k in range(16):
        P.dma_out("sp", xo32v[:, k, :], x32[:, k, :], x_b[k])
        P.dma_out("act", xobv[:, k, :], ob[:, k, :], ob_b[k])
    return P.finish()


def build_L(ntok=TOK):
    P = Prog()
    xT32 = P.dram_in("xT32", [D, ntok], F32)
    ln_g = P.dram_in("ln_g", [128, 16], F32)
    ln_b = P.dram_in("ln_b", [128, 16], F32)
    ones_d = P.dram_in("ones", [128, 128], F32)
    xo32 = P.dram_out("xo32", [D, ntok], F32)
    xob = P.dram_out("xob", [D, ntok], BF16)
    x32 = P.sbuf("x32", [128, 16, ntok], F32)
    x_b = [Buf("x32_%d" % k) for k in range(16)]
    ob = P.sbuf("ob", [128, 16, ntok], BF16)
    ob_b = [Buf("ob_%d" % k) for k in range(16)]
    tmp = Ring(P, "tmp", 4, [128, 512], F32)
    stat = {"mean": P.sbuf("st_mean", [128, 512], F32), "rstd": P.sbuf("st_rstd", [128, 512], F32),
            "msq": P.sbuf("st_msq", [128, 512], F32), "buf": Buf("stat")}
    psr = PsumRing(P, 8)
    ones_sb, ones_b = load_small(P, "ones_sb", ones_d[:, :], [128, 128])
    g2, g2b = load_small(P, "ln_g_sb", ln_g[:, :], [128, 16])
    b2, b2b = load_small(P, "ln_b_sb", ln_b[:, :], [128, 16])
    xv = xT32.rearrange("(k p) t -> p k t", p=128)
    for k in range(16):
        P.dma("act", x32[:, k, :], xv[:, k, :], writes=[x_b[k]])
    gbb = Buf("gbb")
    P.op("dve", lambda e: e.tensor_copy(g2[:, :], g2[:, :]), reads=[g2b, b2b], writes=[gbb])
    emit_ln(P, psr, x32, x_b, ob, ob_b, g2, b2, gbb, ones_sb, ones_b, tmp, stat, ntok)
    xo32v = xo32.rearrange("(k p) t -> p k t", p=128)
    xobv = xob.rearrange("(k p) t -> p k t", p=128)
    for k in range(16):
        P.dma_out("sp", xo32v[:, k, :], x32[:, k, :], x_b[k])
        P.dma_out("act", xobv[:, k, :], ob[:, k, :], ob_b[k])
    return P.finish()


_PROGS = {}


def _prog(key, builder, *a):
    if key not in _PROGS:
        _PROGS[key] = builder(*a)
    return _PROGS[key]


def _lay16(v):
    return np.ascontiguousarray(np.asarray(v, np.float32).reshape(16, 128).T)


def _cnt_tab(core):
    tab = np.zeros((128, 4 * HALO), np.float32)
    for g, w in enumerate(POOL_WINDOWS):
        for i in range(HALO):
            tab[:, g * HALO + i] = 1.0 / min(core * TOK + i + 1, w)
    return tab


def kernel(x, ln_in_g, ln_in_b, w_in, pool_w, pool_scale, w_up_pool, w_up_attn, w_o,
           ln_mix_g, ln_mix_b, ffn_w_gate, ffn_w_up, ffn_w_down, moe_router, moe_w_gate,
           moe_w_up, moe_w_down, ln_ffn_g, ln_ffn_b):
    f32 = lambda a: np.ascontiguousarray(np.asarray(a, np.float32))
    ones = np.ones((128, 128), np.float32)
    cmask = causal_mask_tile()
    x2 = np.asarray(x, np.float32).reshape(SEQ, D)

    ins = [{"xT32": np.ascontiguousarray(x2[c * TOK:(c + 1) * TOK].T), "ln_g": _lay16(ln_in_g), "ln_b": _lay16(ln_in_b),
            "ones": ones} for c in range(NCORES)]
    res = run_prog(_prog("L", build_L), ins)
    x32 = [r["xo32"] for r in res]
    xb = [r["xob"] for r in res]

    for l in range(DEPTH):
        ins = []
        w_in_l = f32(w_in[l])
        pw_l = f32(pool_w[l]).reshape(4 * 256, 256)
        psc_l = np.ascontiguousarray(np.asarray(pool_scale[l], np.float32).reshape(8, 128).T)
        wup_l = f32(w_up_pool[l])
        for c in range(NCORES):
            halo = xb[c - 1][:, TOK - HALO:] if c > 0 else np.zeros((D, HALO), NPBF)
            ins.append({"xT": np.ascontiguousarray(np.concatenate([halo, xb[c]], axis=1)), "w_in": w_in_l, "pool_w": pw_l,
                        "pool_scale": psc_l, "cnt_tab": _cnt_tab(c), "w_up_pool": wup_l})
        resA = run_prog(_prog("A", build_A), ins)
        del ins, w_in_l
        qkv = np.concatenate([r["qkvT"] for r in resA], axis=1)
        ins = []
        for c in range(NCORES):
            r0 = c * 256
            vT = qkv[2 * D + r0:2 * D + r0 + 256]
            v = np.ascontiguousarray(vT.reshape(2, 128, SEQ).transpose(0, 2, 1)).reshape(2 * SEQ, 128)
            ins.append({"qT": np.ascontiguousarray(qkv[r0:r0 + 256]), "kT": np.ascontiguousarray(qkv[D + r0:D + r0 + 256]),
                        "v": v, "cmask": cmask})
        resB = run_prog(_prog("B", build_B), ins)
        del ins, qkv
        o_all = np.stack([r["o"].reshape(2, SEQ, 128) for r in resB], 0).reshape(NH, SEQ, 128)
        oT_all = np.ascontiguousarray(o_all.transpose(0, 2, 1)).reshape(D, SEQ)
        i = l // 2
        variant = "dense" if l % 2 == 0 else "moe"
        ins = []
        common = {"w_up_attn": f32(w_up_attn[l]), "w_o": f32(w_o[l]), "lnm_g": _lay16(ln_mix_g[l]), "lnm_b": _lay16(ln_mix_b[l]),
                  "ones": ones}
        if variant == "dense":
            common.update({"w_gate": f32(ffn_w_gate[i]), "w_up": f32(ffn_w_up[i]), "w_down": f32(ffn_w_down[i]),
                           "lnf_g": _lay16(ln_ffn_g[l]), "lnf_b": _lay16(ln_ffn_b[l])})
        else:
            common.update({"router": f32(moe_router[i])})
        for c in range(NCORES):
            d = {"oT": np.ascontiguousarray(oT_all[:, c * TOK:(c + 1) * TOK]), "sgaT": resA[c]["sgaT"], "mpT": resA[c]["mpT"],
                 "xT32": x32[c]}
            d.update(common)
            ins.append(d)
        resC = run_prog(_prog("C" + variant, build_C, variant), ins)
        del ins, common, oT_all, o_all, resA, resB
        x32 = [r["xo32"] for r in resC]
        xb = [r["xob"] for r in resC]
        if variant == "dense":
            continue
        wfull = np.concatenate([r["wfull"] for r in resC], axis=0)
        sel = wfull > 0
        rank = np.cumsum(sel, axis=1)
        xb_all = np.concatenate(xb, axis=1)
        toks = [np.nonzero(sel[:, e])[0] for e in range(NE)]
        cmax = max(len(t) for t in toks)
        C = max(512, -(-cmax // 512) * 512)
        ins = []
        for e in range(NE):
            xe = np.zeros((D, C), NPBF)
            xe[:, :len(toks[e])] = xb_all[:, toks[e]]
            ins.append({"xeT": xe, "w_gate": f32(moe_w_gate[i][e]), "w_up": f32(moe_w_up[i][e]), "w_down": f32(moe_w_down[i][e])})
        resE = run_prog(_prog("E%d" % C, build_E, C), ins)
        del ins, xb_all
        y1 = np.zeros((D, SEQ), np.float32)
        y2 = np.zeros((D, SEQ), np.float32)
        w1 = np.zeros((SEQ,), np.float32)
        w2 = np.zeros((SEQ,), np.float32)
        for e in range(NE):
            t = toks[e]
            ye = resE[e]["yT"][:, :len(t)]
            first = rank[t, e] == 1
            second = rank[t, e] == 2
            y1[:, t[first]] = ye[:, first]
            y2[:, t[second]] = ye[:, second]
            w1[t[first]] = wfull[t[first], e]
            w2[t[second]] = wfull[t[second], e]
        del resE
        ins = []
        for c in range(NCORES):
            sl = slice(c * TOK, (c + 1) * TOK)
            wbc = np.ascontiguousarray(np.broadcast_to(np.concatenate([w1[sl], w2[sl]])[None, :], (128, 2 * TOK)))
            ins.append({"xT32": x32[c], "y1T": np.ascontiguousarray(y1[:, sl]), "y2T": np.ascontiguousarray(y2[:, sl]), "wb": wbc,
                        "lnf_g": _lay16(ln_ffn_g[l]), "lnf_b": _lay16(ln_ffn_b[l]), "ones": ones})
        resF = run_prog(_prog("F", build_F), ins)
        del ins, y1, y2
        x32 = [r["xo32"] for r in resF]
        xb = [r["xob"] for r in resF]

    out = np.concatenate([a.T for a in x32], axis=0).reshape(1, SEQ, D)
    return np.ascontiguousarray(out.astype(np.float32))
```

```python
import contextlib
import numpy as np
import ml_dtypes
import concourse.bass as bass
import concourse.mybir as mybir
from concourse.bass_utils import run_bass_kernel_spmd

F32 = mybir.dt.float32
BF16 = mybir.dt.bfloat16
ALU = mybir.AluOpType
AF = mybir.ActivationFunctionType
AX = mybir.AxisListType
NPBF = ml_dtypes.bfloat16

NCORES = 8
D = 2048
SEQ = 8192
DEPTH = 4
TOK = SEQ // NCORES
HALO = 16
POOL_WINDOWS = (2, 4, 8, 16)
POOL_WIDTH = 1024
NH = 16
DH = 128
BLK = 256
NBLK = SEQ // BLK
TOPK = 3
IN_WIDTH = POOL_WIDTH + 3 * D + 2 * D
D_FF = 5632
NE = 8
D_FFE = 7168
ALPHA = (2 * DEPTH) ** 0.25
LN_EPS = 1e-5

COMPUTE = ("pe", "act", "dve", "pool")


class Buf:
    __slots__ = ("name", "w", "r", "dsem")

    def __init__(self, name):
        self.name = name
        self.w = None
        self.r = []
        self.dsem = None


class Prog:
    def __init__(self):
        self.nc = bass.Bass("TRN2", target_bir_lowering=False)
        self.es = contextlib.ExitStack()
        self.streams = {e: [] for e in ("pe", "act", "dve", "pool", "sp")}
        self.sems = {}
        self.count = {}
        self.waited = {e: {} for e in self.streams}
        self.ndsem = 0
        for e in COMPUTE:
            self._mksem("c_" + e)
        self.out_bufs = []

    def _mksem(self, key):
        self.sems[key] = self.es.enter_context(self.nc.semaphore(key))
        self.count[key] = 0
        return key

    def dram_in(self, name, shape, dtype):
        return self.nc.dram_tensor(name, list(shape), dtype, kind="ExternalInput").ap()

    def dram_out(self, name, shape, dtype):
        return self.nc.dram_tensor(name, list(shape), dtype, kind="ExternalOutput").ap()

    def sbuf(self, name, shape, dtype):
        return self.es.enter_context(self.nc.sbuf_tensor(name, list(shape), dtype))

    def psum(self, name, shape, dtype=F32):
        return self.es.enter_context(self.nc.psum_tensor(name, list(shape), dtype))

    def _need(self, eng, ev):
        if ev is None:
            return
        key, val = ev
        if self.waited[eng].get(key, 0) >= val:
            return
        self.waited[eng][key] = val
        self.streams[eng].append(("wait", key, val))

    def _deps(self, eng, reads, writes):
        for b in reads:
            self._need(eng, b.w)
        for b in writes:
            self._need(eng, b.w)
            for ev in b.r:
                self._need(eng, ev)

    def _commit(self, ev, reads, writes):
        for b in reads:
            b.r.append(ev)
        for b in writes:
            b.w = ev
            b.r = []

    def op(self, eng, fn, reads=(), writes=(), signal=True):
        self._deps(eng, reads, writes)
        key = "c_" + eng
        if signal:
            self.count[key] += 1
            ev = (key, self.count[key])
            self.streams[eng].append(("op", fn, key, 1))
            self._commit(ev, reads, writes)
        else:
            self.streams[eng].append(("op", fn, None, 0))

    def dma(self, q, out_ap, in_ap, reads=(), writes=(), cont=False):
        bufs = list(reads) + list(writes)
        owner = bufs[0]
        if owner.dsem is None:
            owner.dsem = self._mksem("d%d_%s" % (self.ndsem, owner.name))
            self.ndsem += 1
        key = owner.dsem
        for b in bufs[1:]:
            assert b.dsem is None or b.dsem == key
            b.dsem = key
        if not cont:
            self._deps(q, reads, writes)
        self.count[key] += 16
        ev = (key, self.count[key])
        self.streams[q].append(("op", lambda e, o=out_ap, i=in_ap: e.dma_start(out=o, in_=i), key, 16))
        self._commit(ev, reads, writes)
        return ev

    def dma_out(self, q, out_ap, in_ap, src):
        ev = self.dma(q, out_ap, in_ap, reads=[src])
        self.final_events = getattr(self, "final_events", {})
        self.final_events[ev[0]] = ev[1]

    def finish(self):
        nc = self.nc
        for key, val in getattr(self, "final_events", {}).items():
            self.streams["sp"].append(("wait", key, val))
        for e in COMPUTE:
            if self.count["c_" + e]:
                self.streams["sp"].append(("wait", "c_" + e, self.count["c_" + e]))
        sems = self.sems
        streams = self.streams

        def replay(eng_obj, items):
            for it in items:
                if it[0] == "wait":
                    eng_obj.wait_ge(sems[it[1]], it[2])
                else:
                    ins = it[1](eng_obj)
                    if it[2] is not None:
                        ins.then_inc(sems[it[2]], it[3])

        with nc.Block() as block:
            @block.tensor
            def _(e):
                replay(e, streams["pe"])

            @block.scalar
            def _(e):
                replay(e, streams["act"])

            @block.vector
            def _(e):
                replay(e, streams["dve"])

            @block.gpsimd
            def _(e):
                replay(e, streams["pool"])

            @block.sync
            def _(e):
                replay(e, streams["sp"])
        self.es.close()
        return nc


def run_prog(nc, in_maps):
    res = run_bass_kernel_spmd(nc, in_maps, core_ids=list(range(len(in_maps))))
    return res.results


class PsumRing:
    def __init__(self, P, n, name="ps"):
        self.tiles = [P.psum("%s%d" % (name, i), [128, 512]) for i in range(n)]
        self.bufs = [Buf("%s%d" % (name, i)) for i in range(n)]
        self.i = 0

    def next(self):
        i = self.i % len(self.tiles)
        self.i += 1
        return self.tiles[i], self.bufs[i]


class WRing:
    def __init__(self, P, name, nslots, kc, width):
        self.P = P
        self.kc = kc
        self.width = width
        self.tiles = [P.sbuf("%s%d" % (name, i), [128, kc, width], BF16) for i in range(nslots)]
        self.bufs = [Buf("%s%d" % (name, i)) for i in range(nslots)]
        self.i = 0

    def load(self, w_ap, c0, width=None, kc=None, q="pool"):
        width = width or self.width
        kc = kc or self.kc
        i = self.i % len(self.tiles)
        self.i += 1
        t, b = self.tiles[i], self.bufs[i]
        src = w_ap.rearrange("(k p) f -> p k f", p=128)[:, :, c0:c0 + width]
        h = kc // 2 if kc >= 2 else kc
        self.P.dma(q, t[:, 0:h, 0:width], src[:, 0:h, :], writes=[b])
        if h < kc:
            self.P.dma(q, t[:, h:kc, 0:width], src[:, h:kc, :], writes=[b], cont=True)
        return t, b


def mm_group(P, ps_ap, ps_buf, pairs, reads):
    n = len(pairs)
    for i, (l, r) in enumerate(pairs):
        last = i == n - 1
        P.op("pe", lambda e, l=l, r=r, i=i, last=last: e.matmul(ps_ap, l, r, start=(i == 0), stop=last),
             reads=reads if (i == 0 or last) else (), writes=[ps_buf], signal=last)


def build_A():
    P = Prog()
    TH = TOK + HALO
    xT = P.dram_in("xT", [D, TH], BF16)
    w_in = P.dram_in("w_in", [D, IN_WIDTH], F32)
    pool_w = P.dram_in("pool_w", [4 * 256, 256], F32)
    pool_scale = P.dram_in("pool_scale", [128, 8], F32)
    cnt_tab = P.dram_in("cnt_tab", [128, 4 * HALO], F32)
    w_up_pool = P.dram_in("w_up_pool", [POOL_WIDTH, D], F32)
    qkvT = P.dram_out("qkvT", [3 * D, TOK], BF16)
    sgaT = P.dram_out("sgaT", [D, TOK], BF16)
    mpT = P.dram_out("mpT", [D, TOK], BF16)

    x_sb = P.sbuf("x_sb", [128, 16, TH], BF16)
    x_b = Buf("x")
    u_sb = P.sbuf("u_sb", [128, 8, TH], F32)
    u_b = [Buf("u%d" % c) for c in range(8)]
    pooled = P.sbuf("pooled", [128, 8, TOK], BF16)
    pooled_b = [Buf("pl%d" % c) for c in range(8)]
    ypool = P.sbuf("ypool", [128, 8, TOK], BF16)
    ypool_b = [Buf("yp%d" % c) for c in range(8)]
    tmpa = P.sbuf("tmpa", [128, TH], F32)
    tmpb = P.sbuf("tmpb", [128, TH], F32)
    tmpa_b, tmpb_b = Buf("tmpa"), Buf("tmpb")
    pw_sb = P.sbuf("pw_sb", [128, 8, 256], BF16)
    pw_b = Buf("pw")
    ps_sb = P.sbuf("ps_sb", [128, 8], F32)
    ps_b = Buf("psc")
    tab_sb = P.sbuf("tab_sb", [128, 4 * HALO], F32)
    tab_b = Buf("tab")
    NSTG = 4
    stg = [P.sbuf("stg%d" % i, [128, TOK], BF16) for i in range(NSTG)]
    stg_b = [Buf("stg%d" % i) for i in range(NSTG)]
    sg = [P.sbuf("sg%d" % i, [128, 4, TOK], BF16) for i in range(2)]
    sg_b = [Buf("sg%d" % i) for i in range(2)]
    ring = WRing(P, "win", 3, 16, 512)
    ring2 = WRing(P, "wup", 2, 8, 512)
    psr = PsumRing(P, 8)
    state = {"stg": 0, "evac": 0}

    xv = xT.rearrange("(k p) t -> p k t", p=128)
    for k0 in range(0, 16, 4):
        P.dma("sp", x_sb[:, k0:k0 + 4, :], xv[:, k0:k0 + 4, :], writes=[x_b], cont=k0 > 0)
    P.dma("sp", ps_sb[:, :], pool_scale[:, :], writes=[ps_b])
    P.dma("sp", tab_sb[:, :], cnt_tab[:, :], writes=[tab_b])
    P.dma("pool", pw_sb[:, :, :], pool_w.rearrange("(k p) f -> p k f", p=128), writes=[pw_b])

    def evac_engine():
        state["evac"] += 1
        return "act" if state["evac"] % 2 else "dve"

    def copy_out(eng, dst, src, reads, writes):
        if eng == "act":
            P.op("act", lambda e: e.activation(dst, src, AF.Copy), reads=reads, writes=writes)
        else:
            P.op("dve", lambda e: e.tensor_copy(dst, src), reads=reads, writes=writes)

    def inproj_group(j, kind):
        wt, wb = ring.load(w_in, j * 512)
        for m in range(4):
            f0 = j * 512 + m * 128
            if kind == "u":
                c = f0 // 128
                for (t0, tn) in ((HALO, 512), (HALO + 512, 512), (0, HALO)):
                    pt, pb = psr.next()
                    mm_group(P, pt[:, 0:tn], pb,
                             [(wt[:, k, m * 128:(m + 1) * 128], x_sb[:, k, t0:t0 + tn]) for k in range(16)],
                             reads=[wb, x_b])
                    copy_out(evac_engine(), u_sb[:, c, t0:t0 + tn], pt[:, 0:tn], [pb], [u_b[c]])
                continue
            if kind == "gp":
                dst_t, dst_b = sg[state["sgi"] % 2], sg_b[state["sgi"] % 2]
            else:
                si = state["stg"] % NSTG
                state["stg"] += 1
                dst_t, dst_b = stg[si], stg_b[si]
            for n in range(2):
                pt, pb = psr.next()
                mm_group(P, pt[:, :], pb,
                         [(wt[:, k, m * 128:(m + 1) * 128], x_sb[:, k, HALO + n * 512:HALO + (n + 1) * 512])
                          for k in range(16)], reads=[wb, x_b])
                if kind == "gp":
                    d = dst_t[:, m, n * 512:(n + 1) * 512]
                    P.op("act", lambda e, d=d, s=pt[:, :]: e.activation(d, s, AF.Sigmoid), reads=[pb], writes=[dst_b])
                elif kind == "ga":
                    d = dst_t[:, n * 512:(n + 1) * 512]
                    P.op("act", lambda e, d=d, s=pt[:, :]: e.activation(d, s, AF.Sigmoid), reads=[pb], writes=[dst_b])
                else:
                    copy_out(evac_engine(), dst_t[:, n * 512:(n + 1) * 512], pt[:, :], [pb], [dst_b])
            if kind == "qkv":
                r0 = f0 - POOL_WIDTH
                P.dma_out("sp", qkvT[r0:r0 + 128, :], dst_t[:, :], dst_b)
            elif kind == "ga":
                r0 = f0 - (POOL_WIDTH + 4 * D)
                P.dma_out("sp", sgaT[r0:r0 + 128, :], dst_t[:, :], dst_b)

    def pool_path():
        for c in range(8):
            g = c // 2
            w = POOL_WINDOWS[g]
            cur, cur_b = u_sb[:, c, :], u_b[c]
            s = 1
            outs = [(tmpa, tmpa_b), (tmpb, tmpb_b)]
            oi = 0
            while s < w:
                ot, ob = outs[oi % 2]
                oi += 1
                P.op("dve", lambda e, o=ot[:, s:TH], a=(cur[:, s:TH]), b=(cur[:, 0:TH - s]): e.tensor_tensor(o, a, b, ALU.add),
                     reads=[cur_b], writes=[ob])
                cur, cur_b = ot, ob
                s *= 2
            P.op("dve", lambda e, o=pooled[:, c, :], a=cur[:, HALO:TH], b=u_sb[:, c, HALO:TH], w=w:
                 e.scalar_tensor_tensor(o, a, 1.0 / w, b, ALU.mult, ALU.subtract),
                 reads=[cur_b, u_b[c]], writes=[pooled_b[c]])
            fix, fix_b = outs[oi % 2]
            P.op("dve", lambda e, o=fix[:, 0:HALO], a=cur[:, HALO:2 * HALO], b=tab_sb[:, g * HALO:(g + 1) * HALO]:
                 e.tensor_tensor(o, a, b, ALU.mult), reads=[cur_b, tab_b], writes=[fix_b])
            P.op("dve", lambda e, o=pooled[:, c, 0:HALO], a=fix[:, 0:HALO], b=u_sb[:, c, HALO:2 * HALO]:
                 e.tensor_tensor(o, a, b, ALU.subtract), reads=[fix_b, u_b[c]], writes=[pooled_b[c]])

    def poolw_mm():
        for c in range(8):
            g = c // 2
            mo = c % 2
            for n in range(2):
                pt, pb = psr.next()
                mm_group(P, pt[:, :], pb,
                         [(pw_sb[:, g * 2 + kk, mo * 128:(mo + 1) * 128], pooled[:, g * 2 + kk, n * 512:(n + 1) * 512])
                          for kk in range(2)], reads=[pw_b, pooled_b[g * 2], pooled_b[g * 2 + 1]])
                P.op("act", lambda e, d=ypool[:, c, n * 512:(n + 1) * 512], s=pt[:, :], sc=ps_sb[:, c:c + 1]:
                     e.activation(d, s, AF.Copy, scale=sc), reads=[pb, ps_b], writes=[ypool_b[c]])

    def uppool_group(j):
        wt, wb = ring2.load(w_up_pool, j * 512)
        sgt, sgb = sg[state["sgi"] % 2], sg_b[state["sgi"] % 2]
        for m in range(4):
            si = state["stg"] % NSTG
            state["stg"] += 1
            for n in range(2):
                pt, pb = psr.next()
                mm_group(P, pt[:, :], pb,
                         [(wt[:, k, m * 128:(m + 1) * 128], ypool[:, k, n * 512:(n + 1) * 512]) for k in range(8)],
                         reads=[wb] + ypool_b)
                P.op("dve", lambda e, d=stg[si][:, n * 512:(n + 1) * 512], a=pt[:, :], b=sgt[:, m, n * 512:(n + 1) * 512]:
                     e.tensor_tensor(d, a, b, ALU.mult), reads=[pb, sgb], writes=[stg_b[si]])
            r0 = j * 512 + m * 128
            P.dma_out("sp", mpT[r0:r0 + 128, :], stg[si][:, :], stg_b[si])

    state["sgi"] = 0
    inproj_group(0, "u")
    inproj_group(1, "u")
    for j in range(2, 6):
        inproj_group(j, "qkv")
    pool_path()
    poolw_mm()
    for j in range(6, 14):
        inproj_group(j, "qkv")
    for jj in range(4):
        state["sgi"] = jj
        inproj_group(14 + jj, "gp")
        uppool_group(jj)
    for j in range(18, 22):
        inproj_group(j, "ga")
    return P.finish()


def build_B(nblk=NBLK, hpc=2):
    P = Prog()
    S = nblk * BLK
    NT = S // 128
    qT = P.dram_in("qT", [hpc * 128, S], BF16)
    kT = P.dram_in("kT", [hpc * 128, S], BF16)
    v = P.dram_in("v", [hpc * S, 128], BF16)
    cmask = P.dram_in("cmask", [128, 512], BF16)
    o = P.dram_out("o", [hpc * S, 128], BF16)

    q_sb = [P.sbuf("q_sb%d" % h, [128, S], BF16) for h in range(hpc)]
    k_sb = [P.sbuf("k_sb%d" % h, [128, S], BF16) for h in range(hpc)]
    v_sb = [P.sbuf("v_sb%d" % h, [128, NT, 130], BF16) for h in range(hpc)]
    q_b = [Buf("q%d" % h) for h in range(hpc)]
    k_b = [Buf("k%d" % h) for h in range(hpc)]
    v_b = [Buf("v%d" % h) for h in range(hpc)]
    cm_sb = P.sbuf("cm_sb", [128, 512], BF16)
    cm_b = Buf("cm")
    km32 = P.sbuf("km32", [128, nblk], F32)
    kmh = P.sbuf("kmh", [128, nblk], BF16)
    kml32 = P.sbuf("kml32", [128, nblk], F32)
    kml = P.sbuf("kml", [128, nblk], BF16)
    km_b = Buf("km")
    NG = 2
    g_sb = [P.sbuf("g_sb%d" % i, [128, 2, 32], F32) for i in range(NG)]
    m_sb = [P.sbuf("m_sb%d" % i, [128, 2, 32], F32) for i in range(NG)]
    mx_sb = [P.sbuf("mx_sb%d" % i, [128, 2, 8], F32) for i in range(NG)]
    g_b = [Buf("g%d" % i) for i in range(NG)]
    m_b = [Buf("m%d" % i) for i in range(NG)]
    acc = [P.sbuf("acc%d" % i, [128, 2, 130], F32) for i in range(NG)]
    acc_b = [Buf("acc%d" % i) for i in range(NG)]
    rc = [P.sbuf("rc%d" % i, [128, 2], F32) for i in range(NG)]
    ob = [P.sbuf("ob%d" % i, [128, 2, 128], BF16) for i in range(NG)]
    ob_b = [Buf("ob%d" % i) for i in range(NG)]
    NPT = 3
    pT = [P.sbuf("pT%d" % i, [128, 512], BF16) for i in range(NPT)]
    pT_b = [Buf("pT%d" % i) for i in range(NPT)]
    ps_s = PsumRing(P, 3, "pss")
    ps_o = PsumRing(P, 3, "pso")
    ps_g = PsumRing(P, 2, "psg")
    scale = float(DH) ** -0.5

    P.dma("sp", cm_sb[:, :], cmask[:, :], writes=[cm_b])
    for h in range(hpc):
        P.dma("sp", q_sb[h][:, :], qT[h * 128:(h + 1) * 128, :], writes=[q_b[h]])
        P.dma("sp", k_sb[h][:, :], kT[h * 128:(h + 1) * 128, :], writes=[k_b[h]])
        P.op("pool", lambda e, t=v_sb[h]: e.memset(t[:, :, 128:130], 1.0), writes=[v_b[h]])
        vv = v[h * S:(h + 1) * S, :].rearrange("(t p) d -> p t d", p=128)
        half = NT // 2
        P.dma("sp", v_sb[h][:, 0:half, 0:128], vv[:, 0:half, :], writes=[v_b[h]])
        P.dma("sp", v_sb[h][:, half:NT, 0:128], vv[:, half:NT, :], writes=[v_b[h]], cont=True)

    def pre_head(h):
        P.op("dve", lambda e, h=h: e.tensor_reduce(km32[:, :], k_sb[h][:, :].rearrange("p (n j) -> p n j", j=BLK), AX.X, ALU.add),
             reads=[k_b[h]], writes=[km_b])
        P.op("dve", lambda e: e.tensor_scalar(km32[:, :], km32[:, :], 1.0 / BLK, None, ALU.mult), reads=[km_b], writes=[km_b])
        P.op("dve", lambda e: e.tensor_copy(kmh[:, :], km32[:, :]), reads=[km_b], writes=[km_b])
        P.op("dve", lambda e: e.tensor_tensor(kml32[:, :], km32[:, :], kmh[:, :], ALU.subtract), reads=[km_b], writes=[km_b])
        P.op("dve", lambda e: e.tensor_copy(kml[:, :], kml32[:, :]), reads=[km_b], writes=[km_b])

    def pre_qb(h, qb, gi):
        q0 = qb * BLK
        if qb <= TOPK:
            return
        gt, gb = ps_g.next()
        for t in range(2):
            ql = q_sb[h][:, q0 + t * 128:q0 + (t + 1) * 128]
            mm_group(P, gt[:, t * 32:t * 32 + qb], gb, [(ql, kmh[:, 0:qb]), (ql, kml[:, 0:qb])], reads=[q_b[h], km_b])
        P.op("dve", lambda e: e.memset(g_sb[gi][:, :, :], -1e30), writes=[g_b[gi]])
        P.op("dve", lambda e: e.tensor_copy(
            g_sb[gi][:, :, 0:qb], gt[:, 0:64].rearrange("p (t n) -> p t n", n=32)[:, :, 0:qb]), reads=[gb], writes=[g_b[gi]])
        for t in range(2):
            w8 = max(qb, 8)
            P.op("dve", lambda e, t=t, w8=w8: e.max(out=mx_sb[gi][:, t, :], in_=g_sb[gi][:, t, 0:w8]),
                 reads=[g_b[gi]], writes=[m_b[gi]])
            P.op("dve", lambda e, t=t: e.tensor_scalar(
                m_sb[gi][:, t, 0:qb], g_sb[gi][:, t, 0:qb], mx_sb[gi][:, t, 2:3], None, ALU.is_ge),
                reads=[g_b[gi], m_b[gi]], writes=[m_b[gi]])

    cnt = {"pt": 0}

    def stage1(h, qb, n):
        q0 = qb * BLK
        st, sb = ps_s.next()
        for kh in range(2):
            k0 = n * BLK + kh * 128
            P.op("pe", lambda e, kh=kh, k0=k0: e.matmul(
                st[:, kh * 256:(kh + 1) * 256], k_sb[h][:, k0:k0 + 128], q_sb[h][:, q0:q0 + 256],
                start=True, stop=True), reads=[k_b[h], q_b[h]] if kh == 0 else (), writes=[sb], signal=(kh == 1))
        pi = cnt["pt"] % NPT
        cnt["pt"] += 1
        P.op("act", lambda e: e.activation(pT[pi][:, :], st[:, :], AF.Exp, scale=scale), reads=[sb], writes=[pT_b[pi]])
        if n == qb:
            P.op("pool", lambda e: e.tensor_tensor(pT[pi][:, :], pT[pi][:, :], cm_sb[:, :], ALU.mult),
                 reads=[pT_b[pi], cm_b], writes=[pT_b[pi]])
        return pi

    def stage2(h, qb, n, gi, pi, last):
        q0 = qb * BLK
        ot, otb = ps_o.next()
        for t in range(2):
            for kh in range(2):
                P.op("pe", lambda e, t=t, kh=kh: e.matmul(
                    ot[:, t * 130:t * 130 + 129], pT[pi][:, kh * 256 + t * 128:kh * 256 + (t + 1) * 128],
                    v_sb[h][:, n * 2 + kh, 0:129], start=(kh == 0), stop=(kh == 1)),
                    reads=[pT_b[pi], v_b[h]] if (t == 0 and kh == 0) else (), writes=[otb],
                    signal=(t == 1 and kh == 1))
        if n == qb:
            P.op("dve", lambda e: e.tensor_copy(
                acc[gi][:, :, 0:129], ot[:, 0:260].rearrange("p (t c) -> p t c", c=130)[:, :, 0:129]),
                reads=[otb], writes=[acc_b[gi]])
        elif qb <= TOPK:
            P.op("dve", lambda e: e.tensor_tensor(
                acc[gi][:, :, 0:129], acc[gi][:, :, 0:129],
                ot[:, 0:260].rearrange("p (t c) -> p t c", c=130)[:, :, 0:129], ALU.add),
                reads=[otb, acc_b[gi]], writes=[acc_b[gi]])
        else:
            for t in range(2):
                P.op("dve", lambda e, t=t: e.scalar_tensor_tensor(
                    acc[gi][:, t, 0:129], ot[:, t * 130:t * 130 + 129], m_sb[gi][:, t, n:n + 1],
                    acc[gi][:, t, 0:129], ALU.mult, ALU.add),
                    reads=[otb, acc_b[gi], m_b[gi]], writes=[acc_b[gi]])
        if not last:
            return
        P.op("dve", lambda e: e.reciprocal(rc[gi][:, :], acc[gi][:, :, 128]), reads=[acc_b[gi]], writes=[acc_b[gi]])
        for t in range(2):
            P.op("dve", lambda e, t=t: e.tensor_scalar(
                ob[gi][:, t, :], acc[gi][:, t, 0:128], rc[gi][:, t:t + 1], None, ALU.mult),
                reads=[acc_b[gi]], writes=[ob_b[gi]])
        dst = o[h * S + q0:h * S + q0 + 256, :].rearrange("(t p) d -> p t d", p=128)
        P.dma_out("sp", dst, ob[gi][:, :, :], ob_b[gi])

    prev = None
    for h in range(hpc):
        for qb in range(nblk):
            gi = (h * nblk + qb) % NG
            order = [qb] + list(range(qb))
            for j, n in enumerate(order):
                if j == 0:
                    if qb == 0:
                        pre_head(h)
                    pre_qb(h, qb, gi)
                pi = stage1(h, qb, n)
                if prev is not None:
                    stage2(*prev)
                prev = (h, qb, n, gi, pi, j == len(order) - 1)
    stage2(*prev)
    return P.finish()


def causal_mask_tile():
    m = np.zeros((128, 512), np.float32)
    p = np.arange(128)[:, None]
    for kh in range(2):
        qq = np.arange(256)[None, :]
        m[:, kh * 256:(kh + 1) * 256] = (kh * 128 + p <= qq)
    return m.astype(NPBF)


class Ring:
    def __init__(self, P, name, n, shape, dtype):
        self.tiles = [P.sbuf("%s%d" % (name, i), shape, dtype) for i in range(n)]
        self.bufs = [Buf("%s%d" % (name, i)) for i in range(n)]
        self.i = 0

    def next(self):
        i = self.i % len(self.tiles)
        self.i += 1
        return self.tiles[i], self.bufs[i]


def emit_linear(P, psr, ring, w_ap, kc, nout, rhs_fn, rhs_bufs, ntok, evac, gw=256):
    for j in range(nout // gw):
        wt, wb = ring.load(w_ap, j * gw, width=gw, kc=kc)
        for m in range(gw // 128):
            for n in range(ntok // 512):
                pt, pb = psr.next()
                mm_group(P, pt[:, :], pb,
                         [(wt[:, k, m * 128:(m + 1) * 128], rhs_fn(k, n * 512, (n + 1) * 512)) for k in range(kc)],
                         reads=[wb] + list(rhs_bufs))
                evac(j * (gw // 128) + m, n, pt, pb)


def emit_ln(P, psr, x32, x_b, xb, xb_b, g_sb, b_sb, gb_b, ones_sb, ones_b, tmp, stat, ntok, out_scale=1.0):
    st_mean, st_rstd, st_msq, st_s1 = stat["mean"], stat["rstd"], stat["msq"], stat["s1"]
    st_b = stat["buf"]
    onesb, sqr = stat["onesb"], stat["sqr"]
    if "onesb_init" not in stat:
        stat["onesb_init"] = True
        P.op("dve", lambda e: e.tensor_copy(onesb[:, :], ones_sb[:, :]), reads=[ones_b], writes=[stat["onesb_b"]])
    gs, bs = g_sb, b_sb
    if out_scale != 1.0:
        gs, bs = stat["gs"], stat["bs"]
        P.op("dve", lambda e: e.tensor_scalar(gs[:, :], g_sb[:, :], float(out_scale), None, ALU.mult), reads=[gb_b], writes=[stat["gsb"]])
        P.op("dve", lambda e: e.tensor_scalar(bs[:, :], b_sb[:, :], float(out_scale), None, ALU.mult), reads=[gb_b], writes=[stat["gsb"]])
    gsb = stat["gsb"]

    def one(n):
        c0, c1 = n * 512, (n + 1) * 512
        P.op("dve", lambda e: e.tensor_reduce(st_s1[:, :], x32[:, :, c0:c1].rearrange("p k t -> p t k"), AX.X, ALU.add),
             reads=list(x_b), writes=[st_b])
        p1, p1b = psr.next()
        mm_group(P, p1[:, :], p1b, [(ones_sb[:, :], st_s1[:, :])], reads=[ones_b, st_b])
        p2, p2b = psr.next()
        for k in range(16):
            sq, sqb = sqr.next()
            P.op("act", lambda e, sq=sq, k=k: e.activation(sq[:, :], x32[:, k, c0:c1], AF.Square), reads=[x_b[k]], writes=[sqb])
            P.op("pe", lambda e, sq=sq, k=k: e.matmul(p2[:, :], onesb[:, :], sq[:, :], start=(k == 0), stop=(k == 15)),
                 reads=[sqb, stat["onesb_b"]], writes=[p2b], signal=True)
        P.op("dve", lambda e: e.tensor_scalar(st_mean[:, :], p1[:, :], 1.0 / D, None, ALU.mult), reads=[p1b], writes=[st_b])
        P.op("dve", lambda e: e.tensor_tensor(st_msq[:, :], st_mean[:, :], st_mean[:, :], ALU.mult), reads=[st_b], writes=[st_b])
        P.op("dve", lambda e: e.scalar_tensor_tensor(st_msq[:, :], p2[:, :], 1.0 / D, st_msq[:, :], ALU.mult, ALU.subtract),
             reads=[p2b, st_b], writes=[st_b])
        P.op("dve", lambda e: e.tensor_scalar(st_msq[:, :], st_msq[:, :], LN_EPS, None, ALU.add), reads=[st_b], writes=[st_b])
        P.op("act", lambda e: e.activation(st_rstd[:, :], st_msq[:, :], AF.Sqrt), reads=[st_b], writes=[st_b])
        P.op("dve", lambda e: e.reciprocal(st_rstd[:, :], st_rstd[:, :]), reads=[st_b], writes=[st_b])
        for k in range(16):
            t1, t1b = tmp.next()
            P.op("dve", lambda e, t1=t1, k=k: e.tensor_tensor(t1[:, :], x32[:, k, c0:c1], st_mean[:, :], ALU.subtract),
                 reads=[x_b[k], st_b], writes=[t1b])
            P.op("pool" if k % 3 else "dve", lambda e, t1=t1: e.tensor_tensor(t1[:, :], t1[:, :], st_rstd[:, :], ALU.mult),
                 reads=[t1b, st_b], writes=[t1b])
            P.op("act", lambda e, t1=t1, k=k: e.activation(x32[:, k, c0:c1], t1[:, :], AF.Identity,
                                                           bias=bs[:, k:k + 1], scale=gs[:, k:k + 1]),
                 reads=[t1b, gb_b, gsb], writes=[x_b[k]])
            P.op("act", lambda e, t1=t1, k=k: e.activation(xb[:, k, c0:c1], t1[:, :], AF.Identity,
                                                           bias=b_sb[:, k:k + 1], scale=g_sb[:, k:k + 1]),
                 reads=[t1b, gb_b], writes=[xb_b[k]])

    for n in range(ntok // 512):
        one(n)


def mk_stat(P):
    return {"mean": P.sbuf("st_mean", [128, 512], F32), "rstd": P.sbuf("st_rstd", [128, 512], F32),
            "msq": P.sbuf("st_msq", [128, 512], F32), "s1": P.sbuf("st_s1", [128, 512], F32), "buf": Buf("stat"),
            "onesb": P.sbuf("onesb", [128, 128], BF16), "onesb_b": Buf("onesb"),
            "sqr": Ring(P, "sqr", 3, [128, 512], BF16),
            "gs": P.sbuf("ln_gs", [128, 16], F32), "bs": P.sbuf("ln_bs", [128, 16], F32), "gsb": Buf("gsb")}


def emit_ffn(P, psr, ring_gu, ring_d, hring, tmp, xb, xb_b, acc, acc_b, wg, wu, wd, dff, ntok, first_copy):
    GW = 256
    chunks = [(c, min(c + 512, ntok)) for c in range(0, ntok, 512)]
    for j in range(dff // GW):
        wgt, wgb = ring_gu.load(wg, j * GW, width=GW, kc=16)
        wut, wub = ring_gu.load(wu, j * GW, width=GW, kc=16)
        ht, hb = hring.next()
        for m in range(GW // 128):
            for (c0, c1) in chunks:
                w = c1 - c0
                pg, pgb = psr.next()
                mm_group(P, pg[:, 0:w], pgb, [(wgt[:, k, m * 128:(m + 1) * 128], xb[:, k, c0:c1]) for k in range(16)],
                         reads=[wgb] + list(xb_b))
                pu, pub = psr.next()
                mm_group(P, pu[:, 0:w], pub, [(wut[:, k, m * 128:(m + 1) * 128], xb[:, k, c0:c1]) for k in range(16)],
                         reads=[wub] + list(xb_b))
                s, sb = tmp.next()
                P.op("act", lambda e, s=s, pg=pg, w=w: e.activation(s[:, 0:w], pg[:, 0:w], AF.Silu), reads=[pgb], writes=[sb])
                P.op("dve", lambda e, ht=ht, m=m, s=s, pu=pu, c0=c0, c1=c1, w=w: e.tensor_tensor(ht[:, m, c0:c1], s[:, 0:w], pu[:, 0:w], ALU.mult),
                     reads=[sb, pub], writes=[hb])
        wdt, wdb = ring_d.load_rows(wd, j * GW, GW // 128)
        for fo in range(16):
            for (c0, c1) in chunks:
                w = c1 - c0
                pt, pb = psr.next()
                mm_group(P, pt[:, 0:w], pb, [(wdt[:, kk, fo * 128:(fo + 1) * 128], ht[:, kk, c0:c1]) for kk in range(GW // 128)],
                         reads=[wdb, hb])
                if first_copy and j == 0:
                    P.op("dve", lambda e, fo=fo, pt=pt, c0=c0, c1=c1, w=w: e.tensor_copy(acc[:, fo, c0:c1], pt[:, 0:w]),
                         reads=[pb], writes=[acc_b[fo]])
                else:
                    P.op("dve", lambda e, fo=fo, pt=pt, c0=c0, c1=c1, w=w: e.tensor_tensor(acc[:, fo, c0:c1], acc[:, fo, c0:c1], pt[:, 0:w], ALU.add),
                         reads=[pb, acc_b[fo]], writes=[acc_b[fo]])


class DRing:
    def __init__(self, P, name, nslots, rk):
        self.P = P
        self.tiles = [P.sbuf("%s%d" % (name, i), [128, rk, D], BF16) for i in range(nslots)]
        self.bufs = [Buf("%s%d" % (name, i)) for i in range(nslots)]
        self.i = 0

    def load_rows(self, w_ap, r0, rk, q="pool"):
        i = self.i % len(self.tiles)
        self.i += 1
        t, b = self.tiles[i], self.bufs[i]
        src = w_ap[r0:r0 + rk * 128, :].rearrange("(k p) f -> p k f", p=128)
        self.P.dma(q, t[:, 0:rk, :], src, writes=[b])
        return t, b


def load_small(P, name, dram_ap, shape, dtype=F32, q="sp"):
    t = P.sbuf(name, shape, dtype)
    b = Buf(name)
    P.dma(q, t[tuple(slice(None) for _ in shape)], dram_ap, writes=[b])
    return t, b


def build_C(variant, ntok=TOK):
    P = Prog()
    oT = P.dram_in("oT", [D, ntok], BF16)
    sgaT = P.dram_in("sgaT", [D, ntok], BF16)
    mpT = P.dram_in("mpT", [D, ntok], BF16)
    xT32 = P.dram_in("xT32", [D, ntok], F32)
    w_up_attn = P.dram_in("w_up_attn", [D, D], F32)
    w_o = P.dram_in("w_o", [D, D], F32)
    lnm_g = P.dram_in("lnm_g", [128, 16], F32)
    lnm_b = P.dram_in("lnm_b", [128, 16], F32)
    ones_d = P.dram_in("ones", [128, 128], F32)
    if variant == "dense":
        w_gate = P.dram_in("w_gate", [D, D_FF], F32)
        w_up = P.dram_in("w_up", [D, D_FF], F32)
        w_down = P.dram_in("w_down", [D_FF, D], F32)
        lnf_g = P.dram_in("lnf_g", [128, 16], F32)
        lnf_b = P.dram_in("lnf_b", [128, 16], F32)
    else:
        router = P.dram_in("router", [D, NE], F32)
        wfull = P.dram_out("wfull", [ntok, NE], F32)
    xo32 = P.dram_out("xo32", [D, ntok], F32)
    xob = P.dram_out("xob", [D, ntok], BF16)

    x32 = P.sbuf("x32", [128, 16, ntok], F32)
    x_b = [Buf("x32_%d" % k) for k in range(16)]
    ob = P.sbuf("ob", [128, 16, ntok], BF16)
    ob_b = [Buf("ob_%d" % k) for k in range(16)]
    mb = P.sbuf("mb", [128, 16, ntok], BF16)
    mb_b = [Buf("mb_%d" % k) for k in range(16)]
    ring = WRing(P, "w", 4, 16, 256)
    psr = PsumRing(P, 8)
    tmp = Ring(P, "tmp", 4, [128, 512], F32)
    stat = mk_stat(P)
    sgr = Ring(P, "sgr", 2, [128, ntok], BF16)
    mpr = Ring(P, "mpr", 2, [128, ntok], BF16)
    ones_sb, ones_b = load_small(P, "ones_sb", ones_d[:, :], [128, 128])
    g1, g1b = load_small(P, "lnm_g_sb", lnm_g[:, :], [128, 16])
    b1, b1b = load_small(P, "lnm_b_sb", lnm_b[:, :], [128, 16])
    gb1 = Buf("gb1")
    gb1.w = None
    if variant == "dense":
        g2, g2b = load_small(P, "lnf_g_sb", lnf_g[:, :], [128, 16])
        b2, b2b = load_small(P, "lnf_b_sb", lnf_b[:, :], [128, 16])
    else:
        rt_sb = P.sbuf("rt_sb", [128, 16, NE], F32)
        rt_b = Buf("rt")
        P.dma("sp", rt_sb[:, :, :], router.rearrange("(k p) e -> p k e", p=128), writes=[rt_b])

    ov = oT.rearrange("(k p) t -> p k t", p=128)
    xv = xT32.rearrange("(k p) t -> p k t", p=128)
    for k in range(16):
        P.dma("sp", ob[:, k, :], ov[:, k, :], writes=[ob_b[k]])
    for k in range(16):
        P.dma("act", x32[:, k, :], xv[:, k, :], writes=[x_b[k]])

    cur = {}

    def evac_up(fc, n, pt, pb):
        if n == 0:
            st, sb = sgr.next()
            mt, mtb = mpr.next()
            P.dma("sp", st[:, :], sgaT[fc * 128:(fc + 1) * 128, :], writes=[sb])
            P.dma("sp", mt[:, :], mpT[fc * 128:(fc + 1) * 128, :], writes=[mtb])
            cur["s"] = (st, sb, mt, mtb)
        st, sb, mt, mtb = cur["s"]
        c0, c1 = n * 512, (n + 1) * 512
        t1, t1b = tmp.next()
        P.op("dve", lambda e: e.tensor_tensor(t1[:, :], pt[:, :], st[:, c0:c1], ALU.mult), reads=[pb, sb], writes=[t1b])
        P.op("pool", lambda e: e.tensor_tensor(mb[:, fc, c0:c1], t1[:, :], mt[:, c0:c1], ALU.add), reads=[t1b, mtb], writes=[mb_b[fc]])

    emit_linear(P, psr, ring, w_up_attn, 16, D, lambda k, c0, c1: ob[:, k, c0:c1], ob_b, ntok, evac_up)

    def evac_o(fc, n, pt, pb):
        c0, c1 = n * 512, (n + 1) * 512
        P.op("dve", lambda e: e.scalar_tensor_tensor(x32[:, fc, c0:c1], x32[:, fc, c0:c1], float(ALPHA), pt[:, :], ALU.mult, ALU.add),
             reads=[pb, x_b[fc]], writes=[x_b[fc]])

    emit_linear(P, psr, ring, w_o, 16, D, lambda k, c0, c1: mb[:, k, c0:c1], mb_b, ntok, evac_o)

    gbb = Buf("gbb")
    P.op("dve", lambda e: e.tensor_copy(g1[:, :], g1[:, :]), reads=[g1b, b1b], writes=[gbb])
    emit_ln(P, psr, x32, x_b, ob, ob_b, g1, b1, gbb, ones_sb, ones_b, tmp, stat, ntok,
            out_scale=(float(ALPHA) if variant == "dense" else 1.0))

    if variant == "moe":
        xo32v = xo32.rearrange("(k p) t -> p k t", p=128)
        xobv = xob.rearrange("(k p) t -> p k t", p=128)
        for k in range(16):
            P.dma_out("sp", xo32v[:, k, :], x32[:, k, :], x_b[k])
            P.dma_out("act", xobv[:, k, :], ob[:, k, :], ob_b[k])
        ntile = ntok // 128
        lg = P.sbuf("lg", [128, ntile, NE], F32)
        mx = P.sbuf("mx", [128, ntile, 8], F32)
        msk = P.sbuf("msk", [128, ntile, NE], F32)
        ex = P.sbuf("ex", [128, ntile, NE], F32)
        ssum = P.sbuf("ssum", [128, ntile], F32)
        wf = P.sbuf("wf", [128, ntile, NE], F32)
        r_b = Buf("router_work")
        for tt in range(ntile):
            pt, pb = psr.next()
            mm_group(P, pt[:, 0:NE], pb, [(x32[:, k, tt * 128:(tt + 1) * 128], rt_sb[:, k, :]) for k in range(16)],
                     reads=[rt_b] + x_b)
            P.op("dve", lambda e, tt=tt, pt=pt: e.tensor_copy(lg[:, tt, :], pt[:, 0:NE]), reads=[pb], writes=[r_b])
            P.op("dve", lambda e, tt=tt: e.max(out=mx[:, tt, :], in_=lg[:, tt, :]), reads=[r_b], writes=[r_b])
            P.op("dve", lambda e, tt=tt: e.tensor_scalar(msk[:, tt, :], lg[:, tt, :], mx[:, tt, 1:2], None, ALU.is_ge), reads=[r_b], writes=[r_b])
            P.op("dve", lambda e, tt=tt: e.tensor_scalar(lg[:, tt, :], lg[:, tt, :], mx[:, tt, 0:1], None, ALU.subtract), reads=[r_b], writes=[r_b])
            P.op("act", lambda e, tt=tt: e.activation(ex[:, tt, :], lg[:, tt, :], AF.Exp), reads=[r_b], writes=[r_b])
            P.op("dve", lambda e, tt=tt: e.tensor_tensor(ex[:, tt, :], ex[:, tt, :], msk[:, tt, :], ALU.mult), reads=[r_b], writes=[r_b])
            P.op("dve", lambda e, tt=tt: e.tensor_reduce(ssum[:, tt:tt + 1], ex[:, tt, :], AX.X, ALU.add), reads=[r_b], writes=[r_b])
            P.op("dve", lambda e, tt=tt: e.reciprocal(ssum[:, tt:tt + 1], ssum[:, tt:tt + 1]), reads=[r_b], writes=[r_b])
            P.op("dve", lambda e, tt=tt: e.tensor_scalar(wf[:, tt, :], ex[:, tt, :], ssum[:, tt:tt + 1], None, ALU.mult), reads=[r_b], writes=[r_b])
        P.dma_out("sp", wfull.rearrange("(t p) e -> p t e", p=128), wf[:, :, :], r_b)
        return P.finish()

    ring_d = DRing(P, "wd", 2, 2)
    class HR:
        def __init__(self):
            self.i = 0

        def next(self):
            s = self.i % 4
            self.i += 1
            return mb[:, 2 * s:2 * s + 2, :], HRB[s]
    HRB = [Buf("h%d" % s) for s in range(4)]
    for s in range(4):
        HRB[s].r = list(mb_b[2 * s].r) + list(mb_b[2 * s + 1].r)
        HRB[s].w = mb_b[2 * s].w
    emit_ffn(P, psr, ring, ring_d, HR(), tmp, ob, ob_b, x32, x_b, w_gate, w_up, w_down, D_FF, ntok, first_copy=False)
    gbb2 = Buf("gbb2")
    P.op("dve", lambda e: e.tensor_copy(g2[:, :], g2[:, :]), reads=[g2b, b2b], writes=[gbb2])
    emit_ln(P, psr, x32, x_b, ob, ob_b, g2, b2, gbb2, ones_sb, ones_b, tmp, stat, ntok)
    xo32v = xo32.rearrange("(k p) t -> p k t", p=128)
    xobv = xob.rearrange("(k p) t -> p k t", p=128)
    for k in range(16):
        P.dma_out("sp", xo32v[:, k, :], x32[:, k, :], x_b[k])
        P.dma_out("act", xobv[:, k, :], ob[:, k, :], ob_b[k])
    return P.finish()


def build_E(C, BW):
    P = Prog()
    xeT = P.dram_in("xeT", [D, C], BF16)
    w_gate = P.dram_in("w_gate", [D, D_FFE], F32)
    w_up = P.dram_in("w_up", [D, D_FFE], F32)
    w_down = P.dram_in("w_down", [D_FFE, D], F32)
    yT = P.dram_out("yT", [D, C], F32)
    xb = P.sbuf("xb", [128, 16, BW], BF16)
    xb_b = [Buf("xb_%d" % k) for k in range(16)]
    acc = P.sbuf("acc", [128, 16, BW], F32)
    acc_b = [Buf("acc_%d" % k) for k in range(16)]
    ring = WRing(P, "w", 4, 16, 256)
    ring_d = DRing(P, "wd", 2, 2)
    hring = Ring(P, "h", 3, [128, 2, BW], BF16)
    tmp = Ring(P, "tmp", 3, [128, 512], F32)
    psr = PsumRing(P, 8)
    xv = xeT.rearrange("(k p) t -> p k t", p=128)
    yv = yT.rearrange("(k p) t -> p k t", p=128)
    for off in range(0, C, BW):
        for k in range(16):
            P.dma("sp", xb[:, k, :], xv[:, k, off:off + BW], writes=[xb_b[k]])
        emit_ffn(P, psr, ring, ring_d, hring, tmp, xb, xb_b, acc, acc_b, w_gate, w_up, w_down, D_FFE, BW, first_copy=True)
        for k in range(16):
            P.dma_out("sp", yv[:, k, off:off + BW], acc[:, k, :], acc_b[k])
    return P.finish()


def build_F(ntok=TOK):
    P = Prog()
    xT32 = P.dram_in("xT32", [D, ntok], F32)
    y1T = P.dram_in("y1T", [D, ntok], F32)
    y2T = P.dram_in("y2T", [D, ntok], F32)
    wb = P.dram_in("wb", [128, 2 * ntok], F32)
    lnf_g = P.dram_in("lnf_g", [128, 16], F32)
    lnf_b = P.dram_in("lnf_b", [128, 16], F32)
    ones_d = P.dram_in("ones", [128, 128], F32)
    xo32 = P.dram_out("xo32", [D, ntok], F32)
    xob = P.dram_out("xob", [D, ntok], BF16)
    x32 = P.sbuf("x32", [128, 16, ntok], F32)
    x_b = [Buf("x32_%d" % k) for k in range(16)]
    ob = P.sbuf("ob", [128, 16, ntok], BF16)
    ob_b = [Buf("ob_%d" % k) for k in range(16)]
    y1r = Ring(P, "y1r", 2, [128, ntok], F32)
    y2r = Ring(P, "y2r", 2, [128, ntok], F32)
    tmp = Ring(P, "tmp", 4, [128, 512], F32)
    stat = mk_stat(P)
    psr = PsumRing(P, 8)
    ones_sb, ones_b = load_small(P, "ones_sb", ones_d[:, :], [128, 128])
    g2, g2b = load_small(P, "lnf_g_sb", lnf_g[:, :], [128, 16])
    b2, b2b = load_small(P, "lnf_b_sb", lnf_b[:, :], [128, 16])
    wb_sb, wb_b = load_small(P, "wb_sb", wb[:, :], [128, 2 * ntok])
    xv = xT32.rearrange("(k p) t -> p k t", p=128)
    for k in range(16):
        P.dma("act", x32[:, k, :], xv[:, k, :], writes=[x_b[k]])

    def one(k):
        y1, y1b = y1r.next()
        y2, y2b = y2r.next()
        P.dma("sp", y1[:, :], y1T[k * 128:(k + 1) * 128, :], writes=[y1b])
        P.dma("sp", y2[:, :], y2T[k * 128:(k + 1) * 128, :], writes=[y2b])
        P.op("dve", lambda e: e.tensor_tensor(y1[:, :], y1[:, :], wb_sb[:, 0:ntok], ALU.mult), reads=[y1b, wb_b], writes=[y1b])
        P.op("pool", lambda e: e.tensor_tensor(y2[:, :], y2[:, :], wb_sb[:, ntok:2 * ntok], ALU.mult), reads=[y2b, wb_b], writes=[y2b])
        P.op("dve", lambda e: e.scalar_tensor_tensor(x32[:, k, :], x32[:, k, :], float(ALPHA), y1[:, :], ALU.mult, ALU.add),
             reads=[x_b[k], y1b], writes=[x_b[k]])
        P.op("dve", lambda e: e.tensor_tensor(x32[:, k, :], x32[:, k, :], y2[:, :], ALU.add), reads=[x_b[k], y2b], writes=[x_b[k]])

    for k in range(16):
        one(k)
    gbb = Buf("gbb")
    P.op("dve", lambda e: e.tensor_copy(g2[:, :], g2[:, :]), reads=[g2b, b2b], writes=[gbb])
    emit_ln(P, psr, x32, x_b, ob, ob_b, g2, b2, gbb, ones_sb, ones_b, tmp, stat, ntok)
    xo32v = xo32.rearrange("(k p) t -> p k t", p=128)
    xobv = xob.rearrange("(k p) t -> p k t", p=128)
    for k in range(16):
        P.dma_out("sp", xo32v[:, k, :], x32[:, k, :], x_b[k])
        P.dma_out("act", xobv[:, k, :], ob[:, k, :], ob_b[k])
    return P.finish()


def build_L(ntok=TOK):
    P = Prog()
    xT32 = P.dram_in("xT32", [D, ntok], F32)
    ln_g = P.dram_in("ln_g", [128, 16], F32)
    ln_b = P.dram_in("ln_b", [128, 16], F32)
    ones_d = P.dram_in("ones", [128, 128], F32)
    xo32 = P.dram_out("xo32", [D, ntok], F32)
    xob = P.dram_out("xob", [D, ntok], BF16)
    x32 = P.sbuf("x32", [128, 16, ntok], F32)
    x_b = [Buf("x32_%d" % k) for k in range(16)]
    ob = P.sbuf("ob", [128, 16, ntok], BF16)
    ob_b = [Buf("ob_%d" % k) for k in range(16)]
    tmp = Ring(P, "tmp", 4, [128, 512], F32)
    stat = mk_stat(P)
    psr = PsumRing(P, 8)
    ones_sb, ones_b = load_small(P, "ones_sb", ones_d[:, :], [128, 128])
    g2, g2b = load_small(P, "ln_g_sb", ln_g[:, :], [128, 16])
    b2, b2b = load_small(P, "ln_b_sb", ln_b[:, :], [128, 16])
    xv = xT32.rearrange("(k p) t -> p k t", p=128)
    for k in range(16):
        P.dma("act", x32[:, k, :], xv[:, k, :], writes=[x_b[k]])
    gbb = Buf("gbb")
    P.op("dve", lambda e: e.tensor_copy(g2[:, :], g2[:, :]), reads=[g2b, b2b], writes=[gbb])
    emit_ln(P, psr, x32, x_b, ob, ob_b, g2, b2, gbb, ones_sb, ones_b, tmp, stat, ntok)
    xo32v = xo32.rearrange("(k p) t -> p k t", p=128)
    xobv = xob.rearrange("(k p) t -> p k t", p=128)
    for k in range(16):
        P.dma_out("sp", xo32v[:, k, :], x32[:, k, :], x_b[k])
        P.dma_out("act", xobv[:, k, :], ob[:, k, :], ob_b[k])
    return P.finish()


_PROGS = {}


def _prog(key, builder, *a):
    if key not in _PROGS:
        _PROGS[key] = builder(*a)
    return _PROGS[key]


def _lay16(v):
    return np.ascontiguousarray(np.asarray(v, np.float32).reshape(16, 128).T)


def _cnt_tab(core):
    tab = np.zeros((128, 4 * HALO), np.float32)
    for g, w in enumerate(POOL_WINDOWS):
        for i in range(HALO):
            tab[:, g * HALO + i] = 1.0 / min(core * TOK + i + 1, w)
    return tab


def kernel(x, ln_in_g, ln_in_b, w_in, pool_w, pool_scale, w_up_pool, w_up_attn, w_o,
           ln_mix_g, ln_mix_b, ffn_w_gate, ffn_w_up, ffn_w_down, moe_router, moe_w_gate,
           moe_w_up, moe_w_down, ln_ffn_g, ln_ffn_b):
    f32 = lambda a: np.ascontiguousarray(np.asarray(a, np.float32))
    ones = np.ones((128, 128), np.float32)
    cmask = causal_mask_tile()
    x2 = np.asarray(x, np.float32).reshape(SEQ, D)

    ins = [{"xT32": np.ascontiguousarray(x2[c * TOK:(c + 1) * TOK].T), "ln_g": _lay16(ln_in_g), "ln_b": _lay16(ln_in_b),
            "ones": ones} for c in range(NCORES)]
    res = run_prog(_prog("L", build_L), ins)
    x32 = [r["xo32"] for r in res]
    xb = [r["xob"] for r in res]

    for l in range(DEPTH):
        ins = []
        w_in_l = f32(w_in[l])
        pw_l = f32(pool_w[l]).reshape(4 * 256, 256)
        psc_l = np.ascontiguousarray(np.asarray(pool_scale[l], np.float32).reshape(8, 128).T)
        wup_l = f32(w_up_pool[l])
        for c in range(NCORES):
            halo = xb[c - 1][:, TOK - HALO:] if c > 0 else np.zeros((D, HALO), NPBF)
            ins.append({"xT": np.ascontiguousarray(np.concatenate([halo, xb[c]], axis=1)), "w_in": w_in_l, "pool_w": pw_l,
                        "pool_scale": psc_l, "cnt_tab": _cnt_tab(c), "w_up_pool": wup_l})
        resA = run_prog(_prog("A", build_A), ins)
        del ins, w_in_l
        qkv = np.concatenate([r["qkvT"] for r in resA], axis=1)
        ins = []
        for c in range(NCORES):
            r0 = c * 256
            vT = qkv[2 * D + r0:2 * D + r0 + 256]
            v = np.ascontiguousarray(vT.reshape(2, 128, SEQ).transpose(0, 2, 1)).reshape(2 * SEQ, 128)
            ins.append({"qT": np.ascontiguousarray(qkv[r0:r0 + 256]), "kT": np.ascontiguousarray(qkv[D + r0:D + r0 + 256]),
                        "v": v, "cmask": cmask})
        resB = run_prog(_prog("B", build_B), ins)
        del ins, qkv
        o_all = np.stack([r["o"].reshape(2, SEQ, 128) for r in resB], 0).reshape(NH, SEQ, 128)
        oT_all = np.ascontiguousarray(o_all.transpose(0, 2, 1)).reshape(D, SEQ)
        i = l // 2
        variant = "dense" if l % 2 == 0 else "moe"
        ins = []
        common = {"w_up_attn": f32(w_up_attn[l]), "w_o": f32(w_o[l]), "lnm_g": _lay16(ln_mix_g[l]), "lnm_b": _lay16(ln_mix_b[l]),
                  "ones": ones}
        if variant == "dense":
            common.update({"w_gate": f32(ffn_w_gate[i]), "w_up": f32(ffn_w_up[i]), "w_down": f32(ffn_w_down[i]),
                           "lnf_g": _lay16(ln_ffn_g[l]), "lnf_b": _lay16(ln_ffn_b[l])})
        else:
            common.update({"router": f32(moe_router[i])})
        for c in range(NCORES):
            d = {"oT": np.ascontiguousarray(oT_all[:, c * TOK:(c + 1) * TOK]), "sgaT": resA[c]["sgaT"], "mpT": resA[c]["mpT"],
                 "xT32": x32[c]}
            d.update(common)
            ins.append(d)
        resC = run_prog(_prog("C" + variant, build_C, variant), ins)
        del ins, common, oT_all, o_all, resA, resB
        x32 = [r["xo32"] for r in resC]
        xb = [r["xob"] for r in resC]
        if variant == "dense":
            continue
        wfull = np.concatenate([r["wfull"] for r in resC], axis=0)
        sel = wfull > 0
        rank = np.cumsum(sel, axis=1)
        xb_all = np.concatenate(xb, axis=1)
        toks = [np.nonzero(sel[:, e])[0] for e in range(NE)]
        cmax = max(1, max(len(t) for t in toks))
        nbat = -(-cmax // 1280)
        BW = -(-(-(-cmax // nbat)) // 128) * 128
        C = nbat * BW
        ins = []
        for e in range(NE):
            xe = np.zeros((D, C), NPBF)
            xe[:, :len(toks[e])] = xb_all[:, toks[e]]
            ins.append({"xeT": xe, "w_gate": f32(moe_w_gate[i][e]), "w_up": f32(moe_w_up[i][e]), "w_down": f32(moe_w_down[i][e])})
        resE = run_prog(_prog("E%d_%d" % (C, BW), build_E, C, BW), ins)
        del ins, xb_all
        y1 = np.zeros((D, SEQ), np.float32)
        y2 = np.zeros((D, SEQ), np.float32)
        w1 = np.zeros((SEQ,), np.float32)
        w2 = np.zeros((SEQ,), np.float32)
        for e in range(NE):
            t = toks[e]
            ye = resE[e]["yT"][:, :len(t)]
            first = rank[t, e] == 1
            second = rank[t, e] == 2
            y1[:, t[first]] = ye[:, first]
            y2[:, t[second]] = ye[:, second]
            w1[t[first]] = wfull[t[first], e]
            w2[t[second]] = wfull[t[second], e]
        del resE
        ins = []
        for c in range(NCORES):
            sl = slice(c * TOK, (c + 1) * TOK)
            wbc = np.ascontiguousarray(np.broadcast_to(np.concatenate([w1[sl], w2[sl]])[None, :], (128, 2 * TOK)))
            ins.append({"xT32": x32[c], "y1T": np.ascontiguousarray(y1[:, sl]), "y2T": np.ascontiguousarray(y2[:, sl]), "wb": wbc,
                        "lnf_g": _lay16(ln_ffn_g[l]), "lnf_b": _lay16(ln_ffn_b[l]), "ones": ones})
        resF = run_prog(_prog("F", build_F), ins)
        del ins, y1, y2
        x32 = [r["xo32"] for r in resF]
        xb = [r["xob"] for r in resF]

    out = np.concatenate([a.T for a in x32], axis=0).reshape(1, SEQ, D)
    return np.ascontiguousarray(out.astype(np.float32))
```

```python
import contextlib
import numpy as np
import ml_dtypes
import concourse.bass as bass
import concourse.mybir as mybir
from concourse.bass_utils import run_bass_kernel_spmd

F32 = mybir.dt.float32
BF16 = mybir.dt.bfloat16
ALU = mybir.AluOpType
AF = mybir.ActivationFunctionType
AX = mybir.AxisListType
NPBF = ml_dtypes.bfloat16

NCORES = 8
D = 2048
SEQ = 8192
DEPTH = 4
TOK = SEQ // NCORES
HALO = 16
POOL_WINDOWS = (2, 4, 8, 16)
POOL_WIDTH = 1024
NH = 16
DH = 128
BLK = 256
NBLK = SEQ // BLK
TOPK = 3
IN_WIDTH = POOL_WIDTH + 3 * D + 2 * D
D_FF = 5632
NE = 8
D_FFE = 7168
ALPHA = (2 * DEPTH) ** 0.25
LN_EPS = 1e-5

COMPUTE = ("pe", "act", "dve", "pool")


class Buf:
    __slots__ = ("name", "w", "r", "dsem")

    def __init__(self, name):
        self.name = name
        self.w = None
        self.r = []
        self.dsem = None


class Prog:
    def __init__(self):
        self.nc = bass.Bass("TRN2", target_bir_lowering=False)
        self.es = contextlib.ExitStack()
        self.streams = {e: [] for e in ("pe", "act", "dve", "pool", "sp")}
        self.sems = {}
        self.count = {}
        self.waited = {e: {} for e in self.streams}
        self.ndsem = 0
        for e in COMPUTE:
            self._mksem("c_" + e)
        self.out_bufs = []

    def _mksem(self, key):
        self.sems[key] = self.es.enter_context(self.nc.semaphore(key))
        self.count[key] = 0
        return key

    def dram_in(self, name, shape, dtype):
        return self.nc.dram_tensor(name, list(shape), dtype, kind="ExternalInput").ap()

    def dram_out(self, name, shape, dtype):
        return self.nc.dram_tensor(name, list(shape), dtype, kind="ExternalOutput").ap()

    def sbuf(self, name, shape, dtype):
        return self.es.enter_context(self.nc.sbuf_tensor(name, list(shape), dtype))

    def psum(self, name, shape, dtype=F32):
        return self.es.enter_context(self.nc.psum_tensor(name, list(shape), dtype))

    def _need(self, eng, ev):
        if ev is None:
            return
        key, val = ev
        if self.waited[eng].get(key, 0) >= val:
            return
        self.waited[eng][key] = val
        self.streams[eng].append(("wait", key, val))

    def _deps(self, eng, reads, writes):
        for b in reads:
            self._need(eng, b.w)
        for b in writes:
            self._need(eng, b.w)
            for ev in b.r:
                self._need(eng, ev)

    def _commit(self, ev, reads, writes):
        for b in reads:
            b.r.append(ev)
        for b in writes:
            b.w = ev
            b.r = []

    def op(self, eng, fn, reads=(), writes=(), signal=True):
        self._deps(eng, reads, writes)
        key = "c_" + eng
        if signal:
            self.count[key] += 1
            ev = (key, self.count[key])
            self.streams[eng].append(("op", fn, key, 1))
            self._commit(ev, reads, writes)
        else:
            self.streams[eng].append(("op", fn, None, 0))

    def dma(self, q, out_ap, in_ap, reads=(), writes=(), cont=False):
        bufs = list(reads) + list(writes)
        owner = bufs[0]
        if owner.dsem is None:
            owner.dsem = self._mksem("d%d_%s" % (self.ndsem, owner.name))
            self.ndsem += 1
        key = owner.dsem
        for b in bufs[1:]:
            assert b.dsem is None or b.dsem == key
            b.dsem = key
        if not cont:
            self._deps(q, reads, writes)
        self.count[key] += 16
        ev = (key, self.count[key])
        self.streams[q].append(("op", lambda e, o=out_ap, i=in_ap: e.dma_start(out=o, in_=i), key, 16))
        self._commit(ev, reads, writes)
        return ev

    def dma_out(self, q, out_ap, in_ap, src):
        ev = self.dma(q, out_ap, in_ap, reads=[src])
        self.final_events = getattr(self, "final_events", {})
        self.final_events[ev[0]] = ev[1]

    def finish(self):
        nc = self.nc
        for key, val in getattr(self, "final_events", {}).items():
            self.streams["sp"].append(("wait", key, val))
        for e in COMPUTE:
            if self.count["c_" + e]:
                self.streams["sp"].append(("wait", "c_" + e, self.count["c_" + e]))
        sems = self.sems
        streams = self.streams

        def replay(eng_obj, items):
            for it in items:
                if it[0] == "wait":
                    eng_obj.wait_ge(sems[it[1]], it[2])
                else:
                    ins = it[1](eng_obj)
                    if it[2] is not None:
                        ins.then_inc(sems[it[2]], it[3])

        with nc.Block() as block:
            @block.tensor
            def _(e):
                replay(e, streams["pe"])

            @block.scalar
            def _(e):
                replay(e, streams["act"])

            @block.vector
            def _(e):
                replay(e, streams["dve"])

            @block.gpsimd
            def _(e):
                replay(e, streams["pool"])

            @block.sync
            def _(e):
                replay(e, streams["sp"])
        self.es.close()
        return nc


def run_prog(nc, in_maps):
    res = run_bass_kernel_spmd(nc, in_maps, core_ids=list(range(len(in_maps))))
    return res.results


class PsumRing:
    def __init__(self, P, n, name="ps"):
        self.tiles = [P.psum("%s%d" % (name, i), [128, 512]) for i in range(n)]
        self.bufs = [Buf("%s%d" % (name, i)) for i in range(n)]
        self.i = 0

    def next(self):
        i = self.i % len(self.tiles)
        self.i += 1
        return self.tiles[i], self.bufs[i]


class WRing:
    def __init__(self, P, name, nslots, kc, width):
        self.P = P
        self.kc = kc
        self.width = width
        self.tiles = [P.sbuf("%s%d" % (name, i), [128, kc, width], BF16) for i in range(nslots)]
        self.bufs = [Buf("%s%d" % (name, i)) for i in range(nslots)]
        self.i = 0

    def load(self, w_ap, c0, width=None, kc=None, q="pool"):
        width = width or self.width
        kc = kc or self.kc
        i = self.i % len(self.tiles)
        self.i += 1
        t, b = self.tiles[i], self.bufs[i]
        src = w_ap.rearrange("(k p) f -> p k f", p=128)[:, :, c0:c0 + width]
        h = kc // 2 if kc >= 2 else kc
        self.P.dma(q, t[:, 0:h, 0:width], src[:, 0:h, :], writes=[b])
        if h < kc:
            self.P.dma(q, t[:, h:kc, 0:width], src[:, h:kc, :], writes=[b], cont=True)
        return t, b


def mm_group(P, ps_ap, ps_buf, pairs, reads):
    n = len(pairs)
    for i, (l, r) in enumerate(pairs):
        last = i == n - 1
        P.op("pe", lambda e, l=l, r=r, i=i, last=last: e.matmul(ps_ap, l, r, start=(i == 0), stop=last),
             reads=reads if (i == 0 or last) else (), writes=[ps_buf], signal=last)


def build_A():
    P = Prog()
    TH = TOK + HALO
    xT = P.dram_in("xT", [D, TH], BF16)
    w_in = P.dram_in("w_in", [D, IN_WIDTH], F32)
    pool_w = P.dram_in("pool_w", [4 * 256, 256], F32)
    pool_scale = P.dram_in("pool_scale", [128, 8], F32)
    cnt_tab = P.dram_in("cnt_tab", [128, 4 * HALO], F32)
    w_up_pool = P.dram_in("w_up_pool", [POOL_WIDTH, D], F32)
    qkvT = P.dram_out("qkvT", [3 * D, TOK], BF16)
    sgaT = P.dram_out("sgaT", [D, TOK], BF16)
    mpT = P.dram_out("mpT", [D, TOK], BF16)

    x_sb = P.sbuf("x_sb", [128, 16, TH], BF16)
    x_b = Buf("x")
    u_sb = P.sbuf("u_sb", [128, 8, TH], F32)
    u_b = [Buf("u%d" % c) for c in range(8)]
    pooled = P.sbuf("pooled", [128, 8, TOK], BF16)
    pooled_b = [Buf("pl%d" % c) for c in range(8)]
    ypool = P.sbuf("ypool", [128, 8, TOK], BF16)
    ypool_b = [Buf("yp%d" % c) for c in range(8)]
    tmpa = P.sbuf("tmpa", [128, TH], F32)
    tmpb = P.sbuf("tmpb", [128, TH], F32)
    tmpa_b, tmpb_b = Buf("tmpa"), Buf("tmpb")
    pw_sb = P.sbuf("pw_sb", [128, 8, 256], BF16)
    pw_b = Buf("pw")
    ps_sb = P.sbuf("ps_sb", [128, 8], F32)
    ps_b = Buf("psc")
    tab_sb = P.sbuf("tab_sb", [128, 4 * HALO], F32)
    tab_b = Buf("tab")
    NSTG = 4
    stg = [P.sbuf("stg%d" % i, [128, TOK], BF16) for i in range(NSTG)]
    stg_b = [Buf("stg%d" % i) for i in range(NSTG)]
    sg = [P.sbuf("sg%d" % i, [128, 4, TOK], BF16) for i in range(2)]
    sg_b = [Buf("sg%d" % i) for i in range(2)]
    ring = WRing(P, "win", 3, 16, 512)
    ring2 = WRing(P, "wup", 2, 8, 512)
    psr = PsumRing(P, 8)
    state = {"stg": 0, "evac": 0}

    xv = xT.rearrange("(k p) t -> p k t", p=128)
    for k0 in range(0, 16, 4):
        P.dma("sp", x_sb[:, k0:k0 + 4, :], xv[:, k0:k0 + 4, :], writes=[x_b], cont=k0 > 0)
    P.dma("sp", ps_sb[:, :], pool_scale[:, :], writes=[ps_b])
    P.dma("sp", tab_sb[:, :], cnt_tab[:, :], writes=[tab_b])
    P.dma("pool", pw_sb[:, :, :], pool_w.rearrange("(k p) f -> p k f", p=128), writes=[pw_b])

    def evac_engine():
        state["evac"] += 1
        return "act" if state["evac"] % 2 else "dve"

    def copy_out(eng, dst, src, reads, writes):
        if eng == "act":
            P.op("act", lambda e: e.activation(dst, src, AF.Copy), reads=reads, writes=writes)
        else:
            P.op("dve", lambda e: e.tensor_copy(dst, src), reads=reads, writes=writes)

    def inproj_group(j, kind):
        wt, wb = ring.load(w_in, j * 512)
        for m in range(4):
            f0 = j * 512 + m * 128
            if kind == "u":
                c = f0 // 128
                for (t0, tn) in ((HALO, 512), (HALO + 512, 512), (0, HALO)):
                    pt, pb = psr.next()
                    mm_group(P, pt[:, 0:tn], pb,
                             [(wt[:, k, m * 128:(m + 1) * 128], x_sb[:, k, t0:t0 + tn]) for k in range(16)],
                             reads=[wb, x_b])
                    copy_out(evac_engine(), u_sb[:, c, t0:t0 + tn], pt[:, 0:tn], [pb], [u_b[c]])
                continue
            if kind == "gp":
                dst_t, dst_b = sg[state["sgi"] % 2], sg_b[state["sgi"] % 2]
            else:
                si = state["stg"] % NSTG
                state["stg"] += 1
                dst_t, dst_b = stg[si], stg_b[si]
            for n in range(2):
                pt, pb = psr.next()
                mm_group(P, pt[:, :], pb,
                         [(wt[:, k, m * 128:(m + 1) * 128], x_sb[:, k, HALO + n * 512:HALO + (n + 1) * 512])
                          for k in range(16)], reads=[wb, x_b])
                if kind == "gp":
                    d = dst_t[:, m, n * 512:(n + 1) * 512]
                    P.op("act", lambda e, d=d, s=pt[:, :]: e.activation(d, s, AF.Sigmoid), reads=[pb], writes=[dst_b])
                elif kind == "ga":
                    d = dst_t[:, n * 512:(n + 1) * 512]
                    P.op("act", lambda e, d=d, s=pt[:, :]: e.activation(d, s, AF.Sigmoid), reads=[pb], writes=[dst_b])
                else:
                    copy_out(evac_engine(), dst_t[:, n * 512:(n + 1) * 512], pt[:, :], [pb], [dst_b])
            if kind == "qkv":
                r0 = f0 - POOL_WIDTH
                P.dma_out("sp", qkvT[r0:r0 + 128, :], dst_t[:, :], dst_b)
            elif kind == "ga":
                r0 = f0 - (POOL_WIDTH + 4 * D)
                P.dma_out("sp", sgaT[r0:r0 + 128, :], dst_t[:, :], dst_b)

    def pool_path():
        for c in range(8):
            g = c // 2
            w = POOL_WINDOWS[g]
            cur, cur_b = u_sb[:, c, :], u_b[c]
            s = 1
            outs = [(tmpa, tmpa_b), (tmpb, tmpb_b)]
            oi = 0
            while s < w:
                ot, ob = outs[oi % 2]
                oi += 1
                P.op("dve", lambda e, o=ot[:, s:TH], a=(cur[:, s:TH]), b=(cur[:, 0:TH - s]): e.tensor_tensor(o, a, b, ALU.add),
                     reads=[cur_b], writes=[ob])
                cur, cur_b = ot, ob
                s *= 2
            P.op("dve", lambda e, o=pooled[:, c, :], a=cur[:, HALO:TH], b=u_sb[:, c, HALO:TH], w=w:
                 e.scalar_tensor_tensor(o, a, 1.0 / w, b, ALU.mult, ALU.subtract),
                 reads=[cur_b, u_b[c]], writes=[pooled_b[c]])
            fix, fix_b = outs[oi % 2]
            P.op("dve", lambda e, o=fix[:, 0:HALO], a=cur[:, HALO:2 * HALO], b=tab_sb[:, g * HALO:(g + 1) * HALO]:
                 e.tensor_tensor(o, a, b, ALU.mult), reads=[cur_b, tab_b], writes=[fix_b])
            P.op("dve", lambda e, o=pooled[:, c, 0:HALO], a=fix[:, 0:HALO], b=u_sb[:, c, HALO:2 * HALO]:
                 e.tensor_tensor(o, a, b, ALU.subtract), reads=[fix_b, u_b[c]], writes=[pooled_b[c]])

    def poolw_mm():
        for c in range(8):
            g = c // 2
            mo = c % 2
            for n in range(2):
                pt, pb = psr.next()
                mm_group(P, pt[:, :], pb,
                         [(pw_sb[:, g * 2 + kk, mo * 128:(mo + 1) * 128], pooled[:, g * 2 + kk, n * 512:(n + 1) * 512])
                          for kk in range(2)], reads=[pw_b, pooled_b[g * 2], pooled_b[g * 2 + 1]])
                P.op("act", lambda e, d=ypool[:, c, n * 512:(n + 1) * 512], s=pt[:, :], sc=ps_sb[:, c:c + 1]:
                     e.activation(d, s, AF.Copy, scale=sc), reads=[pb, ps_b], writes=[ypool_b[c]])

    def uppool_group(j):
        wt, wb = ring2.load(w_up_pool, j * 512)
        sgt, sgb = sg[state["sgi"] % 2], sg_b[state["sgi"] % 2]
        for m in range(4):
            si = state["stg"] % NSTG
            state["stg"] += 1
            for n in range(2):
                pt, pb = psr.next()
                mm_group(P, pt[:, :], pb,
                         [(wt[:, k, m * 128:(m + 1) * 128], ypool[:, k, n * 512:(n + 1) * 512]) for k in range(8)],
                         reads=[wb] + ypool_b)
                P.op("dve", lambda e, d=stg[si][:, n * 512:(n + 1) * 512], a=pt[:, :], b=sgt[:, m, n * 512:(n + 1) * 512]:
                     e.tensor_tensor(d, a, b, ALU.mult), reads=[pb, sgb], writes=[stg_b[si]])
            r0 = j * 512 + m * 128
            P.dma_out("sp", mpT[r0:r0 + 128, :], stg[si][:, :], stg_b[si])

    state["sgi"] = 0
    inproj_group(0, "u")
    inproj_group(1, "u")
    for j in range(2, 6):
        inproj_group(j, "qkv")
    pool_path()
    poolw_mm()
    for j in range(6, 14):
        inproj_group(j, "qkv")
    for jj in range(4):
        state["sgi"] = jj
        inproj_group(14 + jj, "gp")
        uppool_group(jj)
    for j in range(18, 22):
        inproj_group(j, "ga")
    return P.finish()


def build_B(nblk=NBLK, hpc=2):
    P = Prog()
    S = nblk * BLK
    NT = S // 128
    qT = P.dram_in("qT", [hpc * 128, S], BF16)
    kT = P.dram_in("kT", [hpc * 128, S], BF16)
    v = P.dram_in("v", [hpc * S, 128], BF16)
    cmask = P.dram_in("cmask", [128, 512], BF16)
    o = P.dram_out("o", [hpc * S, 128], BF16)

    q_sb = [P.sbuf("q_sb%d" % h, [128, S], BF16) for h in range(hpc)]
    k_sb = [P.sbuf("k_sb%d" % h, [128, S], BF16) for h in range(hpc)]
    v_sb = [P.sbuf("v_sb%d" % h, [128, NT, 130], BF16) for h in range(hpc)]
    q_b = [Buf("q%d" % h) for h in range(hpc)]
    k_b = [Buf("k%d" % h) for h in range(hpc)]
    v_b = [Buf("v%d" % h) for h in range(hpc)]
    cm_sb = P.sbuf("cm_sb", [128, 512], BF16)
    cm_b = Buf("cm")
    km32 = P.sbuf("km32", [128, nblk], F32)
    kmh = P.sbuf("kmh", [128, nblk], BF16)
    kml32 = P.sbuf("kml32", [128, nblk], F32)
    kml = P.sbuf("kml", [128, nblk], BF16)
    km_b = Buf("km")
    NG = 2
    g_sb = [P.sbuf("g_sb%d" % i, [128, 2, 32], F32) for i in range(NG)]
    m_sb = [P.sbuf("m_sb%d" % i, [128, 2, 32], F32) for i in range(NG)]
    mx_sb = [P.sbuf("mx_sb%d" % i, [128, 2, 8], F32) for i in range(NG)]
    g_b = [Buf("g%d" % i) for i in range(NG)]
    m_b = [Buf("m%d" % i) for i in range(NG)]
    acc = [P.sbuf("acc%d" % i, [128, 2, 130], F32) for i in range(NG)]
    acc_b = [Buf("acc%d" % i) for i in range(NG)]
    rc = [P.sbuf("rc%d" % i, [128, 2], F32) for i in range(NG)]
    ob = [P.sbuf("ob%d" % i, [128, 2, 128], BF16) for i in range(NG)]
    ob_b = [Buf("ob%d" % i) for i in range(NG)]
    NPT = 3
    pT = [P.sbuf("pT%d" % i, [128, 512], BF16) for i in range(NPT)]
    pT_b = [Buf("pT%d" % i) for i in range(NPT)]
    ps_s = PsumRing(P, 3, "pss")
    ps_o = PsumRing(P, 3, "pso")
    ps_g = PsumRing(P, 2, "psg")
    scale = float(DH) ** -0.5

    P.dma("sp", cm_sb[:, :], cmask[:, :], writes=[cm_b])
    for h in range(hpc):
        P.dma("sp", q_sb[h][:, :], qT[h * 128:(h + 1) * 128, :], writes=[q_b[h]])
        P.dma("sp", k_sb[h][:, :], kT[h * 128:(h + 1) * 128, :], writes=[k_b[h]])
        P.op("pool", lambda e, t=v_sb[h]: e.memset(t[:, :, 128:130], 1.0), writes=[v_b[h]])
        vv = v[h * S:(h + 1) * S, :].rearrange("(t p) d -> p t d", p=128)
        half = NT // 2
        P.dma("sp", v_sb[h][:, 0:half, 0:128], vv[:, 0:half, :], writes=[v_b[h]])
        P.dma("sp", v_sb[h][:, half:NT, 0:128], vv[:, half:NT, :], writes=[v_b[h]], cont=True)

    def pre_head(h):
        P.op("dve", lambda e, h=h: e.tensor_reduce(km32[:, :], k_sb[h][:, :].rearrange("p (n j) -> p n j", j=BLK), AX.X, ALU.add),
             reads=[k_b[h]], writes=[km_b])
        P.op("dve", lambda e: e.tensor_scalar(km32[:, :], km32[:, :], 1.0 / BLK, None, ALU.mult), reads=[km_b], writes=[km_b])
        P.op("dve", lambda e: e.tensor_copy(kmh[:, :], km32[:, :]), reads=[km_b], writes=[km_b])
        P.op("dve", lambda e: e.tensor_tensor(kml32[:, :], km32[:, :], kmh[:, :], ALU.subtract), reads=[km_b], writes=[km_b])
        P.op("dve", lambda e: e.tensor_copy(kml[:, :], kml32[:, :]), reads=[km_b], writes=[km_b])

    def pre_qb(h, qb, gi):
        q0 = qb * BLK
        if qb <= TOPK:
            return
        gt, gb = ps_g.next()
        for t in range(2):
            ql = q_sb[h][:, q0 + t * 128:q0 + (t + 1) * 128]
            mm_group(P, gt[:, t * 32:t * 32 + qb], gb, [(ql, kmh[:, 0:qb]), (ql, kml[:, 0:qb])], reads=[q_b[h], km_b])
        P.op("dve", lambda e: e.memset(g_sb[gi][:, :, :], -1e30), writes=[g_b[gi]])
        P.op("dve", lambda e: e.tensor_copy(
            g_sb[gi][:, :, 0:qb], gt[:, 0:64].rearrange("p (t n) -> p t n", n=32)[:, :, 0:qb]), reads=[gb], writes=[g_b[gi]])
        for t in range(2):
            w8 = max(qb, 8)
            P.op("dve", lambda e, t=t, w8=w8: e.max(out=mx_sb[gi][:, t, :], in_=g_sb[gi][:, t, 0:w8]),
                 reads=[g_b[gi]], writes=[m_b[gi]])
            P.op("dve", lambda e, t=t: e.tensor_scalar(
                m_sb[gi][:, t, 0:qb], g_sb[gi][:, t, 0:qb], mx_sb[gi][:, t, 2:3], None, ALU.is_ge),
                reads=[g_b[gi], m_b[gi]], writes=[m_b[gi]])

    cnt = {"pt": 0}

    def stage1(h, qb, n):
        q0 = qb * BLK
        st, sb = ps_s.next()
        for kh in range(2):
            k0 = n * BLK + kh * 128
            P.op("pe", lambda e, kh=kh, k0=k0: e.matmul(
                st[:, kh * 256:(kh + 1) * 256], k_sb[h][:, k0:k0 + 128], q_sb[h][:, q0:q0 + 256],
                start=True, stop=True), reads=[k_b[h], q_b[h]] if kh == 0 else (), writes=[sb], signal=(kh == 1))
        pi = cnt["pt"] % NPT
        cnt["pt"] += 1
        P.op("act", lambda e: e.activation(pT[pi][:, :], st[:, :], AF.Exp, scale=scale), reads=[sb], writes=[pT_b[pi]])
        if n == qb:
            P.op("pool", lambda e: e.tensor_tensor(pT[pi][:, :], pT[pi][:, :], cm_sb[:, :], ALU.mult),
                 reads=[pT_b[pi], cm_b], writes=[pT_b[pi]])
        return pi

    def stage2(h, qb, n, gi, pi, last):
        q0 = qb * BLK
        ot, otb = ps_o.next()
        for t in range(2):
            for kh in range(2):
                P.op("pe", lambda e, t=t, kh=kh: e.matmul(
                    ot[:, t * 130:t * 130 + 129], pT[pi][:, kh * 256 + t * 128:kh * 256 + (t + 1) * 128],
                    v_sb[h][:, n * 2 + kh, 0:129], start=(kh == 0), stop=(kh == 1)),
                    reads=[pT_b[pi], v_b[h]] if (t == 0 and kh == 0) else (), writes=[otb],
                    signal=(t == 1 and kh == 1))
        if n == qb:
            P.op("dve", lambda e: e.tensor_copy(
                acc[gi][:, :, 0:129], ot[:, 0:260].rearrange("p (t c) -> p t c", c=130)[:, :, 0:129]),
                reads=[otb], writes=[acc_b[gi]])
        elif qb <= TOPK:
            P.op("dve", lambda e: e.tensor_tensor(
                acc[gi][:, :, 0:129], acc[gi][:, :, 0:129],
                ot[:, 0:260].rearrange("p (t c) -> p t c", c=130)[:, :, 0:129], ALU.add),
                reads=[otb, acc_b[gi]], writes=[acc_b[gi]])
        else:
            for t in range(2):
                P.op("dve", lambda e, t=t: e.scalar_tensor_tensor(
                    acc[gi][:, t, 0:129], ot[:, t * 130:t * 130 + 129], m_sb[gi][:, t, n:n + 1],
                    acc[gi][:, t, 0:129], ALU.mult, ALU.add),
                    reads=[otb, acc_b[gi], m_b[gi]], writes=[acc_b[gi]])
        if not last:
            return
        P.op("dve", lambda e: e.reciprocal(rc[gi][:, :], acc[gi][:, :, 128]), reads=[acc_b[gi]], writes=[acc_b[gi]])
        for t in range(2):
            P.op("dve", lambda e, t=t: e.tensor_scalar(
                ob[gi][:, t, :], acc[gi][:, t, 0:128], rc[gi][:, t:t + 1], None, ALU.mult),
                reads=[acc_b[gi]], writes=[ob_b[gi]])
        dst = o[h * S + q0:h * S + q0 + 256, :].rearrange("(t p) d -> p t d", p=128)
        P.dma_out("sp", dst, ob[gi][:, :, :], ob_b[gi])

    prev = None
    for h in range(hpc):
        for qb in range(nblk):
            gi = (h * nblk + qb) % NG
            order = [qb] + list(range(qb))
            for j, n in enumerate(order):
                if j == 0:
                    if qb == 0:
                        pre_head(h)
                    pre_qb(h, qb, gi)
                pi = stage1(h, qb, n)
                if prev is not None:
                    stage2(*prev)
                prev = (h, qb, n, gi, pi, j == len(order) - 1)
    stage2(*prev)
    return P.finish()


def causal_mask_tile():
    m = np.zeros((128, 512), np.float32)
    p = np.arange(128)[:, None]
    for kh in range(2):
        qq = np.arange(256)[None, :]
        m[:, kh * 256:(kh + 1) * 256] = (kh * 128 + p <= qq)
    return m.astype(NPBF)


class Ring:
    def __init__(self, P, name, n, shape, dtype):
        self.tiles = [P.sbuf("%s%d" % (name, i), shape, dtype) for i in range(n)]
        self.bufs = [Buf("%s%d" % (name, i)) for i in range(n)]
        self.i = 0

    def next(self):
        i = self.i % len(self.tiles)
        self.i += 1
        return self.tiles[i], self.bufs[i]


def emit_linear(P, psr, ring, w_ap, kc, nout, rhs_fn, rhs_bufs, ntok, evac, gw=256):
    for j in range(nout // gw):
        wt, wb = ring.load(w_ap, j * gw, width=gw, kc=kc)
        for m in range(gw // 128):
            for n in range(ntok // 512):
                pt, pb = psr.next()
                mm_group(P, pt[:, :], pb,
                         [(wt[:, k, m * 128:(m + 1) * 128], rhs_fn(k, n * 512, (n + 1) * 512)) for k in range(kc)],
                         reads=[wb] + list(rhs_bufs))
                evac(j * (gw // 128) + m, n, pt, pb)


def emit_ln(P, psr, x32, x_b, xb, xb_b, g_sb, b_sb, gb_b, ones_sb, ones_b, tmp, stat, ntok, out_scale=1.0):
    st_mean, st_rstd, st_msq, st_s1 = stat["mean"], stat["rstd"], stat["msq"], stat["s1"]
    st_b = stat["buf"]
    onesb, sqr = stat["onesb"], stat["sqr"]
    if "onesb_init" not in stat:
        stat["onesb_init"] = True
        P.op("dve", lambda e: e.tensor_copy(onesb[:, :], ones_sb[:, :]), reads=[ones_b], writes=[stat["onesb_b"]])
    gs, bs = g_sb, b_sb
    if out_scale != 1.0:
        gs, bs = stat["gs"], stat["bs"]
        P.op("dve", lambda e: e.tensor_scalar(gs[:, :], g_sb[:, :], float(out_scale), None, ALU.mult), reads=[gb_b], writes=[stat["gsb"]])
        P.op("dve", lambda e: e.tensor_scalar(bs[:, :], b_sb[:, :], float(out_scale), None, ALU.mult), reads=[gb_b], writes=[stat["gsb"]])
    gsb = stat["gsb"]

    def one(n):
        c0, c1 = n * 512, (n + 1) * 512
        P.op("dve", lambda e: e.tensor_reduce(st_s1[:, :], x32[:, :, c0:c1].rearrange("p k t -> p t k"), AX.X, ALU.add),
             reads=list(x_b), writes=[st_b])
        p1, p1b = psr.next()
        mm_group(P, p1[:, :], p1b, [(ones_sb[:, :], st_s1[:, :])], reads=[ones_b, st_b])
        p2, p2b = psr.next()
        for k in range(16):
            sq, sqb = sqr.next()
            P.op("act", lambda e, sq=sq, k=k: e.activation(sq[:, :], x32[:, k, c0:c1], AF.Square), reads=[x_b[k]], writes=[sqb])
            P.op("pe", lambda e, sq=sq, k=k: e.matmul(p2[:, :], onesb[:, :], sq[:, :], start=(k == 0), stop=(k == 15)),
                 reads=[sqb, stat["onesb_b"]], writes=[p2b], signal=True)
        P.op("dve", lambda e: e.tensor_scalar(st_mean[:, :], p1[:, :], 1.0 / D, None, ALU.mult), reads=[p1b], writes=[st_b])
        P.op("dve", lambda e: e.tensor_tensor(st_msq[:, :], st_mean[:, :], st_mean[:, :], ALU.mult), reads=[st_b], writes=[st_b])
        P.op("dve", lambda e: e.scalar_tensor_tensor(st_msq[:, :], p2[:, :], 1.0 / D, st_msq[:, :], ALU.mult, ALU.subtract),
             reads=[p2b, st_b], writes=[st_b])
        P.op("dve", lambda e: e.tensor_scalar(st_msq[:, :], st_msq[:, :], LN_EPS, None, ALU.add), reads=[st_b], writes=[st_b])
        P.op("act", lambda e: e.activation(st_rstd[:, :], st_msq[:, :], AF.Sqrt), reads=[st_b], writes=[st_b])
        P.op("dve", lambda e: e.reciprocal(st_rstd[:, :], st_rstd[:, :]), reads=[st_b], writes=[st_b])
        for k in range(16):
            t1, t1b = tmp.next()
            P.op("dve", lambda e, t1=t1, k=k: e.tensor_tensor(t1[:, :], x32[:, k, c0:c1], st_mean[:, :], ALU.subtract),
                 reads=[x_b[k], st_b], writes=[t1b])
            P.op("pool" if k % 3 else "dve", lambda e, t1=t1: e.tensor_tensor(t1[:, :], t1[:, :], st_rstd[:, :], ALU.mult),
                 reads=[t1b, st_b], writes=[t1b])
            P.op("act", lambda e, t1=t1, k=k: e.activation(x32[:, k, c0:c1], t1[:, :], AF.Identity,
                                                           bias=bs[:, k:k + 1], scale=gs[:, k:k + 1]),
                 reads=[t1b, gb_b, gsb], writes=[x_b[k]])
            P.op("act", lambda e, t1=t1, k=k: e.activation(xb[:, k, c0:c1], t1[:, :], AF.Identity,
                                                           bias=b_sb[:, k:k + 1], scale=g_sb[:, k:k + 1]),
                 reads=[t1b, gb_b], writes=[xb_b[k]])

    for n in range(ntok // 512):
        one(n)


def mk_stat(P):
    return {"mean": P.sbuf("st_mean", [128, 512], F32), "rstd": P.sbuf("st_rstd", [128, 512], F32),
            "msq": P.sbuf("st_msq", [128, 512], F32), "s1": P.sbuf("st_s1", [128, 512], F32), "buf": Buf("stat"),
            "onesb": P.sbuf("onesb", [128, 128], BF16), "onesb_b": Buf("onesb"),
            "sqr": Ring(P, "sqr", 3, [128, 512], BF16),
            "gs": P.sbuf("ln_gs", [128, 16], F32), "bs": P.sbuf("ln_bs", [128, 16], F32), "gsb": Buf("gsb")}


def emit_ffn(P, psr, ring_gu, ring_d, hring, tmp, xb, xb_b, acc, acc_b, wg, wu, wd, dff, ntok, first_copy):
    GW = 256
    chunks = [(c, min(c + 512, ntok)) for c in range(0, ntok, 512)]
    for j in range(dff // GW):
        wgt, wgb = ring_gu.load(wg, j * GW, width=GW, kc=16)
        wut, wub = ring_gu.load(wu, j * GW, width=GW, kc=16)
        ht, hb = hring.next()
        for m in range(GW // 128):
            for (c0, c1) in chunks:
                w = c1 - c0
                pg, pgb = psr.next()
                mm_group(P, pg[:, 0:w], pgb, [(wgt[:, k, m * 128:(m + 1) * 128], xb[:, k, c0:c1]) for k in range(16)],
                         reads=[wgb] + list(xb_b))
                pu, pub = psr.next()
                mm_group(P, pu[:, 0:w], pub, [(wut[:, k, m * 128:(m + 1) * 128], xb[:, k, c0:c1]) for k in range(16)],
                         reads=[wub] + list(xb_b))
                s, sb = tmp.next()
                P.op("act", lambda e, s=s, pg=pg, w=w: e.activation(s[:, 0:w], pg[:, 0:w], AF.Silu), reads=[pgb], writes=[sb])
                P.op("dve", lambda e, ht=ht, m=m, s=s, pu=pu, c0=c0, c1=c1, w=w: e.tensor_tensor(ht[:, m, c0:c1], s[:, 0:w], pu[:, 0:w], ALU.mult),
                     reads=[sb, pub], writes=[hb])
        wdt, wdb = ring_d.load_rows(wd, j * GW, GW // 128)
        for fo in range(16):
            for (c0, c1) in chunks:
                w = c1 - c0
                pt, pb = psr.next()
                mm_group(P, pt[:, 0:w], pb, [(wdt[:, kk, fo * 128:(fo + 1) * 128], ht[:, kk, c0:c1]) for kk in range(GW // 128)],
                         reads=[wdb, hb])
                if first_copy and j == 0:
                    P.op("dve", lambda e, fo=fo, pt=pt, c0=c0, c1=c1, w=w: e.tensor_copy(acc[:, fo, c0:c1], pt[:, 0:w]),
                         reads=[pb], writes=[acc_b[fo]])
                else:
                    P.op("dve", lambda e, fo=fo, pt=pt, c0=c0, c1=c1, w=w: e.tensor_tensor(acc[:, fo, c0:c1], acc[:, fo, c0:c1], pt[:, 0:w], ALU.add),
                         reads=[pb, acc_b[fo]], writes=[acc_b[fo]])


class DRing:
    def __init__(self, P, name, nslots, rk):
        self.P = P
        self.tiles = [P.sbuf("%s%d" % (name, i), [128, rk, D], BF16) for i in range(nslots)]
        self.bufs = [Buf("%s%d" % (name, i)) for i in range(nslots)]
        self.i = 0

    def load_rows(self, w_ap, r0, rk, q="pool"):
        i = self.i % len(self.tiles)
        self.i += 1
        t, b = self.tiles[i], self.bufs[i]
        src = w_ap[r0:r0 + rk * 128, :].rearrange("(k p) f -> p k f", p=128)
        self.P.dma(q, t[:, 0:rk, :], src, writes=[b])
        return t, b


def load_small(P, name, dram_ap, shape, dtype=F32, q="sp"):
    t = P.sbuf(name, shape, dtype)
    b = Buf(name)
    P.dma(q, t[tuple(slice(None) for _ in shape)], dram_ap, writes=[b])
    return t, b


def build_C(variant, ntok=TOK):
    P = Prog()
    oT = P.dram_in("oT", [D, ntok], BF16)
    sgaT = P.dram_in("sgaT", [D, ntok], BF16)
    mpT = P.dram_in("mpT", [D, ntok], BF16)
    xT32 = P.dram_in("xT32", [D, ntok], F32)
    w_up_attn = P.dram_in("w_up_attn", [D, D], F32)
    w_o = P.dram_in("w_o", [D, D], F32)
    lnm_g = P.dram_in("lnm_g", [128, 16], F32)
    lnm_b = P.dram_in("lnm_b", [128, 16], F32)
    ones_d = P.dram_in("ones", [128, 128], F32)
    if variant == "dense":
        w_gate = P.dram_in("w_gate", [D, D_FF], F32)
        w_up = P.dram_in("w_up", [D, D_FF], F32)
        w_down = P.dram_in("w_down", [D_FF, D], F32)
        lnf_g = P.dram_in("lnf_g", [128, 16], F32)
        lnf_b = P.dram_in("lnf_b", [128, 16], F32)
    else:
        router = P.dram_in("router", [D, NE], F32)
        wfull = P.dram_out("wfull", [ntok, NE], F32)
    xo32 = P.dram_out("xo32", [D, ntok], F32)
    xob = P.dram_out("xob", [D, ntok], BF16)

    x32 = P.sbuf("x32", [128, 16, ntok], F32)
    x_b = [Buf("x32_%d" % k) for k in range(16)]
    ob = P.sbuf("ob", [128, 16, ntok], BF16)
    ob_b = [Buf("ob_%d" % k) for k in range(16)]
    mb = P.sbuf("mb", [128, 16, ntok], BF16)
    mb_b = [Buf("mb_%d" % k) for k in range(16)]
    ring = WRing(P, "w", 4, 16, 256)
    psr = PsumRing(P, 8)
    tmp = Ring(P, "tmp", 4, [128, 512], F32)
    stat = mk_stat(P)
    sgr = Ring(P, "sgr", 2, [128, ntok], BF16)
    mpr = Ring(P, "mpr", 2, [128, ntok], BF16)
    ones_sb, ones_b = load_small(P, "ones_sb", ones_d[:, :], [128, 128])
    g1, g1b = load_small(P, "lnm_g_sb", lnm_g[:, :], [128, 16])
    b1, b1b = load_small(P, "lnm_b_sb", lnm_b[:, :], [128, 16])
    gb1 = Buf("gb1")
    gb1.w = None
    if variant == "dense":
        g2, g2b = load_small(P, "lnf_g_sb", lnf_g[:, :], [128, 16])
        b2, b2b = load_small(P, "lnf_b_sb", lnf_b[:, :], [128, 16])
    else:
        rt_sb = P.sbuf("rt_sb", [128, 16, NE], F32)
        rt_b = Buf("rt")
        P.dma("sp", rt_sb[:, :, :], router.rearrange("(k p) e -> p k e", p=128), writes=[rt_b])

    ov = oT.rearrange("(k p) t -> p k t", p=128)
    xv = xT32.rearrange("(k p) t -> p k t", p=128)
    for k in range(16):
        P.dma("sp", ob[:, k, :], ov[:, k, :], writes=[ob_b[k]])
    for k in range(16):
        P.dma("act", x32[:, k, :], xv[:, k, :], writes=[x_b[k]])

    cur = {}

    def evac_up(fc, n, pt, pb):
        if n == 0:
            st, sb = sgr.next()
            mt, mtb = mpr.next()
            P.dma("sp", st[:, :], sgaT[fc * 128:(fc + 1) * 128, :], writes=[sb])
            P.dma("sp", mt[:, :], mpT[fc * 128:(fc + 1) * 128, :], writes=[mtb])
            cur["s"] = (st, sb, mt, mtb)
        st, sb, mt, mtb = cur["s"]
        c0, c1 = n * 512, (n + 1) * 512
        t1, t1b = tmp.next()
        P.op("dve", lambda e: e.tensor_tensor(t1[:, :], pt[:, :], st[:, c0:c1], ALU.mult), reads=[pb, sb], writes=[t1b])
        P.op("dve", lambda e: e.tensor_tensor(mb[:, fc, c0:c1], t1[:, :], mt[:, c0:c1], ALU.add), reads=[t1b, mtb], writes=[mb_b[fc]])

    emit_linear(P, psr, ring, w_up_attn, 16, D, lambda k, c0, c1: ob[:, k, c0:c1], ob_b, ntok, evac_up)

    def evac_o(fc, n, pt, pb):
        c0, c1 = n * 512, (n + 1) * 512
        P.op("dve", lambda e: e.scalar_tensor_tensor(x32[:, fc, c0:c1], x32[:, fc, c0:c1], float(ALPHA), pt[:, :], ALU.mult, ALU.add),
             reads=[pb, x_b[fc]], writes=[x_b[fc]])

    emit_linear(P, psr, ring, w_o, 16, D, lambda k, c0, c1: mb[:, k, c0:c1], mb_b, ntok, evac_o)

    gbb = Buf("gbb")
    P.op("dve", lambda e: e.tensor_copy(g1[:, :], g1[:, :]), reads=[g1b, b1b], writes=[gbb])
    emit_ln(P, psr, x32, x_b, ob, ob_b, g1, b1, gbb, ones_sb, ones_b, tmp, stat, ntok,
            out_scale=(float(ALPHA) if variant == "dense" else 1.0))

    if variant == "moe":
        xo32v = xo32.rearrange("(k p) t -> p k t", p=128)
        xobv = xob.rearrange("(k p) t -> p k t", p=128)
        for k in range(16):
            P.dma_out("sp", xo32v[:, k, :], x32[:, k, :], x_b[k])
            P.dma_out("act", xobv[:, k, :], ob[:, k, :], ob_b[k])
        ntile = ntok // 128
        lg = P.sbuf("lg", [128, ntile, NE], F32)
        mx = P.sbuf("mx", [128, ntile, 8], F32)
        msk = P.sbuf("msk", [128, ntile, NE], F32)
        ex = P.sbuf("ex", [128, ntile, NE], F32)
        ssum = P.sbuf("ssum", [128, ntile], F32)
        wf = P.sbuf("wf", [128, ntile, NE], F32)
        r_b = Buf("router_work")
        for tt in range(ntile):
            pt, pb = psr.next()
            mm_group(P, pt[:, 0:NE], pb, [(x32[:, k, tt * 128:(tt + 1) * 128], rt_sb[:, k, :]) for k in range(16)],
                     reads=[rt_b] + x_b)
            P.op("dve", lambda e, tt=tt, pt=pt: e.tensor_copy(lg[:, tt, :], pt[:, 0:NE]), reads=[pb], writes=[r_b])
            P.op("dve", lambda e, tt=tt: e.max(out=mx[:, tt, :], in_=lg[:, tt, :]), reads=[r_b], writes=[r_b])
            P.op("dve", lambda e, tt=tt: e.tensor_scalar(msk[:, tt, :], lg[:, tt, :], mx[:, tt, 1:2], None, ALU.is_ge), reads=[r_b], writes=[r_b])
            P.op("dve", lambda e, tt=tt: e.tensor_scalar(lg[:, tt, :], lg[:, tt, :], mx[:, tt, 0:1], None, ALU.subtract), reads=[r_b], writes=[r_b])
            P.op("act", lambda e, tt=tt: e.activation(ex[:, tt, :], lg[:, tt, :], AF.Exp), reads=[r_b], writes=[r_b])
            P.op("dve", lambda e, tt=tt: e.tensor_tensor(ex[:, tt, :], ex[:, tt, :], msk[:, tt, :], ALU.mult), reads=[r_b], writes=[r_b])
            P.op("dve", lambda e, tt=tt: e.tensor_reduce(ssum[:, tt:tt + 1], ex[:, tt, :], AX.X, ALU.add), reads=[r_b], writes=[r_b])
            P.op("dve", lambda e, tt=tt: e.reciprocal(ssum[:, tt:tt + 1], ssum[:, tt:tt + 1]), reads=[r_b], writes=[r_b])
            P.op("dve", lambda e, tt=tt: e.tensor_scalar(wf[:, tt, :], ex[:, tt, :], ssum[:, tt:tt + 1], None, ALU.mult), reads=[r_b], writes=[r_b])
        P.dma_out("sp", wfull.rearrange("(t p) e -> p t e", p=128), wf[:, :, :], r_b)
        return P.finish()

    ring_d = DRing(P, "wd", 2, 2)
    class HR:
        def __init__(self):
            self.i = 0

        def next(self):
            s = self.i % 4
            self.i += 1
            return mb[:, 2 * s:2 * s + 2, :], HRB[s]
    HRB = [Buf("h%d" % s) for s in range(4)]
    for s in range(4):
        HRB[s].r = list(mb_b[2 * s].r) + list(mb_b[2 * s + 1].r)
        HRB[s].w = mb_b[2 * s].w
    emit_ffn(P, psr, ring, ring_d, HR(), tmp, ob, ob_b, x32, x_b, w_gate, w_up, w_down, D_FF, ntok, first_copy=False)
    gbb2 = Buf("gbb2")
    P.op("dve", lambda e: e.tensor_copy(g2[:, :], g2[:, :]), reads=[g2b, b2b], writes=[gbb2])
    emit_ln(P, psr, x32, x_b, ob, ob_b, g2, b2, gbb2, ones_sb, ones_b, tmp, stat, ntok)
    xo32v = xo32.rearrange("(k p) t -> p k t", p=128)
    xobv = xob.rearrange("(k p) t -> p k t", p=128)
    for k in range(16):
        P.dma_out("sp", xo32v[:, k, :], x32[:, k, :], x_b[k])
        P.dma_out("act", xobv[:, k, :], ob[:, k, :], ob_b[k])
    return P.finish()


def build_E(C, BW):
    P = Prog()
    xeT = P.dram_in("xeT", [D, C], BF16)
    w_gate = P.dram_in("w_gate", [D, D_FFE], F32)
    w_up = P.dram_in("w_up", [D, D_FFE], F32)
    w_down = P.dram_in("w_down", [D_FFE, D], F32)
    yT = P.dram_out("yT", [D, C], F32)
    xb = P.sbuf("xb", [128, 16, BW], BF16)
    xb_b = [Buf("xb_%d" % k) for k in range(16)]
    acc = P.sbuf("acc", [128, 16, BW], F32)
    acc_b = [Buf("acc_%d" % k) for k in range(16)]
    ring = WRing(P, "w", 4, 16, 256)
    ring_d = DRing(P, "wd", 2, 2)
    hring = Ring(P, "h", 3, [128, 2, BW], BF16)
    tmp = Ring(P, "tmp", 3, [128, 512], F32)
    psr = PsumRing(P, 8)
    xv = xeT.rearrange("(k p) t -> p k t", p=128)
    yv = yT.rearrange("(k p) t -> p k t", p=128)
    for off in range(0, C, BW):
        for k in range(16):
            P.dma("sp", xb[:, k, :], xv[:, k, off:off + BW], writes=[xb_b[k]])
        emit_ffn(P, psr, ring, ring_d, hring, tmp, xb, xb_b, acc, acc_b, w_gate, w_up, w_down, D_FFE, BW, first_copy=True)
        for k in range(16):
            P.dma_out("sp", yv[:, k, off:off + BW], acc[:, k, :], acc_b[k])
    return P.finish()


def build_F(ntok=TOK):
    P = Prog()
    xT32 = P.dram_in("xT32", [D, ntok], F32)
    y1T = P.dram_in("y1T", [D, ntok], F32)
    y2T = P.dram_in("y2T", [D, ntok], F32)
    wb = P.dram_in("wb", [128, 2 * ntok], F32)
    lnf_g = P.dram_in("lnf_g", [128, 16], F32)
    lnf_b = P.dram_in("lnf_b", [128, 16], F32)
    ones_d = P.dram_in("ones", [128, 128], F32)
    xo32 = P.dram_out("xo32", [D, ntok], F32)
    xob = P.dram_out("xob", [D, ntok], BF16)
    x32 = P.sbuf("x32", [128, 16, ntok], F32)
    x_b = [Buf("x32_%d" % k) for k in range(16)]
    ob = P.sbuf("ob", [128, 16, ntok], BF16)
    ob_b = [Buf("ob_%d" % k) for k in range(16)]
    y1r = Ring(P, "y1r", 2, [128, ntok], F32)
    y2r = Ring(P, "y2r", 2, [128, ntok], F32)
    tmp = Ring(P, "tmp", 4, [128, 512], F32)
    stat = mk_stat(P)
    psr = PsumRing(P, 8)
    ones_sb, ones_b = load_small(P, "ones_sb", ones_d[:, :], [128, 128])
    g2, g2b = load_small(P, "lnf_g_sb", lnf_g[:, :], [128, 16])
    b2, b2b = load_small(P, "lnf_b_sb", lnf_b[:, :], [128, 16])
    wb_sb, wb_b = load_small(P, "wb_sb", wb[:, :], [128, 2 * ntok])
    xv = xT32.rearrange("(k p) t -> p k t", p=128)
    for k in range(16):
        P.dma("act", x32[:, k, :], xv[:, k, :], writes=[x_b[k]])

    def one(k):
        y1, y1b = y1r.next()
        y2, y2b = y2r.next()
        P.dma("sp", y1[:, :], y1T[k * 128:(k + 1) * 128, :], writes=[y1b])
        P.dma("sp", y2[:, :], y2T[k * 128:(k + 1) * 128, :], writes=[y2b])
        P.op("dve", lambda e: e.tensor_tensor(y1[:, :], y1[:, :], wb_sb[:, 0:ntok], ALU.mult), reads=[y1b, wb_b], writes=[y1b])
        P.op("pool", lambda e: e.tensor_tensor(y2[:, :], y2[:, :], wb_sb[:, ntok:2 * ntok], ALU.mult), reads=[y2b, wb_b], writes=[y2b])
        P.op("dve", lambda e: e.scalar_tensor_tensor(x32[:, k, :], x32[:, k, :], float(ALPHA), y1[:, :], ALU.mult, ALU.add),
             reads=[x_b[k], y1b], writes=[x_b[k]])
        P.op("dve", lambda e: e.tensor_tensor(x32[:, k, :], x32[:, k, :], y2[:, :], ALU.add), reads=[x_b[k], y2b], writes=[x_b[k]])

    for k in range(16):
        one(k)
    gbb = Buf("gbb")
    P.op("dve", lambda e: e.tensor_copy(g2[:, :], g2[:, :]), reads=[g2b, b2b], writes=[gbb])
    emit_ln(P, psr, x32, x_b, ob, ob_b, g2, b2, gbb, ones_sb, ones_b, tmp, stat, ntok)
    xo32v = xo32.rearrange("(k p) t -> p k t", p=128)
    xobv = xob.rearrange("(k p) t -> p k t", p=128)
    for k in range(16):
        P.dma_out("sp", xo32v[:, k, :], x32[:, k, :], x_b[k])
        P.dma_out("act", xobv[:, k, :], ob[:, k, :], ob_b[k])
    return P.finish()


def build_L(ntok=TOK):
    P = Prog()
    xT32 = P.dram_in("xT32", [D, ntok], F32)
    ln_g = P.dram_in("ln_g", [128, 16], F32)
    ln_b = P.dram_in("ln_b", [128, 16], F32)
    ones_d = P.dram_in("ones", [128, 128], F32)
    xo32 = P.dram_out("xo32", [D, ntok], F32)
    xob = P.dram_out("xob", [D, ntok], BF16)
    x32 = P.sbuf("x32", [128, 16, ntok], F32)
    x_b = [Buf("x32_%d" % k) for k in range(16)]
    ob = P.sbuf("ob", [128, 16, ntok], BF16)
    ob_b = [Buf("ob_%d" % k) for k in range(16)]
    tmp = Ring(P, "tmp", 4, [128, 512], F32)
    stat = mk_stat(P)
    psr = PsumRing(P, 8)
    ones_sb, ones_b = load_small(P, "ones_sb", ones_d[:, :], [128, 128])
    g2, g2b = load_small(P, "ln_g_sb", ln_g[:, :], [128, 16])
    b2, b2b = load_small(P, "ln_b_sb", ln_b[:, :], [128, 16])
    xv = xT32.rearrange("(k p) t -> p k t", p=128)
    for k in range(16):
        P.dma("act", x32[:, k, :], xv[:, k, :], writes=[x_b[k]])
    gbb = Buf("gbb")
    P.op("dve", lambda e: e.tensor_copy(g2[:, :], g2[:, :]), reads=[g2b, b2b], writes=[gbb])
    emit_ln(P, psr, x32, x_b, ob, ob_b, g2, b2, gbb, ones_sb, ones_b, tmp, stat, ntok)
    xo32v = xo32.rearrange("(k p) t -> p k t", p=128)
    xobv = xob.rearrange("(k p) t -> p k t", p=128)
    for k in range(16):
        P.dma_out("sp", xo32v[:, k, :], x32[:, k, :], x_b[k])
        P.dma_out("act", xobv[:, k, :], ob[:, k, :], ob_b[k])
    return P.finish()


_PROGS = {}


def _prog(key, builder, *a):
    if key not in _PROGS:
        _PROGS[key] = builder(*a)
    return _PROGS[key]


def _lay16(v):
    return np.ascontiguousarray(np.asarray(v, np.float32).reshape(16, 128).T)


def _cnt_tab(core):
    tab = np.zeros((128, 4 * HALO), np.float32)
    for g, w in enumerate(POOL_WINDOWS):
        for i in range(HALO):
            tab[:, g * HALO + i] = 1.0 / min(core * TOK + i + 1, w)
    return tab


def kernel(x, ln_in_g, ln_in_b, w_in, pool_w, pool_scale, w_up_pool, w_up_attn, w_o,
           ln_mix_g, ln_mix_b, ffn_w_gate, ffn_w_up, ffn_w_down, moe_router, moe_w_gate,
           moe_w_up, moe_w_down, ln_ffn_g, ln_ffn_b):
    f32 = lambda a: np.ascontiguousarray(np.asarray(a, np.float32))
    ones = np.ones((128, 128), np.float32)
    cmask = causal_mask_tile()
    x2 = np.asarray(x, np.float32).reshape(SEQ, D)

    ins = [{"xT32": np.ascontiguousarray(x2[c * TOK:(c + 1) * TOK].T), "ln_g": _lay16(ln_in_g), "ln_b": _lay16(ln_in_b),
            "ones": ones} for c in range(NCORES)]
    res = run_prog(_prog("L", build_L), ins)
    x32 = [r["xo32"] for r in res]
    xb = [r["xob"] for r in res]

    for l in range(DEPTH):
        ins = []
        w_in_l = f32(w_in[l])
        pw_l = f32(pool_w[l]).reshape(4 * 256, 256)
        psc_l = np.ascontiguousarray(np.asarray(pool_scale[l], np.float32).reshape(8, 128).T)
        wup_l = f32(w_up_pool[l])
        for c in range(NCORES):
            halo = xb[c - 1][:, TOK - HALO:] if c > 0 else np.zeros((D, HALO), NPBF)
            ins.append({"xT": np.ascontiguousarray(np.concatenate([halo, xb[c]], axis=1)), "w_in": w_in_l, "pool_w": pw_l,
                        "pool_scale": psc_l, "cnt_tab": _cnt_tab(c), "w_up_pool": wup_l})
        resA = run_prog(_prog("A", build_A), ins)
        del ins, w_in_l
        qkv = np.concatenate([r["qkvT"] for r in resA], axis=1)
        ins = []
        for c in range(NCORES):
            r0 = c * 256
            vT = qkv[2 * D + r0:2 * D + r0 + 256]
            v = np.ascontiguousarray(vT.reshape(2, 128, SEQ).transpose(0, 2, 1)).reshape(2 * SEQ, 128)
            ins.append({"qT": np.ascontiguousarray(qkv[r0:r0 + 256]), "kT": np.ascontiguousarray(qkv[D + r0:D + r0 + 256]),
                        "v": v, "cmask": cmask})
        resB = run_prog(_prog("B", build_B), ins)
        del ins, qkv
        o_all = np.stack([r["o"].reshape(2, SEQ, 128) for r in resB], 0).reshape(NH, SEQ, 128)
        oT_all = np.ascontiguousarray(o_all.transpose(0, 2, 1)).reshape(D, SEQ)
        i = l // 2
        variant = "dense" if l % 2 == 0 else "moe"
        ins = []
        common = {"w_up_attn": f32(w_up_attn[l]), "w_o": f32(w_o[l]), "lnm_g": _lay16(ln_mix_g[l]), "lnm_b": _lay16(ln_mix_b[l]),
                  "ones": ones}
        if variant == "dense":
            common.update({"w_gate": f32(ffn_w_gate[i]), "w_up": f32(ffn_w_up[i]), "w_down": f32(ffn_w_down[i]),
                           "lnf_g": _lay16(ln_ffn_g[l]), "lnf_b": _lay16(ln_ffn_b[l])})
        else:
            common.update({"router": f32(moe_router[i])})
        for c in range(NCORES):
            d = {"oT": np.ascontiguousarray(oT_all[:, c * TOK:(c + 1) * TOK]), "sgaT": resA[c]["sgaT"], "mpT": resA[c]["mpT"],
                 "xT32": x32[c]}
            d.update(common)
            ins.append(d)
        resC = run_prog(_prog("C" + variant, build_C, variant), ins)
        del ins, common, oT_all, o_all, resA, resB
        x32 = [r["xo32"] for r in resC]
        xb = [r["xob"] for r in resC]
        if variant == "dense":
            continue
        wfull = np.concatenate([r["wfull"] for r in resC], axis=0)
        sel = wfull > 0
        rank = np.cumsum(sel, axis=1)
        xb_all = np.concatenate(xb, axis=1)
        toks = [np.nonzero(sel[:, e])[0] for e in range(NE)]
        cmax = max(1, max(len(t) for t in toks))
        nbat = -(-cmax // 1280)
        BW = -(-(-(-cmax // nbat)) // 128) * 128
        C = nbat * BW
        ins = []
        for e in range(NE):
            xe = np.zeros((D, C), NPBF)
            xe[:, :len(toks[e])] = xb_all[:, toks[e]]
            ins.append({"xeT": xe, "w_gate": f32(moe_w_gate[i][e]), "w_up": f32(moe_w_up[i][e]), "w_down": f32(moe_w_down[i][e])})
        resE = run_prog(_prog("E%d_%d" % (C, BW), build_E, C, BW), ins)
        del ins, xb_all
        y1 = np.zeros((D, SEQ), np.float32)
        y2 = np.zeros((D, SEQ), np.float32)
        w1 = np.zeros((SEQ,), np.float32)
        w2 = np.zeros((SEQ,), np.float32)
        for e in range(NE):
            t = toks[e]
            ye = resE[e]["yT"][:, :len(t)]
            first = rank[t, e] == 1
            second = rank[t, e] == 2
            y1[:, t[first]] = ye[:, first]
            y2[:, t[second]] = ye[:, second]
            w1[t[first]] = wfull[t[first], e]
            w2[t[second]] = wfull[t[second], e]
        del resE
        ins = []
        for c in range(NCORES):
            sl = slice(c * TOK, (c + 1) * TOK)
            wbc = np.ascontiguousarray(np.broadcast_to(np.concatenate([w1[sl], w2[sl]])[None, :], (128, 2 * TOK)))
            ins.append({"xT32": x32[c], "y1T": np.ascontiguousarray(y1[:, sl]), "y2T": np.ascontiguousarray(y2[:, sl]), "wb": wbc,
                        "lnf_g": _lay16(ln_ffn_g[l]), "lnf_b": _lay16(ln_ffn_b[l]), "ones": ones})
        resF = run_prog(_prog("F", build_F), ins)
        del ins, y1, y2
        x32 = [r["xo32"] for r in resF]
        xb = [r["xob"] for r in resF]

    out = np.concatenate([a.T for a in x32], axis=0).reshape(1, SEQ, D)
    return np.ascontiguousarray(out.astype(np.float32))
```

```python
import contextlib
import numpy as np
import ml_dtypes
import concourse.bass as bass
import concourse.mybir as mybir
from concourse.bass_utils import run_bass_kernel_spmd

F32 = mybir.dt.float32
BF16 = mybir.dt.bfloat16
ALU = mybir.AluOpType
AF = mybir.ActivationFunctionType
AX = mybir.AxisListType
NPBF = ml_dtypes.bfloat16

NCORES = 8
D = 2048
SEQ = 8192
DEPTH = 4
TOK = SEQ // NCORES
HALO = 16
POOL_WINDOWS = (2, 4, 8, 16)
POOL_WIDTH = 1024
NH = 16
DH = 128
BLK = 256
NBLK = SEQ // BLK
TOPK = 3
IN_WIDTH = POOL_WIDTH + 3 * D + 2 * D
D_FF = 5632
NE = 8
D_FFE = 7168
ALPHA = (2 * DEPTH) ** 0.25
LN_EPS = 1e-5

COMPUTE = ("pe", "act", "dve", "pool")


class Buf:
    __slots__ = ("name", "w", "r", "dsem")

    def __init__(self, name):
        self.name = name
        self.w = None
        self.r = []
        self.dsem = None


class Prog:
    def __init__(self):
        self.nc = bass.Bass("TRN2", target_bir_lowering=False)
        self.es = contextlib.ExitStack()
        self.streams = {e: [] for e in ("pe", "act", "dve", "pool", "sp")}
        self.sems = {}
        self.count = {}
        self.waited = {e: {} for e in self.streams}
        self.ndsem = 0
        for e in COMPUTE:
            self._mksem("c_" + e)
        self.out_bufs = []

    def _mksem(self, key):
        self.sems[key] = self.es.enter_context(self.nc.semaphore(key))
        self.count[key] = 0
        return key

    def dram_in(self, name, shape, dtype):
        return self.nc.dram_tensor(name, list(shape), dtype, kind="ExternalInput").ap()

    def dram_out(self, name, shape, dtype):
        return self.nc.dram_tensor(name, list(shape), dtype, kind="ExternalOutput").ap()

    def sbuf(self, name, shape, dtype):
        return self.es.enter_context(self.nc.sbuf_tensor(name, list(shape), dtype))

    def psum(self, name, shape, dtype=F32):
        return self.es.enter_context(self.nc.psum_tensor(name, list(shape), dtype))

    def _need(self, eng, ev):
        if ev is None:
            return
        key, val = ev
        if self.waited[eng].get(key, 0) >= val:
            return
        self.waited[eng][key] = val
        self.streams[eng].append(("wait", key, val))

    def _deps(self, eng, reads, writes):
        for b in reads:
            self._need(eng, b.w)
        for b in writes:
            self._need(eng, b.w)
            for ev in b.r:
                self._need(eng, ev)

    def _commit(self, ev, reads, writes):
        for b in reads:
            b.r.append(ev)
        for b in writes:
            b.w = ev
            b.r = []

    def op(self, eng, fn, reads=(), writes=(), signal=True):
        self._deps(eng, reads, writes)
        key = "c_" + eng
        if signal:
            self.count[key] += 1
            ev = (key, self.count[key])
            self.streams[eng].append(("op", fn, key, 1))
            self._commit(ev, reads, writes)
        else:
            self.streams[eng].append(("op", fn, None, 0))

    def dma(self, q, out_ap, in_ap, reads=(), writes=(), cont=False):
        bufs = list(reads) + list(writes)
        owner = bufs[0]
        if owner.dsem is None:
            owner.dsem = self._mksem("d%d_%s" % (self.ndsem, owner.name))
            self.ndsem += 1
        key = owner.dsem
        for b in bufs[1:]:
            assert b.dsem is None or b.dsem == key
            b.dsem = key
        if not cont:
            self._deps(q, reads, writes)
        self.count[key] += 16
        ev = (key, self.count[key])
        self.streams[q].append(("op", lambda e, o=out_ap, i=in_ap: e.dma_start(out=o, in_=i), key, 16))
        self._commit(ev, reads, writes)
        return ev

    def dma_out(self, q, out_ap, in_ap, src):
        ev = self.dma(q, out_ap, in_ap, reads=[src])
        self.final_events = getattr(self, "final_events", {})
        self.final_events[ev[0]] = ev[1]

    def finish(self):
        nc = self.nc
        for key, val in getattr(self, "final_events", {}).items():
            self.streams["sp"].append(("wait", key, val))
        for e in COMPUTE:
            if self.count["c_" + e]:
                self.streams["sp"].append(("wait", "c_" + e, self.count["c_" + e]))
        sems = self.sems
        streams = self.streams

        def replay(eng_obj, items):
            for it in items:
                if it[0] == "wait":
                    eng_obj.wait_ge(sems[it[1]], it[2])
                else:
                    ins = it[1](eng_obj)
                    if it[2] is not None:
                        ins.then_inc(sems[it[2]], it[3])

        with nc.Block() as block:
            @block.tensor
            def _(e):
                replay(e, streams["pe"])

            @block.scalar
            def _(e):
                replay(e, streams["act"])

            @block.vector
            def _(e):
                replay(e, streams["dve"])

            @block.gpsimd
            def _(e):
                replay(e, streams["pool"])

            @block.sync
            def _(e):
                replay(e, streams["sp"])
        self.es.close()
        return nc


def run_prog(nc, in_maps):
    res = run_bass_kernel_spmd(nc, in_maps, core_ids=list(range(len(in_maps))))
    return res.results


class PsumRing:
    def __init__(self, P, n, name="ps"):
        self.tiles = [P.psum("%s%d" % (name, i), [128, 512]) for i in range(n)]
        self.bufs = [Buf("%s%d" % (name, i)) for i in range(n)]
        self.i = 0

    def next(self):
        i = self.i % len(self.tiles)
        self.i += 1
        return self.tiles[i], self.bufs[i]


class WRing:
    def __init__(self, P, name, nslots, kc, width):
        self.P = P
        self.kc = kc
        self.width = width
        self.tiles = [P.sbuf("%s%d" % (name, i), [128, kc, width], BF16) for i in range(nslots)]
        self.bufs = [Buf("%s%d" % (name, i)) for i in range(nslots)]
        self.i = 0

    def load(self, w_ap, c0, width=None, kc=None, q="pool"):
        width = width or self.width
        kc = kc or self.kc
        i = self.i % len(self.tiles)
        self.i += 1
        t, b = self.tiles[i], self.bufs[i]
        src = w_ap.rearrange("(k p) f -> p k f", p=128)[:, :, c0:c0 + width]
        h = kc // 2 if kc >= 2 else kc
        self.P.dma(q, t[:, 0:h, 0:width], src[:, 0:h, :], writes=[b])
        if h < kc:
            self.P.dma(q, t[:, h:kc, 0:width], src[:, h:kc, :], writes=[b], cont=True)
        return t, b


def mm_group(P, ps_ap, ps_buf, pairs, reads):
    n = len(pairs)
    for i, (l, r) in enumerate(pairs):
        last = i == n - 1
        P.op("pe", lambda e, l=l, r=r, i=i, last=last: e.matmul(ps_ap, l, r, start=(i == 0), stop=last),
             reads=reads if (i == 0 or last) else (), writes=[ps_buf], signal=last)


def build_A():
    P = Prog()
    TH = TOK + HALO
    xT = P.dram_in("xT", [D, TH], BF16)
    w_in = P.dram_in("w_in", [D, IN_WIDTH], F32)
    pool_w = P.dram_in("pool_w", [4 * 256, 256], F32)
    pool_scale = P.dram_in("pool_scale", [128, 8], F32)
    cnt_tab = P.dram_in("cnt_tab", [128, 4 * HALO], F32)
    w_up_pool = P.dram_in("w_up_pool", [POOL_WIDTH, D], F32)
    qkvT = P.dram_out("qkvT", [3 * D, TOK], BF16)
    sgaT = P.dram_out("sgaT", [D, TOK], BF16)
    mpT = P.dram_out("mpT", [D, TOK], BF16)

    x_sb = P.sbuf("x_sb", [128, 16, TH], BF16)
    x_b = Buf("x")
    u_sb = P.sbuf("u_sb", [128, 8, TH], F32)
    u_b = [Buf("u%d" % c) for c in range(8)]
    pooled = P.sbuf("pooled", [128, 8, TOK], BF16)
    pooled_b = [Buf("pl%d" % c) for c in range(8)]
    ypool = P.sbuf("ypool", [128, 8, TOK], BF16)
    ypool_b = [Buf("yp%d" % c) for c in range(8)]
    tmpa = P.sbuf("tmpa", [128, TH], F32)
    tmpb = P.sbuf("tmpb", [128, TH], F32)
    tmpa_b, tmpb_b = Buf("tmpa"), Buf("tmpb")
    pw_sb = P.sbuf("pw_sb", [128, 8, 256], BF16)
    pw_b = Buf("pw")
    ps_sb = P.sbuf("ps_sb", [128, 8], F32)
    ps_b = Buf("psc")
    tab_sb = P.sbuf("tab_sb", [128, 4 * HALO], F32)
    tab_b = Buf("tab")
    NSTG = 4
    stg = [P.sbuf("stg%d" % i, [128, TOK], BF16) for i in range(NSTG)]
    stg_b = [Buf("stg%d" % i) for i in range(NSTG)]
    sg = [P.sbuf("sg%d" % i, [128, 4, TOK], BF16) for i in range(2)]
    sg_b = [Buf("sg%d" % i) for i in range(2)]
    ring = WRing(P, "win", 3, 16, 512)
    ring2 = WRing(P, "wup", 2, 8, 512)
    psr = PsumRing(P, 8)
    state = {"stg": 0, "evac": 0}

    xv = xT.rearrange("(k p) t -> p k t", p=128)
    for k0 in range(0, 16, 4):
        P.dma("sp", x_sb[:, k0:k0 + 4, :], xv[:, k0:k0 + 4, :], writes=[x_b], cont=k0 > 0)
    P.dma("sp", ps_sb[:, :], pool_scale[:, :], writes=[ps_b])
    P.dma("sp", tab_sb[:, :], cnt_tab[:, :], writes=[tab_b])
    P.dma("pool", pw_sb[:, :, :], pool_w.rearrange("(k p) f -> p k f", p=128), writes=[pw_b])

    def evac_engine():
        state["evac"] += 1
        return "act" if state["evac"] % 2 else "dve"

    def copy_out(eng, dst, src, reads, writes):
        if eng == "act":
            P.op("act", lambda e: e.activation(dst, src, AF.Copy), reads=reads, writes=writes)
        else:
            P.op("dve", lambda e: e.tensor_copy(dst, src), reads=reads, writes=writes)

    def inproj_group(j, kind):
        wt, wb = ring.load(w_in, j * 512)
        for m in range(4):
            f0 = j * 512 + m * 128
            if kind == "u":
                c = f0 // 128
                for (t0, tn) in ((HALO, 512), (HALO + 512, 512), (0, HALO)):
                    pt, pb = psr.next()
                    mm_group(P, pt[:, 0:tn], pb,
                             [(wt[:, k, m * 128:(m + 1) * 128], x_sb[:, k, t0:t0 + tn]) for k in range(16)],
                             reads=[wb, x_b])
                    copy_out(evac_engine(), u_sb[:, c, t0:t0 + tn], pt[:, 0:tn], [pb], [u_b[c]])
                continue
            if kind == "gp":
                dst_t, dst_b = sg[state["sgi"] % 2], sg_b[state["sgi"] % 2]
            else:
                si = state["stg"] % NSTG
                state["stg"] += 1
                dst_t, dst_b = stg[si], stg_b[si]
            for n in range(2):
                pt, pb = psr.next()
                mm_group(P, pt[:, :], pb,
                         [(wt[:, k, m * 128:(m + 1) * 128], x_sb[:, k, HALO + n * 512:HALO + (n + 1) * 512])
                          for k in range(16)], reads=[wb, x_b])
                if kind == "gp":
                    d = dst_t[:, m, n * 512:(n + 1) * 512]
                    P.op("act", lambda e, d=d, s=pt[:, :]: e.activation(d, s, AF.Sigmoid), reads=[pb], writes=[dst_b])
                elif kind == "ga":
                    d = dst_t[:, n * 512:(n + 1) * 512]
                    P.op("act", lambda e, d=d, s=pt[:, :]: e.activation(d, s, AF.Sigmoid), reads=[pb], writes=[dst_b])
                else:
                    copy_out(evac_engine(), dst_t[:, n * 512:(n + 1) * 512], pt[:, :], [pb], [dst_b])
            if kind == "qkv":
                r0 = f0 - POOL_WIDTH
                P.dma_out("sp", qkvT[r0:r0 + 128, :], dst_t[:, :], dst_b)
            elif kind == "ga":
                r0 = f0 - (POOL_WIDTH + 4 * D)
                P.dma_out("sp", sgaT[r0:r0 + 128, :], dst_t[:, :], dst_b)

    def pool_path():
        for c in range(8):
            g = c // 2
            w = POOL_WINDOWS[g]
            cur, cur_b = u_sb[:, c, :], u_b[c]
            s = 1
            outs = [(tmpa, tmpa_b), (tmpb, tmpb_b)]
            oi = 0
            while s < w:
                ot, ob = outs[oi % 2]
                oi += 1
                P.op("dve", lambda e, o=ot[:, s:TH], a=(cur[:, s:TH]), b=(cur[:, 0:TH - s]): e.tensor_tensor(o, a, b, ALU.add),
                     reads=[cur_b], writes=[ob])
                cur, cur_b = ot, ob
                s *= 2
            P.op("dve", lambda e, o=pooled[:, c, :], a=cur[:, HALO:TH], b=u_sb[:, c, HALO:TH], w=w:
                 e.scalar_tensor_tensor(o, a, 1.0 / w, b, ALU.mult, ALU.subtract),
                 reads=[cur_b, u_b[c]], writes=[pooled_b[c]])
            fix, fix_b = outs[oi % 2]
            P.op("dve", lambda e, o=fix[:, 0:HALO], a=cur[:, HALO:2 * HALO], b=tab_sb[:, g * HALO:(g + 1) * HALO]:
                 e.tensor_tensor(o, a, b, ALU.mult), reads=[cur_b, tab_b], writes=[fix_b])
            P.op("dve", lambda e, o=pooled[:, c, 0:HALO], a=fix[:, 0:HALO], b=u_sb[:, c, HALO:2 * HALO]:
                 e.tensor_tensor(o, a, b, ALU.subtract), reads=[fix_b, u_b[c]], writes=[pooled_b[c]])

    def poolw_mm():
        for c in range(8):
            g = c // 2
            mo = c % 2
            for n in range(2):
                pt, pb = psr.next()
                mm_group(P, pt[:, :], pb,
                         [(pw_sb[:, g * 2 + kk, mo * 128:(mo + 1) * 128], pooled[:, g * 2 + kk, n * 512:(n + 1) * 512])
                          for kk in range(2)], reads=[pw_b, pooled_b[g * 2], pooled_b[g * 2 + 1]])
                P.op("act", lambda e, d=ypool[:, c, n * 512:(n + 1) * 512], s=pt[:, :], sc=ps_sb[:, c:c + 1]:
                     e.activation(d, s, AF.Copy, scale=sc), reads=[pb, ps_b], writes=[ypool_b[c]])

    def uppool_group(j):
        wt, wb = ring2.load(w_up_pool, j * 512)
        sgt, sgb = sg[state["sgi"] % 2], sg_b[state["sgi"] % 2]
        for m in range(4):
            si = state["stg"] % NSTG
            state["stg"] += 1
            for n in range(2):
                pt, pb = psr.next()
                mm_group(P, pt[:, :], pb,
                         [(wt[:, k, m * 128:(m + 1) * 128], ypool[:, k, n * 512:(n + 1) * 512]) for k in range(8)],
                         reads=[wb] + ypool_b)
                P.op("dve", lambda e, d=stg[si][:, n * 512:(n + 1) * 512], a=pt[:, :], b=sgt[:, m, n * 512:(n + 1) * 512]:
                     e.tensor_tensor(d, a, b, ALU.mult), reads=[pb, sgb], writes=[stg_b[si]])
            r0 = j * 512 + m * 128
            P.dma_out("sp", mpT[r0:r0 + 128, :], stg[si][:, :], stg_b[si])

    state["sgi"] = 0
    inproj_group(0, "u")
    inproj_group(1, "u")
    for j in range(2, 6):
        inproj_group(j, "qkv")
    pool_path()
    poolw_mm()
    for j in range(6, 14):
        inproj_group(j, "qkv")
    for jj in range(4):
        state["sgi"] = jj
        inproj_group(14 + jj, "gp")
        uppool_group(jj)
    for j in range(18, 22):
        inproj_group(j, "ga")
    return P.finish()


def build_B(nblk=NBLK, hpc=2):
    P = Prog()
    S = nblk * BLK
    NT = S // 128
    qT = P.dram_in("qT", [hpc * 128, S], BF16)
    kT = P.dram_in("kT", [hpc * 128, S], BF16)
    v = P.dram_in("v", [hpc * S, 128], BF16)
    cmask = P.dram_in("cmask", [128, 512], BF16)
    o = P.dram_out("o", [hpc * S, 128], BF16)

    q_sb = [P.sbuf("q_sb%d" % h, [128, S], BF16) for h in range(hpc)]
    k_sb = [P.sbuf("k_sb%d" % h, [128, S], BF16) for h in range(hpc)]
    v_sb = [P.sbuf("v_sb%d" % h, [128, NT, 130], BF16) for h in range(hpc)]
    q_b = [Buf("q%d" % h) for h in range(hpc)]
    k_b = [Buf("k%d" % h) for h in range(hpc)]
    v_b = [Buf("v%d" % h) for h in range(hpc)]
    cm_sb = P.sbuf("cm_sb", [128, 512], BF16)
    cm_b = Buf("cm")
    km32 = P.sbuf("km32", [128, nblk], F32)
    kmh = P.sbuf("kmh", [128, nblk], BF16)
    kml32 = P.sbuf("kml32", [128, nblk], F32)
    kml = P.sbuf("kml", [128, nblk], BF16)
    km_b = Buf("km")
    NG = 3
    g_sb = [P.sbuf("g_sb%d" % i, [128, 2, 32], F32) for i in range(NG)]
    m_sb = [P.sbuf("m_sb%d" % i, [128, 2, 32], F32) for i in range(NG)]
    mx_sb = [P.sbuf("mx_sb%d" % i, [128, 2, 8], F32) for i in range(NG)]
    g_b = [Buf("g%d" % i) for i in range(NG)]
    m_b = [Buf("m%d" % i) for i in range(NG)]
    acc = [P.sbuf("acc%d" % i, [128, 2, 130], F32) for i in range(NG)]
    acc_b = [Buf("acc%d" % i) for i in range(NG)]
    rc = [P.sbuf("rc%d" % i, [128, 2], F32) for i in range(NG)]
    ob = [P.sbuf("ob%d" % i, [128, 2, 128], BF16) for i in range(NG)]
    ob_b = [Buf("ob%d" % i) for i in range(NG)]
    NPT = 4
    pT = [P.sbuf("pT%d" % i, [128, 512], BF16) for i in range(NPT)]
    pT_b = [Buf("pT%d" % i) for i in range(NPT)]
    ps_s = PsumRing(P, 3, "pss")
    ps_o = PsumRing(P, 3, "pso")
    ps_g = PsumRing(P, 2, "psg")
    scale = float(DH) ** -0.5

    P.dma("sp", cm_sb[:, :], cmask[:, :], writes=[cm_b])
    for h in range(hpc):
        P.dma("sp", q_sb[h][:, :], qT[h * 128:(h + 1) * 128, :], writes=[q_b[h]])
        P.dma("sp", k_sb[h][:, :], kT[h * 128:(h + 1) * 128, :], writes=[k_b[h]])
        P.op("pool", lambda e, t=v_sb[h]: e.memset(t[:, :, 128:130], 1.0), writes=[v_b[h]])
        vv = v[h * S:(h + 1) * S, :].rearrange("(t p) d -> p t d", p=128)
        half = NT // 2
        P.dma("sp", v_sb[h][:, 0:half, 0:128], vv[:, 0:half, :], writes=[v_b[h]])
        P.dma("sp", v_sb[h][:, half:NT, 0:128], vv[:, half:NT, :], writes=[v_b[h]], cont=True)

    def pre_head(h):
        P.op("dve", lambda e, h=h: e.tensor_reduce(km32[:, :], k_sb[h][:, :].rearrange("p (n j) -> p n j", j=BLK), AX.X, ALU.add),
             reads=[k_b[h]], writes=[km_b])
        P.op("dve", lambda e: e.tensor_scalar(km32[:, :], km32[:, :], 1.0 / BLK, None, ALU.mult), reads=[km_b], writes=[km_b])
        P.op("dve", lambda e: e.tensor_copy(kmh[:, :], km32[:, :]), reads=[km_b], writes=[km_b])
        P.op("dve", lambda e: e.tensor_tensor(kml32[:, :], km32[:, :], kmh[:, :], ALU.subtract), reads=[km_b], writes=[km_b])
        P.op("dve", lambda e: e.tensor_copy(kml[:, :], kml32[:, :]), reads=[km_b], writes=[km_b])

    def pre_qb(h, qb, gi):
        q0 = qb * BLK
        if qb <= TOPK:
            return
        gt, gb = ps_g.next()
        for t in range(2):
            ql = q_sb[h][:, q0 + t * 128:q0 + (t + 1) * 128]
            mm_group(P, gt[:, t * 32:t * 32 + qb], gb, [(ql, kmh[:, 0:qb]), (ql, kml[:, 0:qb])], reads=[q_b[h], km_b])
        P.op("dve", lambda e: e.memset(g_sb[gi][:, :, :], -1e30), writes=[g_b[gi]])
        P.op("dve", lambda e: e.tensor_copy(
            g_sb[gi][:, :, 0:qb], gt[:, 0:64].rearrange("p (t n) -> p t n", n=32)[:, :, 0:qb]), reads=[gb], writes=[g_b[gi]])
        for t in range(2):
            w8 = max(qb, 8)
            P.op("dve", lambda e, t=t, w8=w8: e.max(out=mx_sb[gi][:, t, :], in_=g_sb[gi][:, t, 0:w8]),
                 reads=[g_b[gi]], writes=[m_b[gi]])
            P.op("dve", lambda e, t=t: e.tensor_scalar(
                m_sb[gi][:, t, 0:qb], g_sb[gi][:, t, 0:qb], mx_sb[gi][:, t, 2:3], None, ALU.is_ge),
                reads=[g_b[gi], m_b[gi]], writes=[m_b[gi]])

    cnt = {"pt": 0}

    def stage1(h, qb, n):
        q0 = qb * BLK
        st, sb = ps_s.next()
        for kh in range(2):
            k0 = n * BLK + kh * 128
            P.op("pe", lambda e, kh=kh, k0=k0: e.matmul(
                st[:, kh * 256:(kh + 1) * 256], k_sb[h][:, k0:k0 + 128], q_sb[h][:, q0:q0 + 256],
                start=True, stop=True), reads=[k_b[h], q_b[h]] if kh == 0 else (), writes=[sb], signal=(kh == 1))
        pi = cnt["pt"] % NPT
        cnt["pt"] += 1
        P.op("act", lambda e: e.activation(pT[pi][:, :], st[:, :], AF.Exp, scale=scale), reads=[sb], writes=[pT_b[pi]])
        if n == qb:
            P.op("pool", lambda e: e.tensor_tensor(pT[pi][:, :], pT[pi][:, :], cm_sb[:, :], ALU.mult),
                 reads=[pT_b[pi], cm_b], writes=[pT_b[pi]])
        return pi

    def stage2(h, qb, n, gi, pi, last):
        q0 = qb * BLK
        ot, otb = ps_o.next()
        for t in range(2):
            for kh in range(2):
                P.op("pe", lambda e, t=t, kh=kh: e.matmul(
                    ot[:, t * 130:t * 130 + 129], pT[pi][:, kh * 256 + t * 128:kh * 256 + (t + 1) * 128],
                    v_sb[h][:, n * 2 + kh, 0:129], start=(kh == 0), stop=(kh == 1)),
                    reads=[pT_b[pi], v_b[h]] if (t == 0 and kh == 0) else (), writes=[otb],
                    signal=(t == 1 and kh == 1))
        if n == qb:
            P.op("dve", lambda e: e.tensor_copy(
                acc[gi][:, :, 0:129], ot[:, 0:260].rearrange("p (t c) -> p t c", c=130)[:, :, 0:129]),
                reads=[otb], writes=[acc_b[gi]])
        elif qb <= TOPK:
            P.op("dve", lambda e: e.tensor_tensor(
                acc[gi][:, :, 0:129], acc[gi][:, :, 0:129],
                ot[:, 0:260].rearrange("p (t c) -> p t c", c=130)[:, :, 0:129], ALU.add),
                reads=[otb, acc_b[gi]], writes=[acc_b[gi]])
        else:
            for t in range(2):
                P.op("dve", lambda e, t=t: e.scalar_tensor_tensor(
                    acc[gi][:, t, 0:129], ot[:, t * 130:t * 130 + 129], m_sb[gi][:, t, n:n + 1],
                    acc[gi][:, t, 0:129], ALU.mult, ALU.add),
                    reads=[otb, acc_b[gi], m_b[gi]], writes=[acc_b[gi]])
        if not last:
            return
        P.op("dve", lambda e: e.reciprocal(rc[gi][:, :], acc[gi][:, :, 128]), reads=[acc_b[gi]], writes=[acc_b[gi]])
        for t in range(2):
            P.op("dve", lambda e, t=t: e.tensor_scalar(
                ob[gi][:, t, :], acc[gi][:, t, 0:128], rc[gi][:, t:t + 1], None, ALU.mult),
                reads=[acc_b[gi]], writes=[ob_b[gi]])
        dst = o[h * S + q0:h * S + q0 + 256, :].rearrange("(t p) d -> p t d", p=128)
        P.dma_out("sp", dst, ob[gi][:, :, :], ob_b[gi])

    LOOK = 2
    pend = []
    for h in range(hpc):
        for qb in range(nblk):
            gi = (h * nblk + qb) % NG
            order = [qb] + list(range(qb))
            for j, n in enumerate(order):
                if j == 0:
                    if qb == 0:
                        pre_head(h)
                    pre_qb(h, qb, gi)
                pi = stage1(h, qb, n)
                pend.append((h, qb, n, gi, pi, j == len(order) - 1))
                if len(pend) > LOOK:
                    stage2(*pend.pop(0))
    while pend:
        stage2(*pend.pop(0))
    return P.finish()


def causal_mask_tile():
    m = np.zeros((128, 512), np.float32)
    p = np.arange(128)[:, None]
    for kh in range(2):
        qq = np.arange(256)[None, :]
        m[:, kh * 256:(kh + 1) * 256] = (kh * 128 + p <= qq)
    return m.astype(NPBF)


class Ring:
    def __init__(self, P, name, n, shape, dtype):
        self.tiles = [P.sbuf("%s%d" % (name, i), shape, dtype) for i in range(n)]
        self.bufs = [Buf("%s%d" % (name, i)) for i in range(n)]
        self.i = 0

    def next(self):
        i = self.i % len(self.tiles)
        self.i += 1
        return self.tiles[i], self.bufs[i]


def emit_linear(P, psr, ring, w_ap, kc, nout, rhs_fn, rhs_bufs, ntok, evac, gw=256):
    for j in range(nout // gw):
        wt, wb = ring.load(w_ap, j * gw, width=gw, kc=kc)
        for m in range(gw // 128):
            for n in range(ntok // 512):
                pt, pb = psr.next()
                mm_group(P, pt[:, :], pb,
                         [(wt[:, k, m * 128:(m + 1) * 128], rhs_fn(k, n * 512, (n + 1) * 512)) for k in range(kc)],
                         reads=[wb] + list(rhs_bufs))
                evac(j * (gw // 128) + m, n, pt, pb)


def emit_ln(P, psr, x32, x_b, xb, xb_b, g_sb, b_sb, gb_b, ones_sb, ones_b, tmp, stat, ntok, out_scale=1.0):
    st_mean, st_rstd, st_msq, st_s1 = stat["mean"], stat["rstd"], stat["msq"], stat["s1"]
    st_b = stat["buf"]
    onesb, sqr = stat["onesb"], stat["sqr"]
    if "onesb_init" not in stat:
        stat["onesb_init"] = True
        P.op("dve", lambda e: e.tensor_copy(onesb[:, :], ones_sb[:, :]), reads=[ones_b], writes=[stat["onesb_b"]])
    gs, bs = g_sb, b_sb
    if out_scale != 1.0:
        gs, bs = stat["gs"], stat["bs"]
        P.op("dve", lambda e: e.tensor_scalar(gs[:, :], g_sb[:, :], float(out_scale), None, ALU.mult), reads=[gb_b], writes=[stat["gsb"]])
        P.op("dve", lambda e: e.tensor_scalar(bs[:, :], b_sb[:, :], float(out_scale), None, ALU.mult), reads=[gb_b], writes=[stat["gsb"]])
    gsb = stat["gsb"]

    def one(n):
        c0, c1 = n * 512, (n + 1) * 512
        P.op("dve", lambda e: e.tensor_reduce(st_s1[:, :], x32[:, :, c0:c1].rearrange("p k t -> p t k"), AX.X, ALU.add),
             reads=list(x_b), writes=[st_b])
        p1, p1b = psr.next()
        mm_group(P, p1[:, :], p1b, [(ones_sb[:, :], st_s1[:, :])], reads=[ones_b, st_b])
        p2, p2b = psr.next()
        for k in range(16):
            sq, sqb = sqr.next()
            P.op("act", lambda e, sq=sq, k=k: e.activation(sq[:, :], x32[:, k, c0:c1], AF.Square), reads=[x_b[k]], writes=[sqb])
            P.op("pe", lambda e, sq=sq, k=k: e.matmul(p2[:, :], onesb[:, :], sq[:, :], start=(k == 0), stop=(k == 15)),
                 reads=[sqb, stat["onesb_b"]], writes=[p2b], signal=True)
        P.op("dve", lambda e: e.tensor_scalar(st_mean[:, :], p1[:, :], 1.0 / D, None, ALU.mult), reads=[p1b], writes=[st_b])
        P.op("dve", lambda e: e.tensor_tensor(st_msq[:, :], st_mean[:, :], st_mean[:, :], ALU.mult), reads=[st_b], writes=[st_b])
        P.op("dve", lambda e: e.scalar_tensor_tensor(st_msq[:, :], p2[:, :], 1.0 / D, st_msq[:, :], ALU.mult, ALU.subtract),
             reads=[p2b, st_b], writes=[st_b])
        P.op("dve", lambda e: e.tensor_scalar(st_msq[:, :], st_msq[:, :], LN_EPS, None, ALU.add), reads=[st_b], writes=[st_b])
        P.op("act", lambda e: e.activation(st_rstd[:, :], st_msq[:, :], AF.Sqrt), reads=[st_b], writes=[st_b])
        P.op("dve", lambda e: e.reciprocal(st_rstd[:, :], st_rstd[:, :]), reads=[st_b], writes=[st_b])
        for k in range(16):
            t1, t1b = tmp.next()
            P.op("dve", lambda e, t1=t1, k=k: e.tensor_tensor(t1[:, :], x32[:, k, c0:c1], st_mean[:, :], ALU.subtract),
                 reads=[x_b[k], st_b], writes=[t1b])
            P.op("pool" if k % 3 else "dve", lambda e, t1=t1: e.tensor_tensor(t1[:, :], t1[:, :], st_rstd[:, :], ALU.mult),
                 reads=[t1b, st_b], writes=[t1b])
            P.op("act", lambda e, t1=t1, k=k: e.activation(x32[:, k, c0:c1], t1[:, :], AF.Identity,
                                                           bias=bs[:, k:k + 1], scale=gs[:, k:k + 1]),
                 reads=[t1b, gb_b, gsb], writes=[x_b[k]])
            P.op("act", lambda e, t1=t1, k=k: e.activation(xb[:, k, c0:c1], t1[:, :], AF.Identity,
                                                           bias=b_sb[:, k:k + 1], scale=g_sb[:, k:k + 1]),
                 reads=[t1b, gb_b], writes=[xb_b[k]])

    for n in range(ntok // 512):
        one(n)


def mk_stat(P):
    return {"mean": P.sbuf("st_mean", [128, 512], F32), "rstd": P.sbuf("st_rstd", [128, 512], F32),
            "msq": P.sbuf("st_msq", [128, 512], F32), "s1": P.sbuf("st_s1", [128, 512], F32), "buf": Buf("stat"),
            "onesb": P.sbuf("onesb", [128, 128], BF16), "onesb_b": Buf("onesb"),
            "sqr": Ring(P, "sqr", 3, [128, 512], BF16),
            "gs": P.sbuf("ln_gs", [128, 16], F32), "bs": P.sbuf("ln_bs", [128, 16], F32), "gsb": Buf("gsb")}


def emit_ffn(P, psr, ring_gu, ring_d, hring, tmp, xb, xb_b, acc, acc_b, wg, wu, wd, dff, ntok, first_copy):
    GW = 256
    chunks = [(c, min(c + 512, ntok)) for c in range(0, ntok, 512)]
    for j in range(dff // GW):
        wgt, wgb = ring_gu.load(wg, j * GW, width=GW, kc=16)
        wut, wub = ring_gu.load(wu, j * GW, width=GW, kc=16)
        ht, hb = hring.next()
        for m in range(GW // 128):
            for (c0, c1) in chunks:
                w = c1 - c0
                pg, pgb = psr.next()
                mm_group(P, pg[:, 0:w], pgb, [(wgt[:, k, m * 128:(m + 1) * 128], xb[:, k, c0:c1]) for k in range(16)],
                         reads=[wgb] + list(xb_b))
                pu, pub = psr.next()
                mm_group(P, pu[:, 0:w], pub, [(wut[:, k, m * 128:(m + 1) * 128], xb[:, k, c0:c1]) for k in range(16)],
                         reads=[wub] + list(xb_b))
                s, sb = tmp.next()
                P.op("act", lambda e, s=s, pg=pg, w=w: e.activation(s[:, 0:w], pg[:, 0:w], AF.Silu), reads=[pgb], writes=[sb])
                P.op("dve", lambda e, ht=ht, m=m, s=s, pu=pu, c0=c0, c1=c1, w=w: e.tensor_tensor(ht[:, m, c0:c1], s[:, 0:w], pu[:, 0:w], ALU.mult),
                     reads=[sb, pub], writes=[hb])
        wdt, wdb = ring_d.load_rows(wd, j * GW, GW // 128)
        for fo in range(16):
            for (c0, c1) in chunks:
                w = c1 - c0
                pt, pb = psr.next()
                mm_group(P, pt[:, 0:w], pb, [(wdt[:, kk, fo * 128:(fo + 1) * 128], ht[:, kk, c0:c1]) for kk in range(GW // 128)],
                         reads=[wdb, hb])
                if first_copy and j == 0:
                    P.op("dve", lambda e, fo=fo, pt=pt, c0=c0, c1=c1, w=w: e.tensor_copy(acc[:, fo, c0:c1], pt[:, 0:w]),
                         reads=[pb], writes=[acc_b[fo]])
                else:
                    P.op("dve", lambda e, fo=fo, pt=pt, c0=c0, c1=c1, w=w: e.tensor_tensor(acc[:, fo, c0:c1], acc[:, fo, c0:c1], pt[:, 0:w], ALU.add),
                         reads=[pb, acc_b[fo]], writes=[acc_b[fo]])


class DRing:
    def __init__(self, P, name, nslots, rk):
        self.P = P
        self.tiles = [P.sbuf("%s%d" % (name, i), [128, rk, D], BF16) for i in range(nslots)]
        self.bufs = [Buf("%s%d" % (name, i)) for i in range(nslots)]
        self.i = 0

    def load_rows(self, w_ap, r0, rk, q="pool"):
        i = self.i % len(self.tiles)
        self.i += 1
        t, b = self.tiles[i], self.bufs[i]
        src = w_ap[r0:r0 + rk * 128, :].rearrange("(k p) f -> p k f", p=128)
        self.P.dma(q, t[:, 0:rk, :], src, writes=[b])
        return t, b


def load_small(P, name, dram_ap, shape, dtype=F32, q="sp"):
    t = P.sbuf(name, shape, dtype)
    b = Buf(name)
    P.dma(q, t[tuple(slice(None) for _ in shape)], dram_ap, writes=[b])
    return t, b


def build_C(variant, ntok=TOK):
    P = Prog()
    oT = P.dram_in("oT", [D, ntok], BF16)
    sgaT = P.dram_in("sgaT", [D, ntok], BF16)
    mpT = P.dram_in("mpT", [D, ntok], BF16)
    xT32 = P.dram_in("xT32", [D, ntok], F32)
    w_up_attn = P.dram_in("w_up_attn", [D, D], F32)
    w_o = P.dram_in("w_o", [D, D], F32)
    lnm_g = P.dram_in("lnm_g", [128, 16], F32)
    lnm_b = P.dram_in("lnm_b", [128, 16], F32)
    ones_d = P.dram_in("ones", [128, 128], F32)
    if variant == "dense":
        w_gate = P.dram_in("w_gate", [D, D_FF], F32)
        w_up = P.dram_in("w_up", [D, D_FF], F32)
        w_down = P.dram_in("w_down", [D_FF, D], F32)
        lnf_g = P.dram_in("lnf_g", [128, 16], F32)
        lnf_b = P.dram_in("lnf_b", [128, 16], F32)
    else:
        router = P.dram_in("router", [D, NE], F32)
        wfull = P.dram_out("wfull", [ntok, NE], F32)
    xo32 = P.dram_out("xo32", [D, ntok], F32)
    xob = P.dram_out("xob", [D, ntok], BF16)

    x32 = P.sbuf("x32", [128, 16, ntok], F32)
    x_b = [Buf("x32_%d" % k) for k in range(16)]
    ob = P.sbuf("ob", [128, 16, ntok], BF16)
    ob_b = [Buf("ob_%d" % k) for k in range(16)]
    mb = P.sbuf("mb", [128, 16, ntok], BF16)
    mb_b = [Buf("mb_%d" % k) for k in range(16)]
    ring = WRing(P, "w", 4, 16, 256)
    psr = PsumRing(P, 8)
    tmp = Ring(P, "tmp", 4, [128, 512], F32)
    stat = mk_stat(P)
    sgr = Ring(P, "sgr", 2, [128, ntok], BF16)
    mpr = Ring(P, "mpr", 2, [128, ntok], BF16)
    ones_sb, ones_b = load_small(P, "ones_sb", ones_d[:, :], [128, 128])
    g1, g1b = load_small(P, "lnm_g_sb", lnm_g[:, :], [128, 16])
    b1, b1b = load_small(P, "lnm_b_sb", lnm_b[:, :], [128, 16])
    gb1 = Buf("gb1")
    gb1.w = None
    if variant == "dense":
        g2, g2b = load_small(P, "lnf_g_sb", lnf_g[:, :], [128, 16])
        b2, b2b = load_small(P, "lnf_b_sb", lnf_b[:, :], [128, 16])
    else:
        rt_sb = P.sbuf("rt_sb", [128, 16, NE], F32)
        rt_b = Buf("rt")
        P.dma("sp", rt_sb[:, :, :], router.rearrange("(k p) e -> p k e", p=128), writes=[rt_b])

    ov = oT.rearrange("(k p) t -> p k t", p=128)
    xv = xT32.rearrange("(k p) t -> p k t", p=128)
    for k in range(16):
        P.dma("sp", ob[:, k, :], ov[:, k, :], writes=[ob_b[k]])
    for k in range(16):
        P.dma("act", x32[:, k, :], xv[:, k, :], writes=[x_b[k]])

    cur = {}

    def evac_up(fc, n, pt, pb):
        if n == 0:
            st, sb = sgr.next()
            mt, mtb = mpr.next()
            P.dma("sp", st[:, :], sgaT[fc * 128:(fc + 1) * 128, :], writes=[sb])
            P.dma("sp", mt[:, :], mpT[fc * 128:(fc + 1) * 128, :], writes=[mtb])
            cur["s"] = (st, sb, mt, mtb)
        st, sb, mt, mtb = cur["s"]
        c0, c1 = n * 512, (n + 1) * 512
        t1, t1b = tmp.next()
        P.op("dve", lambda e: e.tensor_tensor(t1[:, :], pt[:, :], st[:, c0:c1], ALU.mult), reads=[pb, sb], writes=[t1b])
        P.op("dve", lambda e: e.tensor_tensor(mb[:, fc, c0:c1], t1[:, :], mt[:, c0:c1], ALU.add), reads=[t1b, mtb], writes=[mb_b[fc]])

    emit_linear(P, psr, ring, w_up_attn, 16, D, lambda k, c0, c1: ob[:, k, c0:c1], ob_b, ntok, evac_up)

    def evac_o(fc, n, pt, pb):
        c0, c1 = n * 512, (n + 1) * 512
        P.op("dve", lambda e: e.scalar_tensor_tensor(x32[:, fc, c0:c1], x32[:, fc, c0:c1], float(ALPHA), pt[:, :], ALU.mult, ALU.add),
             reads=[pb, x_b[fc]], writes=[x_b[fc]])

    emit_linear(P, psr, ring, w_o, 16, D, lambda k, c0, c1: mb[:, k, c0:c1], mb_b, ntok, evac_o)

    gbb = Buf("gbb")
    P.op("dve", lambda e: e.tensor_copy(g1[:, :], g1[:, :]), reads=[g1b, b1b], writes=[gbb])
    emit_ln(P, psr, x32, x_b, ob, ob_b, g1, b1, gbb, ones_sb, ones_b, tmp, stat, ntok,
            out_scale=(float(ALPHA) if variant == "dense" else 1.0))

    if variant == "moe":
        xo32v = xo32.rearrange("(k p) t -> p k t", p=128)
        xobv = xob.rearrange("(k p) t -> p k t", p=128)
        for k in range(16):
            P.dma_out("sp", xo32v[:, k, :], x32[:, k, :], x_b[k])
            P.dma_out("act", xobv[:, k, :], ob[:, k, :], ob_b[k])
        ntile = ntok // 128
        lg = P.sbuf("lg", [128, ntile, NE], F32)
        mx = P.sbuf("mx", [128, ntile, 8], F32)
        msk = P.sbuf("msk", [128, ntile, NE], F32)
        ex = P.sbuf("ex", [128, ntile, NE], F32)
        ssum = P.sbuf("ssum", [128, ntile], F32)
        wf = P.sbuf("wf", [128, ntile, NE], F32)
        r_b = Buf("router_work")
        for tt in range(ntile):
            pt, pb = psr.next()
            mm_group(P, pt[:, 0:NE], pb, [(x32[:, k, tt * 128:(tt + 1) * 128], rt_sb[:, k, :]) for k in range(16)],
                     reads=[rt_b] + x_b)
            P.op("dve", lambda e, tt=tt, pt=pt: e.tensor_copy(lg[:, tt, :], pt[:, 0:NE]), reads=[pb], writes=[r_b])
            P.op("dve", lambda e, tt=tt: e.max(out=mx[:, tt, :], in_=lg[:, tt, :]), reads=[r_b], writes=[r_b])
            P.op("dve", lambda e, tt=tt: e.tensor_scalar(msk[:, tt, :], lg[:, tt, :], mx[:, tt, 1:2], None, ALU.is_ge), reads=[r_b], writes=[r_b])
            P.op("dve", lambda e, tt=tt: e.tensor_scalar(lg[:, tt, :], lg[:, tt, :], mx[:, tt, 0:1], None, ALU.subtract), reads=[r_b], writes=[r_b])
            P.op("act", lambda e, tt=tt: e.activation(ex[:, tt, :], lg[:, tt, :], AF.Exp), reads=[r_b], writes=[r_b])
            P.op("dve", lambda e, tt=tt: e.tensor_tensor(ex[:, tt, :], ex[:, tt, :], msk[:, tt, :], ALU.mult), reads=[r_b], writes=[r_b])
            P.op("dve", lambda e, tt=tt: e.tensor_reduce(ssum[:, tt:tt + 1], ex[:, tt, :], AX.X, ALU.add), reads=[r_b], writes=[r_b])
            P.op("dve", lambda e, tt=tt: e.reciprocal(ssum[:, tt:tt + 1], ssum[:, tt:tt + 1]), reads=[r_b], writes=[r_b])
            P.op("dve", lambda e, tt=tt: e.tensor_scalar(wf[:, tt, :], ex[:, tt, :], ssum[:, tt:tt + 1], None, ALU.mult), reads=[r_b], writes=[r_b])
        P.dma_out("sp", wfull.rearrange("(t p) e -> p t e", p=128), wf[:, :, :], r_b)
        return P.finish()

    ring_d = DRing(P, "wd", 2, 2)
    class HR:
        def __init__(self):
            self.i = 0

        def next(self):
            s = self.i % 4
            self.i += 1
            return mb[:, 2 * s:2 * s + 2, :], HRB[s]
    HRB = [Buf("h%d" % s) for s in range(4)]
    for s in range(4):
        HRB[s].r = list(mb_b[2 * s].r) + list(mb_b[2 * s + 1].r)
        HRB[s].w = mb_b[2 * s].w
    emit_ffn(P, psr, ring, ring_d, HR(), tmp, ob, ob_b, x32, x_b, w_gate, w_up, w_down, D_FF, ntok, first_copy=False)
    gbb2 = Buf("gbb2")
    P.op("dve", lambda e: e.tensor_copy(g2[:, :], g2[:, :]), reads=[g2b, b2b], writes=[gbb2])
    emit_ln(P, psr, x32, x_b, ob, ob_b, g2, b2, gbb2, ones_sb, ones_b, tmp, stat, ntok)
    xo32v = xo32.rearrange("(k p) t -> p k t", p=128)
    xobv = xob.rearrange("(k p) t -> p k t", p=128)
    for k in range(16):
        P.dma_out("sp", xo32v[:, k, :], x32[:, k, :], x_b[k])
        P.dma_out("act", xobv[:, k, :], ob[:, k, :], ob_b[k])
    return P.finish()


def build_E(C, BW):
    P = Prog()
    xeT = P.dram_in("xeT", [D, C], BF16)
    w_gate = P.dram_in("w_gate", [D, D_FFE], F32)
    w_up = P.dram_in("w_up", [D, D_FFE], F32)
    w_down = P.dram_in("w_down", [D_FFE, D], F32)
    yT = P.dram_out("yT", [D, C], F32)
    xb = P.sbuf("xb", [128, 16, BW], BF16)
    xb_b = [Buf("xb_%d" % k) for k in range(16)]
    acc = P.sbuf("acc", [128, 16, BW], F32)
    acc_b = [Buf("acc_%d" % k) for k in range(16)]
    ring = WRing(P, "w", 4, 16, 256)
    ring_d = DRing(P, "wd", 2, 2)
    hring = Ring(P, "h", 3, [128, 2, BW], BF16)
    tmp = Ring(P, "tmp", 3, [128, 512], F32)
    psr = PsumRing(P, 8)
    xv = xeT.rearrange("(k p) t -> p k t", p=128)
    yv = yT.rearrange("(k p) t -> p k t", p=128)
    for off in range(0, C, BW):
        for k in range(16):
            P.dma("sp", xb[:, k, :], xv[:, k, off:off + BW], writes=[xb_b[k]])
        emit_ffn(P, psr, ring, ring_d, hring, tmp, xb, xb_b, acc, acc_b, w_gate, w_up, w_down, D_FFE, BW, first_copy=True)
        for k in range(16):
            P.dma_out("sp", yv[:, k, off:off + BW], acc[:, k, :], acc_b[k])
    return P.finish()


def build_F(ntok=TOK):
    P = Prog()
    xT32 = P.dram_in("xT32", [D, ntok], F32)
    y1T = P.dram_in("y1T", [D, ntok], F32)
    y2T = P.dram_in("y2T", [D, ntok], F32)
    wb = P.dram_in("wb", [128, 2 * ntok], F32)
    lnf_g = P.dram_in("lnf_g", [128, 16], F32)
    lnf_b = P.dram_in("lnf_b", [128, 16], F32)
    ones_d = P.dram_in("ones", [128, 128], F32)
    xo32 = P.dram_out("xo32", [D, ntok], F32)
    xob = P.dram_out("xob", [D, ntok], BF16)
    x32 = P.sbuf("x32", [128, 16, ntok], F32)
    x_b = [Buf("x32_%d" % k) for k in range(16)]
    ob = P.sbuf("ob", [128, 16, ntok], BF16)
    ob_b = [Buf("ob_%d" % k) for k in range(16)]
    y1r = Ring(P, "y1r", 2, [128, ntok], F32)
    y2r = Ring(P, "y2r", 2, [128, ntok], F32)
    tmp = Ring(P, "tmp", 4, [128, 512], F32)
    stat = mk_stat(P)
    psr = PsumRing(P, 8)
    ones_sb, ones_b = load_small(P, "ones_sb", ones_d[:, :], [128, 128])
    g2, g2b = load_small(P, "lnf_g_sb", lnf_g[:, :], [128, 16])
    b2, b2b = load_small(P, "lnf_b_sb", lnf_b[:, :], [128, 16])
    wb_sb, wb_b = load_small(P, "wb_sb", wb[:, :], [128, 2 * ntok])
    xv = xT32.rearrange("(k p) t -> p k t", p=128)
    for k in range(16):
        P.dma("act", x32[:, k, :], xv[:, k, :], writes=[x_b[k]])

    def one(k):
        y1, y1b = y1r.next()
        y2, y2b = y2r.next()
        P.dma("sp", y1[:, :], y1T[k * 128:(k + 1) * 128, :], writes=[y1b])
        P.dma("sp", y2[:, :], y2T[k * 128:(k + 1) * 128, :], writes=[y2b])
        P.op("dve", lambda e: e.tensor_tensor(y1[:, :], y1[:, :], wb_sb[:, 0:ntok], ALU.mult), reads=[y1b, wb_b], writes=[y1b])
        P.op("pool", lambda e: e.tensor_tensor(y2[:, :], y2[:, :], wb_sb[:, ntok:2 * ntok], ALU.mult), reads=[y2b, wb_b], writes=[y2b])
        P.op("dve", lambda e: e.scalar_tensor_tensor(x32[:, k, :], x32[:, k, :], float(ALPHA), y1[:, :], ALU.mult, ALU.add),
             reads=[x_b[k], y1b], writes=[x_b[k]])
        P.op("dve", lambda e: e.tensor_tensor(x32[:, k, :], x32[:, k, :], y2[:, :], ALU.add), reads=[x_b[k], y2b], writes=[x_b[k]])

    for k in range(16):
        one(k)
    gbb = Buf("gbb")
    P.op("dve", lambda e: e.tensor_copy(g2[:, :], g2[:, :]), reads=[g2b, b2b], writes=[gbb])
    emit_ln(P, psr, x32, x_b, ob, ob_b, g2, b2, gbb, ones_sb, ones_b, tmp, stat, ntok)
    xo32v = xo32.rearrange("(k p) t -> p k t", p=128)
    xobv = xob.rearrange("(k p) t -> p k t", p=128)
    for k in range(16):
        P.dma_out("sp", xo32v[:, k, :], x32[:, k, :], x_b[k])
        P.dma_out("act", xobv[:, k, :], ob[:, k, :], ob_b[k])
    return P.finish()


def build_L(ntok=TOK):
    P = Prog()
    xT32 = P.dram_in("xT32", [D, ntok], F32)
    ln_g = P.dram_in("ln_g", [128, 16], F32)
    ln_b = P.dram_in("ln_b", [128, 16], F32)
    ones_d = P.dram_in("ones", [128, 128], F32)
    xo32 = P.dram_out("xo32", [D, ntok], F32)
    xob = P.dram_out("xob", [D, ntok], BF16)
    x32 = P.sbuf("x32", [128, 16, ntok], F32)
    x_b = [Buf("x32_%d" % k) for k in range(16)]
    ob = P.sbuf("ob", [128, 16, ntok], BF16)
    ob_b = [Buf("ob_%d" % k) for k in range(16)]
    tmp = Ring(P, "tmp", 4, [128, 512], F32)
    stat = mk_stat(P)
    psr = PsumRing(P, 8)
    ones_sb, ones_b = load_small(P, "ones_sb", ones_d[:, :], [128, 128])
    g2, g2b = load_small(P, "ln_g_sb", ln_g[:, :], [128, 16])
    b2, b2b = load_small(P, "ln_b_sb", ln_b[:, :], [128, 16])
    xv = xT32.rearrange("(k p) t -> p k t", p=128)
    for k in range(16):
        P.dma("act", x32[:, k, :], xv[:, k, :], writes=[x_b[k]])
    gbb = Buf("gbb")
    P.op("dve", lambda e: e.tensor_copy(g2[:, :], g2[:, :]), reads=[g2b, b2b], writes=[gbb])
    emit_ln(P, psr, x32, x_b, ob, ob_b, g2, b2, gbb, ones_sb, ones_b, tmp, stat, ntok)
    xo32v = xo32.rearrange("(k p) t -> p k t", p=128)
    xobv = xob.rearrange("(k p) t -> p k t", p=128)
    for k in range(16):
        P.dma_out("sp", xo32v[:, k, :], x32[:, k, :], x_b[k])
        P.dma_out("act", xobv[:, k, :], ob[:, k, :], ob_b[k])
    return P.finish()


_PROGS = {}


def _prog(key, builder, *a):
    if key not in _PROGS:
        _PROGS[key] = builder(*a)
    return _PROGS[key]


def _lay16(v):
    return np.ascontiguousarray(np.asarray(v, np.float32).reshape(16, 128).T)


def _cnt_tab(core):
    tab = np.zeros((128, 4 * HALO), np.float32)
    for g, w in enumerate(POOL_WINDOWS):
        for i in range(HALO):
            tab[:, g * HALO + i] = 1.0 / min(core * TOK + i + 1, w)
    return tab


def kernel(x, ln_in_g, ln_in_b, w_in, pool_w, pool_scale, w_up_pool, w_up_attn, w_o,
           ln_mix_g, ln_mix_b, ffn_w_gate, ffn_w_up, ffn_w_down, moe_router, moe_w_gate,
           moe_w_up, moe_w_down, ln_ffn_g, ln_ffn_b):
    f32 = lambda a: np.ascontiguousarray(np.asarray(a, np.float32))
    ones = np.ones((128, 128), np.float32)
    cmask = causal_mask_tile()
    x2 = np.asarray(x, np.float32).reshape(SEQ, D)

    ins = [{"xT32": np.ascontiguousarray(x2[c * TOK:(c + 1) * TOK].T), "ln_g": _lay16(ln_in_g), "ln_b": _lay16(ln_in_b),
            "ones": ones} for c in range(NCORES)]
    res = run_prog(_prog("L", build_L), ins)
    x32 = [r["xo32"] for r in res]
    xb = [r["xob"] for r in res]

    for l in range(DEPTH):
        ins = []
        w_in_l = f32(w_in[l])
        pw_l = f32(pool_w[l]).reshape(4 * 256, 256)
        psc_l = np.ascontiguousarray(np.asarray(pool_scale[l], np.float32).reshape(8, 128).T)
        wup_l = f32(w_up_pool[l])
        for c in range(NCORES):
            halo = xb[c - 1][:, TOK - HALO:] if c > 0 else np.zeros((D, HALO), NPBF)
            ins.append({"xT": np.ascontiguousarray(np.concatenate([halo, xb[c]], axis=1)), "w_in": w_in_l, "pool_w": pw_l,
                        "pool_scale": psc_l, "cnt_tab": _cnt_tab(c), "w_up_pool": wup_l})
        resA = run_prog(_prog("A", build_A), ins)
        del ins, w_in_l
        qkv = np.concatenate([r["qkvT"] for r in resA], axis=1)
        ins = []
        for c in range(NCORES):
            r0 = c * 256
            vT = qkv[2 * D + r0:2 * D + r0 + 256]
            v = np.ascontiguousarray(vT.reshape(2, 128, SEQ).transpose(0, 2, 1)).reshape(2 * SEQ, 128)
            ins.append({"qT": np.ascontiguousarray(qkv[r0:r0 + 256]), "kT": np.ascontiguousarray(qkv[D + r0:D + r0 + 256]),
                        "v": v, "cmask": cmask})
        resB = run_prog(_prog("B", build_B), ins)
        del ins, qkv
        o_all = np.stack([r["o"].reshape(2, SEQ, 128) for r in resB], 0).reshape(NH, SEQ, 128)
        oT_all = np.ascontiguousarray(o_all.transpose(0, 2, 1)).reshape(D, SEQ)
        i = l // 2
        variant = "dense" if l % 2 == 0 else "moe"
        ins = []
        common = {"w_up_attn": f32(w_up_attn[l]), "w_o": f32(w_o[l]), "lnm_g": _lay16(ln_mix_g[l]), "lnm_b": _lay16(ln_mix_b[l]),
                  "ones": ones}
        if variant == "dense":
            common.update({"w_gate": f32(ffn_w_gate[i]), "w_up": f32(ffn_w_up[i]), "w_down": f32(ffn_w_down[i]),
                           "lnf_g": _lay16(ln_ffn_g[l]), "lnf_b": _lay16(ln_ffn_b[l])})
        else:
            common.update({"router": f32(moe_router[i])})
        for c in range(NCORES):
            d = {"oT": np.ascontiguousarray(oT_all[:, c * TOK:(c + 1) * TOK]), "sgaT": resA[c]["sgaT"], "mpT": resA[c]["mpT"],
                 "xT32": x32[c]}
            d.update(common)
            ins.append(d)
        resC = run_prog(_prog("C" + variant, build_C, variant), ins)
        del ins, common, oT_all, o_all, resA, resB
        x32 = [r["xo32"] for r in resC]
        xb = [r["xob"] for r in resC]
        if variant == "dense":
            continue
        wfull = np.concatenate([r["wfull"] for r in resC], axis=0)
        sel = wfull > 0
        rank = np.cumsum(sel, axis=1)
        xb_all = np.concatenate(xb, axis=1)
        toks = [np.nonzero(sel[:, e])[0] for e in range(NE)]
        cmax = max(1, max(len(t) for t in toks))
        nbat = -(-cmax // 1280)
        BW = -(-(-(-cmax // nbat)) // 128) * 128
        C = nbat * BW
        ins = []
        for e in range(NE):
            xe = np.zeros((D, C), NPBF)
            xe[:, :len(toks[e])] = xb_all[:, toks[e]]
            ins.append({"xeT": xe, "w_gate": f32(moe_w_gate[i][e]), "w_up": f32(moe_w_up[i][e]), "w_down": f32(moe_w_down[i][e])})
        resE = run_prog(_prog("E%d_%d" % (C, BW), build_E, C, BW), ins)
        del ins, xb_all
        y1 = np.zeros((D, SEQ), np.float32)
        y2 = np.zeros((D, SEQ), np.float32)
        w1 = np.zeros((SEQ,), np.float32)
        w2 = np.zeros((SEQ,), np.float32)
        for e in range(NE):
            t = toks[e]
            ye = resE[e]["yT"][:, :len(t)]
            first = rank[t, e] == 1
            second = rank[t, e] == 2
            y1[:, t[first]] = ye[:, first]
            y2[:, t[second]] = ye[:, second]
            w1[t[first]] = wfull[t[first], e]
            w2[t[second]] = wfull[t[second], e]
        del resE
        ins = []
        for c in range(NCORES):
            sl = slice(c * TOK, (c + 1) * TOK)
            wbc = np.ascontiguousarray(np.broadcast_to(np.concatenate([w1[sl], w2[sl]])[None, :], (128, 2 * TOK)))
            ins.append({"xT32": x32[c], "y1T": np.ascontiguousarray(y1[:, sl]), "y2T": np.ascontiguousarray(y2[:, sl]), "wb": wbc,
                        "lnf_g": _lay16(ln_ffn_g[l]), "lnf_b": _lay16(ln_ffn_b[l]), "ones": ones})
        resF = run_prog(_prog("F", build_F), ins)
        del ins, y1, y2
        x32 = [r["xo32"] for r in resF]
        xb = [r["xob"] for r in resF]

    out = np.concatenate([a.T for a in x32], axis=0).reshape(1, SEQ, D)
    return np.ascontiguousarray(out.astype(np.float32))
```
